# Optimizing a Trainium2 kernel written in Bass

```python
import math
import jax
import jax.numpy as jnp
from jax import lax
import numpy as np

D_MODEL = 1024
BATCH = 2
SEQ = 8192
DEPTH = 2

GRID_W = 64
CTX_LEN = 256
CHUNK = 128
CONV_W = 5
RMS_EPS = 1e-6
LN_EPS = 1e-5
ROPE_BASE = 10000.0

SSD_HEADS = 16
SSD_HEAD_DIM = 64
SSD_INNER = SSD_HEADS * SSD_HEAD_DIM
SSD_GROUPS = 4
SSD_STATE = 128
SSD_CONV_DIM = SSD_INNER + 2 * SSD_GROUPS * SSD_STATE

RET_HEADS = 4
RET_QK_DIM = 128
RET_V_DIM = 256
RET_QK = RET_HEADS * RET_QK_DIM
RET_V = RET_HEADS * RET_V_DIM

AB_IN_SIZES = (SSD_INNER, SSD_CONV_DIM, SSD_HEADS, RET_QK, RET_QK, RET_V, RET_V)
AB_IN = sum(AB_IN_SIZES)
AB_SPLITS = tuple(sum(AB_IN_SIZES[:i + 1]) for i in range(len(AB_IN_SIZES) - 1))
AB_OUT = SSD_INNER + RET_V

MLSTM_HEADS = 4
MLSTM_INNER = 2 * D_MODEL
MLSTM_HEAD_DIM = MLSTM_INNER // MLSTM_HEADS
QKV_BLOCK = 4
N_QKV_BLOCKS = MLSTM_INNER // QKV_BLOCK
N_GATES = 4 * MLSTM_HEADS

FFN_HIDDEN = -(-(8 * D_MODEL) // (3 * 256)) * 256
N_EVEN = (DEPTH + 1) // 2
N_ODD = DEPTH // 2

kernel_name = 'hybrid_ssd_retention_mlstm_dit'


def rmsnorm(x, w):
    xf = x.astype(jnp.float32)
    y = xf * lax.rsqrt(jnp.mean(xf * xf, axis=-1, keepdims=True) + RMS_EPS)
    return (y * w).astype(x.dtype)


def head_layernorm(y):
    yf = y.astype(jnp.float32)
    mu = jnp.mean(yf, axis=-1, keepdims=True)
    d = yf - mu
    return d * lax.rsqrt(jnp.mean(d * d, axis=-1, keepdims=True) + LN_EPS)


def modulation(cvec, w, b):
    return jax.nn.silu(cvec) @ w + b


def modulate(hn, shift, scale):
    return hn * (1.0 + scale) + shift


def swiglu(h, w1, w3, w2):
    return (jax.nn.silu(h @ w1) * (h @ w3)) @ w2


def dwconv(x, w, b):
    ch = x.shape[-1]
    y = lax.conv_general_dilated(x, w[:, None, :].astype(x.dtype), window_strides=(1,),
                                 padding=[(CONV_W // 2, CONV_W // 2)],
                                 dimension_numbers=('NWC', 'WIO', 'NWC'), feature_group_count=ch)
    return y + b


def flip(a):
    return jnp.flip(a, axis=1)


def axial_rope(rows):
    r = jnp.repeat(jnp.arange(rows, dtype=jnp.float32), GRID_W)
    col = jnp.tile(jnp.arange(GRID_W, dtype=jnp.float32), rows)
    nf = RET_QK_DIM // 4
    inv = ROPE_BASE ** (-jnp.arange(nf, dtype=jnp.float32) / nf)
    ang = jnp.concatenate([r[:, None] * inv, col[:, None] * inv], axis=-1)
    return jnp.cos(ang), jnp.sin(ang)


def apply_rope(x, cos, sin):
    x1, x2 = jnp.split(x, 2, axis=-1)
    cc, ss = cos[None, :, None, :], sin[None, :, None, :]
    return jnp.concatenate([x1 * cc - x2 * ss, x1 * ss + x2 * cc], axis=-1).astype(x.dtype)


def to_chunks(a):
    b, n = a.shape[:2]
    return jnp.moveaxis(a.reshape(b, n // CHUNK, CHUNK, *a.shape[2:]), 1, 0)


def from_chunks(a):
    nc, b, l = a.shape[:3]
    return jnp.moveaxis(a, 0, 1).reshape(b, nc * l, *a.shape[3:])


def decay_scan(q, k, v, log_a, s0):
    tril = jnp.tril(jnp.ones((CHUNK, CHUNK), bool))[None, :, :, None]

    def body(s, inp):
        qc, kc, vc, ac = inp
        cs = jnp.cumsum(ac, axis=1)
        decay = jnp.exp(jnp.where(tril, cs[:, :, None, :] - cs[:, None, :, :], -jnp.inf))
        scores = jnp.einsum('bihd,bjhd->bijh', qc, kc) * decay
        y = (jnp.einsum('bijh,bjhe->bihe', scores, vc)
             + jnp.einsum('bihd,bhde->bihe', qc * jnp.exp(cs)[..., None], s))
        w_end = jnp.exp(cs[:, -1:, :] - cs)[..., None]
        s_new = (jnp.exp(cs[:, -1, :])[..., None, None] * s
                 + jnp.einsum('bjhd,bjhe->bhde', kc * w_end, vc))
        return s_new, y

    s_fin, ys = lax.scan(body, s0, (to_chunks(q), to_chunks(k), to_chunks(v), to_chunks(log_a)))
    return from_chunks(ys), s_fin


def bidir_decay_scan(q, k, v_f, v_b, la_f, la_b, s_f, s_b):
    y_f, s_f = decay_scan(q, k, v_f, la_f, s_f)
    y_b, s_b = decay_scan(flip(q), flip(k), flip(v_b), flip(la_b), s_b)
    return y_f + flip(y_b), s_f, s_b


def mlstm_scan(q, k, v, i_pre, log_f, state):
    tril = jnp.tril(jnp.ones((CHUNK, CHUNK), bool))[None, :, :, None]

    def body(carry, inp):
        c_mat, n_vec, m = carry
        qc, kc, vc, ic, fc = inp
        bcum = jnp.cumsum(fc, axis=1)
        d = jnp.where(tril, bcum[:, :, None, :] - bcum[:, None, :, :] + ic[:, None, :, :], -jnp.inf)
        prev = bcum + m[:, None, :]
        m_out = jnp.maximum(prev, jnp.max(d, axis=2))
        s = jnp.einsum('bihd,bjhd->bijh', qc, kc) * jnp.exp(d - m_out[:, :, None, :])
        w_prev = jnp.exp(prev - m_out)
        num = (jnp.einsum('bijh,bjhe->bihe', s, vc)
               + w_prev[..., None] * jnp.einsum('bihd,bhde->bihe', qc, c_mat))
        den = jnp.sum(s, axis=2) + w_prev * jnp.einsum('bihd,bhd->bih', qc, n_vec)
        hc = num / jnp.maximum(jnp.abs(den), jnp.exp(-m_out))[..., None]
        b_end = bcum[:, -1, :]
        d_end = b_end[:, None, :] - bcum + ic
        m_new = jnp.maximum(b_end + m, jnp.max(d_end, axis=1))
        w_k = jnp.exp(d_end - m_new[:, None, :])[..., None] * kc
        w_c = jnp.exp(b_end + m - m_new)
        c_new = w_c[..., None, None] * c_mat + jnp.einsum('bjhd,bjhe->bhde', w_k, vc)
        n_new = w_c[..., None] * n_vec + jnp.sum(w_k, axis=1)
        return (c_new, n_new, m_new), hc

    carry, hs = lax.scan(body, state, tuple(map(to_chunks, (q, k, v, i_pre, log_f))))
    return from_chunks(hs), carry


def ab_zero_states(b):
    s_ssd = jnp.zeros((b, SSD_HEADS, SSD_STATE, SSD_HEAD_DIM), jnp.float32)
    s_ret = jnp.zeros((b, RET_HEADS, RET_QK_DIM, RET_V_DIM), jnp.float32)
    return (s_ssd, s_ssd, s_ret, s_ret)


def mlstm_zero_states(b):
    st = (jnp.zeros((b, MLSTM_HEADS, MLSTM_HEAD_DIM, MLSTM_HEAD_DIM), jnp.float32),
          jnp.zeros((b, MLSTM_HEADS, MLSTM_HEAD_DIM), jnp.float32),
          jnp.zeros((b, MLSTM_HEADS), jnp.float32))
    return (st, st)


def ssd_retention_mixer(h, rope, states, in_w, conv_w, conv_b, dt_bias_f, dt_bias_b, a_log_f, a_log_b,
                        d_skip, ssd_norm_w, ret_logit_f, ret_logit_b, out_w):
    b, n, _ = h.shape
    z, xbc, dt_raw, q, k, v, g = jnp.split(h @ in_w, AB_SPLITS, axis=-1)
    xbc = jax.nn.silu(dwconv(xbc, conv_w, conv_b))
    xs, bm, cm = jnp.split(xbc, [SSD_INNER, SSD_INNER + SSD_GROUPS * SSD_STATE], axis=-1)
    xs = xs.reshape(b, n, SSD_HEADS, SSD_HEAD_DIM)
    rep = SSD_HEADS // SSD_GROUPS
    bm = jnp.repeat(bm.reshape(b, n, SSD_GROUPS, SSD_STATE), rep, axis=2)
    cm = jnp.repeat(cm.reshape(b, n, SSD_GROUPS, SSD_STATE), rep, axis=2)
    dt_raw = dt_raw.astype(jnp.float32)
    dt_f = jax.nn.softplus(dt_raw + dt_bias_f)
    dt_b = jax.nn.softplus(dt_raw + dt_bias_b)
    la_f = -dt_f * jnp.exp(a_log_f.astype(jnp.float32))
    la_b = -dt_b * jnp.exp(a_log_b.astype(jnp.float32))
    y_ssd, sf, sb = bidir_decay_scan(cm, bm, xs * dt_f[..., None], xs * dt_b[..., None],
                                     la_f, la_b, states[0], states[1])
    y_ssd = (y_ssd + d_skip[:, None] * xs).reshape(b, n, SSD_INNER) * jax.nn.silu(z)
    y_ssd = rmsnorm(y_ssd.reshape(b, n, SSD_GROUPS, SSD_INNER // SSD_GROUPS),
                    ssd_norm_w.reshape(SSD_GROUPS, SSD_INNER // SSD_GROUPS)).reshape(b, n, SSD_INNER)
    q = q.reshape(b, n, RET_HEADS, RET_QK_DIM)
    k = k.reshape(b, n, RET_HEADS, RET_QK_DIM) * RET_QK_DIM ** -0.5
    if rope is not None:
        q = apply_rope(q, *rope)
        k = apply_rope(k, *rope)
    v = v.reshape(b, n, RET_HEADS, RET_V_DIM)
    lg_f = jnp.broadcast_to(jax.nn.log_sigmoid(ret_logit_f.astype(jnp.float32)), (b, n, RET_HEADS))
    lg_b = jnp.broadcast_to(jax.nn.log_sigmoid(ret_logit_b.astype(jnp.float32)), (b, n, RET_HEADS))
    y_ret, rf, rb = bidir_decay_scan(q, k, v, v, lg_f, lg_b, states[2], states[3])
    y_ret = head_layernorm(y_ret).reshape(b, n, RET_V) * jax.nn.silu(g)
    out = jnp.concatenate([y_ssd, y_ret], axis=-1).astype(h.dtype) @ out_w
    return out, (sf, sb, rf, rb)


def mlstm_mixer(h, states, up_w, conv_w, conv_b, wq, wk, wv, gate_w, gate_b, norm_w, skip, down_w):
    b, n, _ = h.shape
    xm, z = jnp.split(h @ up_w, 2, axis=-1)
    xc = jax.nn.silu(dwconv(xm, conv_w, conv_b))

    def blockwise(a, w):
        return jnp.einsum('bnkd,kde->bnke', a.reshape(b, n, N_QKV_BLOCKS, QKV_BLOCK), w).reshape(b, n, MLSTM_INNER)

    q, k, v = blockwise(xc, wq), blockwise(xc, wk), blockwise(xm, wv)
    gates = (jnp.concatenate([q, k, v], axis=-1) @ gate_w + gate_b).astype(jnp.float32)
    i_f, f_f, i_b, f_b = jnp.split(gates, 4, axis=-1)

    def heads(a):
        return a.reshape(b, n, MLSTM_HEADS, MLSTM_HEAD_DIM)

    qh, kh, vh = heads(q), heads(k) * MLSTM_HEAD_DIM ** -0.5, heads(v)
    h_f, st_f = mlstm_scan(qh, kh, vh, i_f, jax.nn.log_sigmoid(f_f), states[0])
    h_b, st_b = mlstm_scan(flip(qh), flip(kh), flip(vh), flip(i_b), flip(jax.nn.log_sigmoid(f_b)), states[1])
    hn = head_layernorm(h_f + flip(h_b)).reshape(b, n, MLSTM_INNER) * norm_w
    y = (hn + skip * xc) * jax.nn.silu(z)
    return y.astype(h.dtype) @ down_w, (st_f, st_b)


def block_update(x, y, mod, ffn_norm_w, w1, w3, w2):
    x = x + (mod[2] * y).astype(x.dtype)
    hf = modulate(rmsnorm(x, ffn_norm_w), mod[3], mod[4])
    return x + (mod[5] * swiglu(hf, w1, w3, w2)).astype(x.dtype)


def setup_inputs(seed: int = 0) -> dict:
    key = jax.random.key(seed)
    ks = iter(jax.random.split(key, 64))

    def nrm(shape, scale):
        return scale * jax.random.normal(next(ks), shape, jnp.float32)

    def gain(shape):
        return 1.0 + nrm(shape, 0.02)

    def dt_bias():
        dt = jnp.exp(jax.random.uniform(next(ks), (N_EVEN, SSD_HEADS), jnp.float32,
                                        math.log(1e-3), math.log(1e-1)))
        return dt + jnp.log(-jnp.expm1(-dt))

    def a_log():
        return jnp.log(jax.random.uniform(next(ks), (N_EVEN, SSD_HEADS), jnp.float32, 1.0, 16.0))

    gamma0 = 1.0 - 2.0 ** (-5.0 - jnp.arange(RET_HEADS, dtype=jnp.float32))
    logit0 = jnp.log(gamma0) - jnp.log1p(-gamma0)
    f_bias0 = jnp.linspace(3.0, 6.0, MLSTM_HEADS, dtype=jnp.float32)
    D = D_MODEL
    return {
        'x': nrm((BATCH, SEQ, D), 1.0),
        'c': nrm((BATCH, D), 1.0),
        'ctx': nrm((BATCH, CTX_LEN, D), 1.0),
        'c_ctx': nrm((D,), 1.0),
        'ada_w': nrm((DEPTH, D, 6 * D), D ** -0.5),
        'ada_b': nrm((DEPTH, 6 * D), 0.02),
        'norm_mix_w': gain((DEPTH, D)),
        'norm_ffn_w': gain((DEPTH, D)),
        'ffn_w1': nrm((DEPTH, D, FFN_HIDDEN), D ** -0.5),
        'ffn_w3': nrm((DEPTH, D, FFN_HIDDEN), D ** -0.5),
        'ffn_w2': nrm((DEPTH, FFN_HIDDEN, D), FFN_HIDDEN ** -0.5),
        'ab_in_w': nrm((N_EVEN, D, AB_IN), D ** -0.5),
        'ab_conv_w': nrm((N_EVEN, CONV_W, SSD_CONV_DIM), CONV_W ** -0.5),
        'ab_conv_b': nrm((N_EVEN, SSD_CONV_DIM), 0.02),
        'ssd_dt_bias_f': dt_bias(),
        'ssd_dt_bias_b': dt_bias(),
        'ssd_a_log_f': a_log(),
        'ssd_a_log_b': a_log(),
        'ssd_d': 1.0 + nrm((N_EVEN, SSD_HEADS), 0.1),
        'ssd_norm_w': gain((N_EVEN, SSD_INNER)),
        'ret_logit_f': logit0 + nrm((N_EVEN, RET_HEADS), 0.1),
        'ret_logit_b': logit0 + nrm((N_EVEN, RET_HEADS), 0.1),
        'ab_out_w': nrm((N_EVEN, AB_OUT, D), AB_OUT ** -0.5),
        'ml_up_w': nrm((N_ODD, D, 2 * MLSTM_INNER), D ** -0.5),
        'ml_conv_w': nrm((N_ODD, CONV_W, MLSTM_INNER), CONV_W ** -0.5),
        'ml_conv_b': nrm((N_ODD, MLSTM_INNER), 0.02),
        'ml_wq': nrm((N_ODD, N_QKV_BLOCKS, QKV_BLOCK, QKV_BLOCK), QKV_BLOCK ** -0.5),
        'ml_wk': nrm((N_ODD, N_QKV_BLOCKS, QKV_BLOCK, QKV_BLOCK), QKV_BLOCK ** -0.5),
        'ml_wv': nrm((N_ODD, N_QKV_BLOCKS, QKV_BLOCK, QKV_BLOCK), QKV_BLOCK ** -0.5),
        'ml_gate_w': nrm((N_ODD, 3 * MLSTM_INNER, N_GATES), 0.1 * (3 * MLSTM_INNER) ** -0.5),
        'ml_gate_b': jnp.concatenate([nrm((N_ODD, MLSTM_HEADS), 0.1),
                                      f_bias0 + nrm((N_ODD, MLSTM_HEADS), 0.1),
                                      nrm((N_ODD, MLSTM_HEADS), 0.1),
                                      f_bias0 + nrm((N_ODD, MLSTM_HEADS), 0.1)], axis=-1),
        'ml_norm_w': gain((N_ODD, MLSTM_INNER)),
        'ml_skip': gain((N_ODD, MLSTM_INNER)),
        'ml_down_w': nrm((N_ODD, MLSTM_INNER, D), MLSTM_INNER ** -0.5),
        'final_norm_w': gain((D,)),
    }


def reference(x, c, ctx, c_ctx, ada_w, ada_b, norm_mix_w, norm_ffn_w, ffn_w1, ffn_w3, ffn_w2,
              ab_in_w, ab_conv_w, ab_conv_b, ssd_dt_bias_f, ssd_dt_bias_b, ssd_a_log_f, ssd_a_log_b,
              ssd_d, ssd_norm_w, ret_logit_f, ret_logit_b, ab_out_w,
              ml_up_w, ml_conv_w, ml_conv_b, ml_wq, ml_wk, ml_wv, ml_gate_w, ml_gate_b,
              ml_norm_w, ml_skip, ml_down_w, final_norm_w):
    b = x.shape[0]
    rows = x.shape[1] // GRID_W
    rope = axial_rope(rows)
    lat, cx = x, ctx
    for layer in range(DEPTH):
        mod_l = jnp.split(modulation(c, ada_w[layer], ada_b[layer])[:, None, :], 6, axis=-1)
        mod_c = jnp.split(modulation(c_ctx[None, :], ada_w[layer], ada_b[layer])[:, None, :], 6, axis=-1)
        h_l = modulate(rmsnorm(lat, norm_mix_w[layer]), mod_l[0], mod_l[1])
        h_c = modulate(rmsnorm(cx, norm_mix_w[layer]), mod_c[0], mod_c[1])
        j = layer // 2
        if layer % 2 == 0:
            p = (ab_in_w[j], ab_conv_w[j], ab_conv_b[j], ssd_dt_bias_f[j], ssd_dt_bias_b[j],
                 ssd_a_log_f[j], ssd_a_log_b[j], ssd_d[j], ssd_norm_w[j], ret_logit_f[j],
                 ret_logit_b[j], ab_out_w[j])
            y_c, st = ssd_retention_mixer(h_c, None, ab_zero_states(b), *p)
            y_l, _ = ssd_retention_mixer(h_l, rope, st, *p)
        else:
            p = (ml_up_w[j], ml_conv_w[j], ml_conv_b[j], ml_wq[j], ml_wk[j], ml_wv[j],
                 ml_gate_w[j], ml_gate_b[j], ml_norm_w[j], ml_skip[j], ml_down_w[j])
            y_c, st = mlstm_mixer(h_c, mlstm_zero_states(b), *p)
            y_l, _ = mlstm_mixer(h_l, st, *p)
        lat = block_update(lat, y_l, mod_l, norm_ffn_w[layer], ffn_w1[layer], ffn_w3[layer], ffn_w2[layer])
        if layer < DEPTH - 1:
            cx = block_update(cx, y_c, mod_c, norm_ffn_w[layer], ffn_w1[layer], ffn_w3[layer], ffn_w2[layer])
    return rmsnorm(lat, final_norm_w)
```

```python
import numpy as np
from contextlib import ExitStack
import concourse.bass as bass
import concourse.mybir as mybir
from concourse.bass_utils import run_bass_kernel_spmd

F32 = mybir.dt.float32
AF = mybir.ActivationFunctionType
ALU = mybir.AluOpType
AX = mybir.AxisListType

NCORE = 8
DEBUG = None
GC = 2
NCH = 17
L = 128
RMS_EPS = 1e-6
LN_EPS = 1e-5


class View:
    def __init__(self, b, ap):
        self.b = b
        self.ap = ap

    def __getitem__(self, k):
        return View(self.b, self.ap[k])

    def unsq(self, ax):
        return View(self.b, self.ap.unsqueeze(ax))

    def bc(self, shape):
        return View(self.b, self.ap.to_broadcast(list(shape)))

    def re(self, s, **kw):
        return View(self.b, self.ap.rearrange(s, **kw))


class Buf:
    def __init__(self, name, t, psum=False):
        self.name = name
        self.t = t
        self.psum = psum
        self.last_w = None
        self.reads = {}

    def __getitem__(self, k):
        return View(self, self.t[k])


def _bufs(*vs):
    out = []
    for v in vs:
        if isinstance(v, View) and v.b not in out:
            out.append(v.b)
    return out


def _ap(v):
    return v.ap if isinstance(v, View) else v


class FW:
    ENGS = ("pe", "act", "dve", "pool", "sp")
    EOBJ = {"pe": "tensor", "act": "scalar", "dve": "vector", "pool": "gpsimd", "sp": "sync"}
    NSLOT = 8

    def __init__(self, nc, stack):
        self.nc = nc
        self.sem = {e: stack.enter_context(nc.semaphore("s_" + e)) for e in self.ENGS}
        self.count = {e: 0 for e in self.ENGS}
        self.prog = {e: [] for e in self.ENGS}
        self.waited = {e: {} for e in self.ENGS}
        self.slots = {}
        for q in ("sp", "pool", "act"):
            self.slots[q] = [[stack.enter_context(nc.semaphore("d_%s%d" % (q, i))), 0] for i in range(self.NSLOT)]
        self.slot_i = {q: 0 for q in self.slots}
        self.nbuf = 0
        self.stack = None
        self.dq = 0

    def sb(self, shape, name=None):
        self.nbuf += 1
        name = (name or "sb") + "_%d" % self.nbuf
        return Buf(name, self.stack.enter_context(self.nc.sbuf_tensor(name, list(shape), F32)))

    def ps(self, shape, name=None):
        self.nbuf += 1
        name = (name or "ps") + "_%d" % self.nbuf
        return Buf(name, self.stack.enter_context(self.nc.psum_tensor(name, list(shape), F32)), psum=True)

    def dram(self, name, shape, kind="Internal"):
        return Buf(name, self.nc.dram_tensor(name, list(shape), F32, kind=kind).ap())

    def _waits(self, eng, reads, writes):
        need = {}

        def add(tok):
            if tok is None:
                return
            s, v = tok
            if id(s) not in need or need[id(s)][1] < v:
                need[id(s)] = (s, v)

        for b in reads:
            add(b.last_w)
            if b.psum:
                for k_, tok in b.reads.items():
                    if k_ != eng:
                        add(tok)
        for b in writes:
            add(b.last_w)
            for tok in b.reads.values():
                add(tok)
        out = []
        for k, (s, v) in need.items():
            if eng == "pe" and s is self.sem["pe"]:
                continue
            if self.waited[eng].get(k, 0) >= v:
                continue
            self.waited[eng][k] = v
            out.append((s, v))
        return out

    def op(self, eng, fn, reads, writes):
        waits = self._waits(eng, reads, writes)
        self.count[eng] += 1
        tok = (self.sem[eng], self.count[eng])
        self.prog[eng].append((waits, fn, self.sem[eng], 1))
        for b in reads:
            b.reads[eng] = tok
        for b in writes:
            b.last_w = tok
            b.reads = {}
        return tok

    def dma(self, out, in_, q=None):
        if q is None:
            q = ("sp", "pool")[self.dq % 2]
            self.dq += 1
        reads, writes = [in_.b], [out.b]
        sl = self.slots[q]
        i = self.slot_i[q]
        self.slot_i[q] = (i + 1) % len(sl)
        s, v = sl[i]
        waits = self._waits(q, reads, writes)
        if v > 0 and self.waited[q].get(id(s), 0) < v:
            self.waited[q][id(s)] = v
            waits.append((s, v))
        sl[i][1] = v + 16
        tok = (s, v + 16)
        oa, ia = out.ap, in_.ap
        self.prog[q].append((waits, lambda e: e.dma_start(out=oa, in_=ia), s, 16))
        for b in reads:
            b.reads[("dma", q, i)] = tok
        for b in writes:
            b.last_w = tok
            b.reads = {}
        return tok

    def flush(self, final=False):
        fin = {}
        for q in self.slots:
            for s, v in self.slots[q]:
                if v > 0:
                    fin[id(s)] = (s, v)
        for e in self.ENGS:
            if self.count[e] > 0:
                fin[id(self.sem[e])] = (self.sem[e], self.count[e])
        progs = self.prog
        self.prog = {e: [] for e in self.ENGS}
        with self.nc.Block() as block:
            for e in self.ENGS:
                extra = []
                for k, (s, v) in fin.items():
                    if self.waited[e].get(k, 0) < v:
                        self.waited[e][k] = v
                        extra.append((s, v))

                def body(eng, prog=progs[e], extra=extra):
                    for waits, fn, s, inc in prog:
                        for ws, wv in waits:
                            eng.wait_ge(ws, wv)
                        fn(eng).then_inc(s, inc)
                    for ws, wv in extra:
                        eng.wait_ge(ws, wv)

                getattr(block, self.EOBJ[e])(body)

    def mm(self, out, lhsT, rhs, start=True, stop=True):
        o, l, r = out.ap, lhsT.ap, rhs.ap
        return self.op("pe", lambda e: e.matmul(o, l, r, start=start, stop=stop), _bufs(lhsT, rhs), _bufs(out))

    def tr(self, out, in_, ident):
        o, i, d = out.ap, in_.ap, ident.ap
        return self.op("pe", lambda e: e.transpose(o, i, d), _bufs(in_, ident), _bufs(out))

    def act(self, out, in_, func, bias=None, scale=None):
        kw = {}
        if bias is not None:
            kw["bias"] = _ap(bias)
        if scale is not None:
            kw["scale"] = _ap(scale)
        o, i = out.ap, in_.ap
        return self.op("act", lambda e: e.activation(o, i, func, **kw), _bufs(in_, bias, scale), _bufs(out))

    def tt(self, eng, out, a, b, op):
        o, x, y = out.ap, a.ap, b.ap
        return self.op(eng, lambda e: e.tensor_tensor(o, x, y, op), _bufs(a, b), _bufs(out))

    def ts(self, eng, out, a, s1, op0, s2=None, op1=None):
        o, x, p1, p2 = out.ap, a.ap, _ap(s1), _ap(s2)
        if op1 is None:
            fn = lambda e: e.tensor_scalar(o, x, p1, None, op0)
        else:
            fn = lambda e: e.tensor_scalar(o, x, p1, p2, op0, op1)
        return self.op(eng, fn, _bufs(a, s1, s2), _bufs(out))

    def stt(self, eng, out, a, sc, b, op0, op1):
        o, x, s, y = out.ap, a.ap, _ap(sc), b.ap
        return self.op(eng, lambda e: e.scalar_tensor_tensor(o, x, s, y, op0, op1), _bufs(a, sc, b), _bufs(out))

    def copy(self, eng, out, in_):
        o, i = out.ap, in_.ap
        if eng == "act":
            return self.op("act", lambda e: e.copy(o, i), _bufs(in_), _bufs(out))
        return self.op(eng, lambda e: e.tensor_copy(o, i), _bufs(in_), _bufs(out))

    def memset(self, eng, out, val):
        o = out.ap
        return self.op(eng, lambda e: e.memset(o, val), [], _bufs(out))

    def rsum(self, eng, out, in_):
        o, i = out.ap, in_.ap
        return self.op(eng, lambda e: e.reduce_sum(o, i, AX.X), _bufs(in_), _bufs(out))

    def rmax(self, eng, out, in_):
        o, i = out.ap, in_.ap
        return self.op(eng, lambda e: e.reduce_max(o, i, AX.X), _bufs(in_), _bufs(out))


class Pool:
    def __init__(self, fw, shape, n, psum=False, name=None):
        self.bufs = [(fw.ps if psum else fw.sb)(shape, name) for _ in range(n)]
        self.i = 0

    def get(self):
        b = self.bufs[self.i]
        self.i = (self.i + 1) % len(self.bufs)
        return b


class Consts:
    def __init__(self, fw, cdram):
        self.t = fw.sb([128, 6, 128], "consts")
        fw.dma(self.t[:], cdram[:])
        self.ident = self.t[:, 0, :]
        self.m_le = self.t[:, 1, :]
        self.m_ge = self.t[:, 2, :]
        self.m_gt = self.t[:, 3, :]
        self.m_lt = self.t[:, 4, :]
        self.ones = self.t[:, 5, :]


def host_consts():
    j = np.arange(128)[:, None]
    i = np.arange(128)[None, :]
    c = np.stack([(j == i), (j <= i), (j >= i), (j > i), (j < i), np.ones((128, 128), bool)], 0).astype(np.float32)
    return np.ascontiguousarray(c.transpose(1, 0, 2))


def emit_norm(fw, K, xin, hout, c0, c1, gs, sh, col, nk=8, dim=1024.0):
    n = c1 - c0
    ss = K["ps"].get()
    for kt in range(nk):
        sq = K["t512"].get()
        fw.act(sq[:, :n], xin[:, kt, c0:c1], AF.Square)
        fw.mm(ss[:, :n], K["c"].ones, sq[:, :n], kt == 0, kt == nk - 1)
    if "nrm" not in K:
        K["nrm"] = Pool(fw, [128, 512], 2, name="nrm")
    t = K["nrm"].get()
    fw.ts("dve", t[:, :n], ss[:, :n], 1.0 / dim, ALU.mult, RMS_EPS, ALU.add)
    fw.act(t[:, :n], t[:, :n], AF.Ln)
    r = K["nrm"].get()
    fw.act(r[:, :n], t[:, :n], AF.Exp, scale=-0.5)
    for kt in range(nk):
        t2 = K["t512"].get()
        fw.stt("dve", t2[:, :n], xin[:, kt, c0:c1], gs[:, kt, col:col + 1], r[:, :n], ALU.mult, ALU.mult)
        if sh is None:
            fw.copy("act", hout[:, kt, c0:c1], t2[:, :n])
        else:
            fw.act(hout[:, kt, c0:c1], t2[:, :n], AF.Identity, bias=sh[:, kt, col:col + 1])


def emit_mods(fw, K, cvec_d, adaw_d, adab_d, nw_d, nwf_d, mod_out_d):
    cv = fw.sb([128, 8, 2], "cvec")
    fw.dma(cv[:], cvec_d[:])
    sc = fw.sb([128, 8, 2], "silu_c")
    fw.act(sc[:], cv[:], AF.Silu)
    ab = fw.sb([128, 48], "adab")
    fw.dma(ab[:], adab_d[:])
    mod = fw.sb([128, 48, 2], "mod")
    wp = Pool(fw, [128, 8, 128], 3, name="adaw")
    for ft in range(48):
        w = wp.get()
        fw.dma(w[:], adaw_d[ft])
        p = K["ps"].get()
        for kt in range(8):
            fw.mm(p[:, 0:2], w[:, kt, :], sc[:, kt, :], kt == 0, kt == 7)
        fw.ts("dve", mod[:, ft, :], p[:, 0:2], ab[:, ft:ft + 1], ALU.add)
    if mod_out_d is not None:
        fw.dma(mod_out_d[:], mod[:])
    return mod


def emit_gs(fw, mod, nw_d, scale_ft0, name):
    nw = fw.sb([128, 8], name + "_nw")
    fw.dma(nw[:], nw_d[:])
    gs = fw.sb([128, 8, 2], name)
    fw.ts("dve", gs[:], mod[:, scale_ft0:scale_ft0 + 8, :], 1.0, ALU.add)
    fw.tt("dve", gs[:], gs[:], nw[:].unsq(2).bc([128, 8, 2]), ALU.mult)
    return gs


SEGS = ((0, 16, 2052), (16, 1, 132))


def l0p1(fw, D):
    st = fw.stack
    K = {"ps": Pool(fw, [128, 512], 8, psum=True, name="ps"), "t512": Pool(fw, [128, 512], 6, name="t512")}
    K["c"] = C = Consts(fw, D["consts"])
    mod = emit_mods(fw, K, D["cvec"], D["adaw"], D["adab"], None, None, D["mod_out"])
    gs = emit_gs(fw, mod, D["nw"], 8, "gs")
    sh = mod[:, 0:8, :]
    hm = fw.sb([128, 4], "hm"); fw.dma(hm[:], D["hm"][:])
    cw = fw.sb([128, 16, 5], "cw"); fw.dma(cw[:], D["cw"][:])
    cb = fw.sb([128, 16], "cb"); fw.dma(cb[:], D["cb"][:])
    wdt = fw.sb([128, 8, 16], "wdt"); fw.dma(wdt[:], D["w_dt"][:])
    sm = fw.sb([128, 72], "small"); fw.dma(sm[:], D["small"][:])
    iota = fw.sb([128, 2], "iota"); fw.dma(iota[:], D["iota"][:])
    negA = fw.sb([128, 32], "negA")
    fw.act(negA[:], sm[:, 32:64], AF.Exp)
    fw.ts("dve", negA[:], negA[:], -1.0, ALU.mult)
    lg = fw.sb([128, 8], "lg")
    fw.act(lg[:], sm[:, 64:72], AF.Exp, scale=-1.0)
    fw.act(lg[:], lg[:], AF.Ln, bias=1.0)
    fw.ts("dve", lg[:], lg[:], -1.0, ALU.mult)
    rsc = fw.sb([128, 16], "rsc")
    ip1 = fw.sb([128, 4], "ip1")
    fw.ts("dve", ip1[:, 0:1], iota[:, 0:1], 1.0, ALU.add)
    fw.ts("dve", ip1[:, 1:2], iota[:, 1:2], 1.0, ALU.add)
    fw.copy("dve", ip1[:, 2:3], iota[:, 1:2])
    fw.copy("dve", ip1[:, 3:4], iota[:, 0:1])
    for n_, (lgc, ic) in enumerate(((0, 0), (4, 1), (0, 2), (4, 3))):
        fw.ts("dve", rsc[:, 4 * n_:4 * n_ + 4], lg[:, lgc:lgc + 4], ip1[:, ic:ic + 1], ALU.mult)
    fw.act(rsc[:], rsc[:], AF.Exp)
    Aret = fw.sb([128, 8], "Aret")
    fw.act(Aret[:], lg[:], AF.Exp, scale=128.0)

    xg = fw.sb([128, 8, 128 * GC + 4], "xg")
    hT = fw.sb([128, 8, 128 * GC + 4], "hT")
    xbc = fw.sb([128, 16, 128 * GC], "xbcT")
    wxp = Pool(fw, [128, 8, 128], 3, name="wx")
    wtp = Pool(fw, [128, 8, 256], 2, name="wt")
    cacc = Pool(fw, [128, 128], 3, name="cacc")
    tokp = Pool(fw, [128, 1024], 5, name="tok")
    tok5 = Pool(fw, [128, 512], 10, name="tok5")
    fmp = Pool(fw, [128, 16, 128], 2, name="fm")
    kstp = Pool(fw, [128, 1024], 3, name="kst")
    qkst = Pool(fw, [128, 512], 2 * GC, name="qkst")
    smallp = Pool(fw, [128, 64], 8, name="sm")
    Stot = fw.sb([128, 4, 1024], "Stot")
    Pb = fw.sb([128, 20], "Pb")
    Af = fw.sb([128, 20], "Af")
    zst = fw.sb([128, GC, 1024], "zstage")

    for si, (ch0, nch, ncol) in enumerate(SEGS):
        xT_d = D["xT%d" % si]
        cs_d = D["cs%d" % si]
        fw.memset("pool", Stot[:], 0.0)
        fw.memset("pool", Pb[:], 1.0)
        fw.memset("pool", Af[:], 1.0)
        ngrp = (nch + GC - 1) // GC
        for g in range(ngrp):
            gch = min(GC, nch - GC * g)
            W = gch * 128
            c0 = 128 * GC * g
            fw.dma(xg[:, :, 0:W + 4], xT_d[:, :, c0:c0 + W + 4])
            for a in range(0, W + 4, 512):
                b_ = min(a + 512, W + 4)
                emit_norm(fw, K, xg, hT, a, b_, gs, sh, si)
            if g == 0:
                fw.ts("dve", hT[:, :, 0:2], hT[:, :, 0:2], hm[:, 2 * si:2 * si + 1], ALU.mult)
            if g == ngrp - 1:
                fw.ts("dve", hT[:, :, W + 2:W + 4], hT[:, :, W + 2:W + 4], hm[:, 2 * si + 1:2 * si + 2], ALU.mult)
            for ft in range(16):
                w = wxp.get()
                fw.dma(w[:], D["w_xbc"][ft])
                for j in range(gch):
                    p = K["ps"].get()
                    for kt in range(8):
                        fw.mm(p[:, 0:132], w[:, kt, :], hT[:, kt, 128 * j:128 * j + 132], kt == 0, kt == 7)
                    acc = cacc.get()
                    fw.ts("dve", acc[:], p[:, 0:128], cw[:, ft, 0:1], ALU.mult)
                    for k in range(1, 5):
                        fw.stt("dve", acc[:], p[:, k:k + 128], cw[:, ft, k:k + 1], acc[:], ALU.mult, ALU.add)
                    fw.act(xbc[:, ft, 128 * j:128 * j + 128], acc[:], AF.Silu, bias=cb[:, ft:ft + 1])
            kstc = {}
            qkT = {}
            for name, b0, nb in (("z", 0, 4), ("q", 4, 2), ("k", 6, 2), ("v", 8, 4), ("g", 12, 4)):
                stg = {}
                for bi in range(nb):
                    w = wtp.get()
                    fw.dma(w[:], D["w_tm"][b0 + bi])
                    for j in range(gch):
                        ch = ch0 + GC * g + j
                        if bi == 0:
                            stg[j] = zst[:, j, :] if name == "v" else (tokp.get()[:] if name in "zg" else qkst.get()[:])
                        p = K["ps"].get()
                        for kt in range(8):
                            fw.mm(p[:, 0:256], hT[:, kt, 2 + 128 * j:2 + 128 * j + 128], w[:, kt, :], kt == 0, kt == 7)
                        o = stg[j][:, 256 * bi:256 * bi + 256]
                        if name in "zg":
                            fw.act(o, p[:, 0:256], AF.Silu)
                        elif name == "k":
                            fw.act(o, p[:, 0:256], AF.Copy, scale=float(128.0 ** -0.5))
                        else:
                            fw.copy("act", o, p[:, 0:256])
                        if bi == nb - 1:
                            if name == "z":
                                fw.dma(D["o_z"][ch], stg[j])
                            elif name == "g":
                                fw.dma(D["o_g"][ch], stg[j])
                            elif name == "v":
                                fw.dma(D["o_v"][ch], stg[j])
                            elif name == "q":
                                qkT[j] = fmp.get()
                                emit_rope_q(fw, K, C, stg[j], cs_d, GC * g + j, rsc, qkT[j], tok5)
                            else:
                                kstc[j] = emit_rope_k(fw, K, C, stg[j], cs_d, GC * g + j, rsc, qkT[j], tok5, kstp)
                                fw.dma(D["o_qk"][ch], qkT[j][:])
                                fw.dma(D["o_kst"][ch], kstc[j][:])
            for j in range(gch):
                ch = ch0 + GC * g + j
                p = K["ps"].get()
                for kt in range(8):
                    fw.mm(p[:, 0:16], hT[:, kt, 2 + 128 * j:2 + 128 * j + 128], wdt[:, kt, :], kt == 0, kt == 7)
                dl = smallp.get()
                fw.tt("dve", dl[:, 0:16], p[:, 0:16], sm[:, 0:16], ALU.add)
                fw.tt("dve", dl[:, 16:32], p[:, 0:16], sm[:, 16:32], ALU.add)
                fw.act(dl[:, 0:32], dl[:, 0:32], AF.Exp)
                fw.act(dl[:, 0:32], dl[:, 0:32], AF.Ln, bias=1.0)
                fw.tt("dve", dl[:, 32:64], dl[:, 0:32], negA[:], ALU.mult)
                fw.dma(D["o_dl"][ch], dl[:])
                fw.dma(D["o_cb"][ch], xbc[:, 8:16, 128 * j:128 * j + 128])
                xs = tokp.get()
                for ft in range(8):
                    pt = K["ps"].get()
                    fw.tr(pt[:, 0:128], xbc[:, ft, 128 * j:128 * j + 128], C.ident)
                    fw.copy("act" if ft % 2 else "dve", xs[:, 128 * ft:128 * ft + 128], pt[:, 0:128])
                fw.dma(D["o_xs"][ch], xs[:])
                bm = tok5.get()
                for ft in range(4):
                    pt = K["ps"].get()
                    fw.tr(pt[:, 0:128], xbc[:, 8 + ft, 128 * j:128 * j + 128], C.ident)
                    fw.copy("act" if ft % 2 else "dve", bm[:, 128 * ft:128 * ft + 128], pt[:, 0:128])
                fw.dma(D["o_bm"][ch], bm[:])
                emit_ssd_state(fw, K, C, dl, xs, bm, Stot[:, 0, :], Stot[:, 1, :], Af, Pb, smallp, tokp, "p1")
                emit_ret_state(fw, K, kstc[j], zst[:, j, :], Stot[:, 2, :], Stot[:, 3, :], Af, Pb, Aret, tokp, "p1")
        for k in range(4):
            fw.dma(D["o_tot"][si, k], Stot[:, k, :])
        ta = smallp.get()
        fw.copy("dve", ta[:, 0:20], Af[:])
        fw.copy("dve", ta[:, 20:40], Pb[:])
        fw.dma(D["o_totA"][si], ta[:, 0:40])


def emit_rope(fw, src, cs, out, tok5, scale):
    s3 = src.re("p (h d) -> p h d", h=4)
    o3 = out[:].re("p (h d) -> p h d", h=4)
    cos = cs[:, 0:64].unsq(1).bc([128, 4, 64])
    sin = cs[:, 64:128].unsq(1).bc([128, 4, 64])
    t1 = tok5.get(); t2 = tok5.get()
    a = t1[:, 0:256].re("p (h d) -> p h d", h=4)
    b = t1[:, 256:512].re("p (h d) -> p h d", h=4)
    c = t2[:, 0:256].re("p (h d) -> p h d", h=4)
    d = t2[:, 256:512].re("p (h d) -> p h d", h=4)
    fw.tt("dve", a, s3[:, :, 0:64], cos, ALU.mult)
    fw.tt("dve", b, s3[:, :, 64:128], sin, ALU.mult)
    fw.tt("dve", c, s3[:, :, 0:64], sin, ALU.mult)
    fw.tt("dve", d, s3[:, :, 64:128], cos, ALU.mult)
    fw.tt("pool", o3[:, :, 0:64], a, b, ALU.subtract)
    fw.tt("pool", o3[:, :, 64:128], c, d, ALU.add)


def emit_rope_q(fw, K, C, qs, cs_d, lc, rsc, qk, tok5):
    cs = tok5.get()
    fw.dma(cs[:, 0:128], cs_d[lc])
    qr = tok5.get()
    emit_rope(fw, qs, cs[:, 0:128], qr, tok5, 1.0)
    variants = [qr]
    for v in range(2):
        o = tok5.get()
        fw.tt("dve" if v else "pool", o[:].re("p (h d) -> p h d", h=4), qr[:].re("p (h d) -> p h d", h=4),
              rsc[:, 4 * v:4 * v + 4].unsq(2).bc([128, 4, 128]), ALU.mult)
        variants.append(o)
    for vi, src in enumerate(variants):
        for h in range(4):
            pt = K["ps"].get()
            fw.tr(pt[:, 0:128], src[:, 128 * h:128 * h + 128], C.ident)
            fw.copy("act" if h % 2 else "dve", qk[:, 4 * vi + h, :], pt[:, 0:128])


def emit_rope_k(fw, K, C, ks, cs_d, lc, rsc, qk, tok5, kstp):
    cs = tok5.get()
    fw.dma(cs[:, 0:128], cs_d[lc])
    kr = tok5.get()
    emit_rope(fw, ks, cs[:, 0:128], kr, tok5, 1.0)
    for h in range(4):
        pt = K["ps"].get()
        fw.tr(pt[:, 0:128], kr[:, 128 * h:128 * h + 128], C.ident)
        fw.copy("act" if h % 2 else "dve", qk[:, 12 + h, :], pt[:, 0:128])
    kst = kstp.get()
    for v in range(2):
        fw.tt("dve" if v else "pool", kst[:, 512 * v:512 * v + 512].re("p (h d) -> p h d", h=4),
              kr[:].re("p (h d) -> p h d", h=4), rsc[:, 8 + 4 * v:12 + 4 * v].unsq(2).bc([128, 4, 128]), ALU.mult)
    return kst


def emit_ssd_state(fw, K, C, dl, xs, bm, Sf, Sb, Af, Pb, smallp, tokp, mode):
    dirs = {"p1": (0, 1), "f": (0,), "b": (1,)}[mode]
    for d in dirs:
        la = dl[:, 32 + 16 * d:48 + 16 * d]
        pr = K["ps"].get()
        fw.mm(pr[:, 0:16], C.m_lt if d else C.m_gt, la, True, True)
        fw.mm(pr[:, 16:32], C.ones, la, True, True)
        wv = smallp.get()
        fw.act(wv[:, 0:32], pr[:, 0:32], AF.Exp)
        fw.tt("dve", wv[:, 0:16], wv[:, 0:16], dl[:, 16 * d:16 * d + 16], ALU.mult)
        vs = tokp.get()
        fw.tt("pool", vs[:].re("p (h e) -> p h e", h=16), xs[:].re("p (h e) -> p h e", h=16),
              wv[:, 0:16].unsq(2).bc([128, 16, 64]), ALU.mult)
        S = Sb if d else Sf
        for half in range(2):
            pf = K["ps"].get()
            for gg in range(2):
                g4 = 2 * half + gg
                fw.mm(pf[:, 256 * gg:256 * gg + 256], bm[:, 128 * g4:128 * g4 + 128], vs[:, 256 * g4:256 * g4 + 256], True, True)
            Sh = S[:, 512 * half:512 * half + 512].re("p (h e) -> p h e", h=8)
            Abc = wv[:, 16 + 8 * half:24 + 8 * half].unsq(2).bc([128, 8, 64])
            pf3 = pf[:].re("p (h e) -> p h e", h=8)
            if mode == "p1" and d == 1:
                t = K["t512"].get()
                fw.tt("dve", t[:].re("p (h e) -> p h e", h=8), pf3, Pb[:, 8 * half:8 * half + 8].unsq(2).bc([128, 8, 64]), ALU.mult)
                fw.tt("pool", S[:, 512 * half:512 * half + 512], S[:, 512 * half:512 * half + 512], t[:], ALU.add)
            else:
                fw.tt("dve", Sh, Sh, Abc, ALU.mult)
                fw.tt("dve", Sh, Sh, pf3, ALU.add)
        if mode == "p1":
            if d == 0:
                fw.tt("dve", Af[:, 0:16], Af[:, 0:16], wv[:, 16:32], ALU.mult)
            else:
                fw.tt("dve", Pb[:, 0:16], Pb[:, 0:16], wv[:, 16:32], ALU.mult)


def emit_ret_state(fw, K, kst, v, Sf, Sb, Af, Pb, Aret, tokp, mode):
    dirs = {"p1": (0, 1), "f": (0,), "b": (1,)}[mode]
    for d in dirs:
        S = Sb if d else Sf
        for half in range(2):
            pf = K["ps"].get()
            for hh in range(2):
                h = 2 * half + hh
                fw.mm(pf[:, 256 * hh:256 * hh + 256], kst[:, 512 * d + 128 * h:512 * d + 128 * h + 128],
                      v[:, 256 * h:256 * h + 256], True, True)
            Sh = S[:, 512 * half:512 * half + 512].re("p (h e) -> p h e", h=2)
            pf3 = pf[:].re("p (h e) -> p h e", h=2)
            if mode == "p1" and d == 1:
                t = K["t512"].get()
                fw.tt("dve", t[:].re("p (h e) -> p h e", h=2), pf3, Pb[:, 16 + 2 * half:18 + 2 * half].unsq(2).bc([128, 2, 256]), ALU.mult)
                fw.tt("pool", S[:, 512 * half:512 * half + 512], S[:, 512 * half:512 * half + 512], t[:], ALU.add)
            else:
                fw.tt("dve", Sh, Sh, Aret[:, 4 * d + 2 * half:4 * d + 2 * half + 2].unsq(2).bc([128, 2, 256]), ALU.mult)
                fw.tt("dve", Sh, Sh, pf3, ALU.add)
        if mode == "p1":
            if d == 0:
                fw.tt("dve", Af[:, 16:20], Af[:, 16:20], Aret[:, 0:4], ALU.mult)
            else:
                fw.tt("dve", Pb[:, 16:20], Pb[:, 16:20], Aret[:, 4:8], ALU.mult)


def emit_rstd(fw, out, in_, scale, eps):
    fw.ts("dve", out, in_, scale, ALU.mult, eps, ALU.add)
    fw.act(out, out, AF.Ln)
    fw.act(out, out, AF.Exp, scale=-0.5)


def l0p3_sweeps(fw, D):
    K = {"ps": Pool(fw, [128, 512], 4, psum=True, name="ps"), "t512": Pool(fw, [128, 512], 4, name="t512")}
    K["c"] = C = Consts(fw, D["consts"])
    py = [fw.ps([128, 512], "py") for _ in range(2)]
    pr = [fw.ps([128, 512], "pr") for _ in range(2)]
    sm = fw.sb([128, 72], "small"); fw.dma(sm[:], D["small"][:])
    dsk = fw.sb([128, 16], "dskip"); fw.dma(dsk[:], D["dskip"][:])
    nws = fw.sb([128, 1024], "nws"); fw.dma(nws[:], D["nws"][:])
    diff = fw.sb([128, 2, 128], "diff"); fw.dma(diff[:], D["diff"][:])
    lg = fw.sb([128, 8], "lg")
    fw.act(lg[:], sm[:, 64:72], AF.Exp, scale=-1.0)
    fw.act(lg[:], lg[:], AF.Ln, bias=1.0)
    fw.ts("dve", lg[:], lg[:], -1.0, ALU.mult)
    Aret = fw.sb([128, 8], "Aret")
    fw.act(Aret[:], lg[:], AF.Exp, scale=128.0)
    DT = fw.sb([128, 8, 128], "DT")
    for d in range(2):
        for h in range(4):
            fw.ts("dve", DT[:, 4 * d + h, :], diff[:, d, :], lg[:, 4 * d + h:4 * d + h + 1], ALU.mult)
            fw.act(DT[:, 4 * d + h, :], DT[:, 4 * d + h, :], AF.Exp)
            fw.tt("dve", DT[:, 4 * d + h, :], DT[:, 4 * d + h, :], C.m_ge if d else C.m_le, ALU.mult)
    preA = fw.sb([128, 2, 5, 40], "preA"); fw.dma(preA[:], D["pre_A"][:])
    S = fw.sb([128, 4, 1024], "S")
    tokp = Pool(fw, [128, 1024], 7, name="tok")
    t128 = Pool(fw, [128, 128], 8, name="t128")
    smallp = Pool(fw, [128, 64], 8, name="sm")
    ld = {n: Pool(fw, sh, 2, name="ld_" + n) for n, sh in
          (("cb", [128, 8, 128]), ("dl", [128, 64]), ("xs", [128, 1024]), ("bm", [128, 512]),
           ("qk", [128, 16, 128]), ("v", [128, 1024]), ("kst", [128, 1024]))}
    yfp = Pool(fw, [128, 2048], 1, name="yf")
    SCb = fw.sb([128, 4, 128], "SC")
    ycp = Pool(fw, [128, 16, 128], 2, name="ycT")
    h16 = lambda v: v.re("p (h e) -> p h e", h=16)
    h4 = lambda v: v.re("p (h e) -> p h e", h=4)

    for si, (ch0, nch, ncol) in enumerate(SEGS):
        fw.memset("pool", S[:], 0.0)
        for e in range(5):
            for k in range(4):
                Fb = tokp.get()
                fw.dma(Fb[:], D["pre_F"][si, e, k])
                if k < 2:
                    A = preA[:, si, e, 20 * k:20 * k + 16].unsq(2).bc([128, 16, 64])
                    Sv = h16(S[:, k, :])
                else:
                    A = preA[:, si, e, 20 * (k - 2) + 16:20 * (k - 2) + 20].unsq(2).bc([128, 4, 256])
                    Sv = h4(S[:, k, :])
                fw.tt("dve", Sv, Sv, A, ALU.mult)
                fw.tt("pool", S[:, k, :], S[:, k, :], Fb[:], ALU.add)
        for d in range(2):
            order = range(nch) if d == 0 else range(nch - 1, -1, -1)
            for lc in order:
                ch = ch0 + lc
                t = {}
                for n, src in (("cb", "o_cb"), ("dl", "o_dl"), ("xs", "o_xs"), ("bm", "o_bm"),
                               ("qk", "o_qk"), ("v", "o_v"), ("kst", "o_kst")):
                    t[n] = ld[n].get()
                    fw.dma(t[n][:], D[src][ch])
                cbm, dl, xs, bm, qk, v, kst = (t[n] for n in ("cb", "dl", "xs", "bm", "qk", "v", "kst"))
                for g4 in range(4):
                    p = K["ps"].get()
                    fw.mm(p[:, 0:128], cbm[:, g4, :], cbm[:, 4 + g4, :])
                    fw.tt("dve", SCb[:, g4, :], p[:, 0:128], C.m_ge if d else C.m_le, ALU.mult)
                vdt = tokp.get()
                fw.tt("pool", h16(vdt[:]), h16(xs[:]), dl[:, 16 * d:16 * d + 16].unsq(2).bc([128, 16, 64]), ALU.mult)
                la = dl[:, 32 + 16 * d:48 + 16 * d]
                for h in range(16):
                    Lm = t128.get()
                    fw.ts("pool" if h % 2 else "dve", Lm[:], C.m_lt if d else C.m_gt, la[:, h:h + 1], ALU.mult)
                    pd = K["ps"].get()
                    fw.mm(pd[:, 0:128], Lm[:], C.m_ge if d else C.m_le)
                    E = t128.get()
                    fw.act(E[:], pd[:, 0:128], AF.Exp)
                    P = t128.get()
                    fw.tt("dve" if h % 2 else "pool", P[:], E[:], SCb[:, h // 4, :], ALU.mult)
                    fw.mm(py[h // 8][:, 64 * (h % 8):64 * (h % 8) + 64], P[:], vdt[:, 64 * h:64 * h + 64])
                pu = [K["ps"].get(), K["ps"].get()]
                for g4 in range(4):
                    fw.mm(pu[g4 // 2][:, 256 * (g4 % 2):256 * (g4 % 2) + 256], cbm[:, 4 + g4, :], S[:, d, 256 * g4:256 * g4 + 256])
                pc = K["ps"].get()
                fw.mm(pc[:, 0:16], C.m_ge if d else C.m_le, la)
                ecs = smallp.get()
                fw.act(ecs[:, 0:16], pc[:, 0:16], AF.Exp)
                y = tokp.get()
                for half in range(2):
                    tq = K["t512"].get()
                    fw.tt("dve", tq[:].re("p (h e) -> p h e", h=8), pu[half][:].re("p (h e) -> p h e", h=8),
                          ecs[:, 8 * half:8 * half + 8].unsq(2).bc([128, 8, 64]), ALU.mult)
                    fw.tt("dve", y[:, 512 * half:512 * half + 512], tq[:], py[half][:], ALU.add)
                emit_ssd_state(fw, K, C, dl, xs, bm, S[:, 0, :], S[:, 1, :], None, None, smallp, tokp, "b" if d else "f")
                for h in range(4):
                    p = K["ps"].get()
                    fw.mm(p[:, 0:128], qk[:, 12 + h, :], qk[:, h, :])
                    P = t128.get()
                    fw.tt("dve", P[:], p[:, 0:128], DT[:, 4 * d + h, :], ALU.mult)
                    o = pr[h // 2][:, 256 * (h % 2):256 * (h % 2) + 256]
                    fw.mm(o, P[:], v[:, 256 * h:256 * h + 256], True, False)
                    fw.mm(o, qk[:, 4 * (1 + d) + h, :], S[:, 2 + d, 256 * h:256 * h + 256], False, True)
                yr = tokp.get()
                fw.copy("act", yr[:, 0:512], pr[0][:])
                fw.copy("act", yr[:, 512:1024], pr[1][:])
                emit_ret_state(fw, K, kst, v[:], S[:, 2, :], S[:, 3, :], None, None, Aret, tokp, "b" if d else "f")
                if d == 0:
                    fw.dma(D["yf"][ch, :, 0:1024], y[:])
                    fw.dma(D["yf"][ch, :, 1024:2048], yr[:])
                    continue
                yf = yfp.get()
                fw.dma(yf[:], D["yf"][ch])
                sz = tokp.get(); fw.dma(sz[:], D["o_z"][ch])
                sg = tokp.get(); fw.dma(sg[:], D["o_g"][ch])
                fw.tt("dve", y[:], y[:], yf[:, 0:1024], ALU.add)
                tq = tokp.get()
                fw.tt("pool", h16(tq[:]), h16(xs[:]), dsk[:].unsq(2).bc([128, 16, 64]), ALU.mult)
                fw.tt("pool", y[:], y[:], tq[:], ALU.add)
                fw.tt("dve", y[:], y[:], sz[:], ALU.mult)
                fw.act(tq[:], y[:], AF.Square)
                st_ = smallp.get()
                fw.rsum("dve", st_[:, 0:4], h4(tq[:]))
                emit_rstd(fw, st_[:, 0:4], st_[:, 0:4], 1.0 / 256, RMS_EPS)
                fw.tt("dve", h4(y[:]), h4(y[:]), st_[:, 0:4].unsq(2).bc([128, 4, 256]), ALU.mult)
                fw.tt("pool", y[:], y[:], nws[:], ALU.mult)
                fw.tt("dve", yr[:], yr[:], yf[:, 1024:2048], ALU.add)
                fw.rsum("dve", st_[:, 8:12], h4(yr[:]))
                fw.ts("dve", st_[:, 8:12], st_[:, 8:12], -1.0 / 256, ALU.mult)
                fw.tt("dve", h4(yr[:]), h4(yr[:]), st_[:, 8:12].unsq(2).bc([128, 4, 256]), ALU.add)
                fw.act(tq[:], yr[:], AF.Square)
                fw.rsum("dve", st_[:, 12:16], h4(tq[:]))
                emit_rstd(fw, st_[:, 12:16], st_[:, 12:16], 1.0 / 256, LN_EPS)
                fw.tt("dve", h4(yr[:]), h4(yr[:]), st_[:, 12:16].unsq(2).bc([128, 4, 256]), ALU.mult)
                fw.tt("pool", yr[:], yr[:], sg[:], ALU.mult)
                ycT = ycp.get()
                for kt in range(16):
                    src = y if kt < 8 else yr
                    pt = K["ps"].get()
                    fw.tr(pt[:, 0:128], src[:, 128 * (kt % 8):128 * (kt % 8) + 128], C.ident)
                    fw.copy("act" if kt % 2 else "dve", ycT[:, kt, :], pt[:, 0:128])
                fw.dma(D["ycT"][:, :, 128 * ch:128 * ch + 128], ycT[:])


def emit_tail(fw, D, nk_in, last):
    K = {"ps": Pool(fw, [128, 512], 8, psum=True, name="ps"), "t512": Pool(fw, [128, 512], 6, name="t512")}
    K["c"] = Consts(fw, D["consts"])
    mod = fw.sb([128, 48, 2], "mod"); fw.dma(mod[:], D["mod_out"][:])
    gsf = emit_gs(fw, mod, D["nwf"], 32, "gsf")
    shf = mod[:, 24:32, :]
    if last:
        gfin = fw.sb([128, 8, 1], "gfin"); fw.dma(gfin[:, :, 0], D["nwfin"][:])
    ycat = fw.sb([128, nk_in, 512], "ycat")
    x1 = fw.sb([128, 8, 512], "x1")
    h2 = fw.sb([128, 8, 512], "h2")
    uT = fw.sb([128, 22, 512], "uT")
    wop = Pool(fw, [128, nk_in, 128], 1, name="wo")
    w13 = Pool(fw, [128, 8, 128], 4, name="w13")
    w2p = Pool(fw, [128, 22, 128], 1, name="w2")
    for si, (ch0, nch, ncol) in enumerate(SEGS):
        if last and si == 1:
            continue
        for c0 in range(0, nch * 128, 512):
            W = min(512, nch * 128 - c0)
            fw.dma(ycat[:, :, 0:W], D["ycT"][:, :, 128 * ch0 + c0:128 * ch0 + c0 + W])
            fw.dma(x1[:, :, 0:W], D["xT%d" % si][:, :, 2 + c0:2 + c0 + W])
            for ft in range(8):
                w = wop.get()
                fw.dma(w[:], D["w_out"][ft])
                p = K["ps"].get()
                for kt in range(nk_in):
                    fw.mm(p[:, 0:W], w[:, kt, :], ycat[:, kt, 0:W], kt == 0, kt == nk_in - 1)
                fw.stt("dve", x1[:, ft, 0:W], p[:, 0:W], mod[:, 16 + ft, si:si + 1], x1[:, ft, 0:W], ALU.mult, ALU.add)
            emit_norm(fw, K, x1, h2, 0, W, gsf, shf, si)
            for ft in range(22):
                wa = w13.get(); fw.dma(wa[:], D["w1"][ft])
                wb = w13.get(); fw.dma(wb[:], D["w3"][ft])
                pa = K["ps"].get(); pb = K["ps"].get()
                for kt in range(8):
                    fw.mm(pa[:, 0:W], wa[:, kt, :], h2[:, kt, 0:W], kt == 0, kt == 7)
                for kt in range(8):
                    fw.mm(pb[:, 0:W], wb[:, kt, :], h2[:, kt, 0:W], kt == 0, kt == 7)
                sa = K["t512"].get()
                fw.act(sa[:, 0:W], pa[:, 0:W], AF.Silu)
                fw.tt("dve", uT[:, ft, 0:W], sa[:, 0:W], pb[:, 0:W], ALU.mult)
            for f2 in range(8):
                w = w2p.get()
                fw.dma(w[:], D["w2"][f2])
                p = K["ps"].get()
                for ft in range(22):
                    fw.mm(p[:, 0:W], w[:, ft, :], uT[:, ft, 0:W], ft == 0, ft == 21)
                fw.stt("dve", h2[:, f2, 0:W], p[:, 0:W], mod[:, 40 + f2, si:si + 1], x1[:, f2, 0:W], ALU.mult, ALU.add)
            if last:
                emit_norm(fw, K, h2, x1, 0, W, gfin, None, 0)
                fw.dma(D["xo%d" % si][:, :, c0:c0 + W], x1[:, :, 0:W])
            else:
                fw.dma(D["xo%d" % si][:, :, c0:c0 + W], h2[:, :, 0:W])


def fm(x2d):
    T, F = x2d.shape
    return np.ascontiguousarray(x2d.T.reshape(F // 128, 128, T).transpose(1, 0, 2))


def wblk(W, bw):
    K_, N = W.shape
    return np.ascontiguousarray(W.reshape(K_ // 128, 128, N // bw, bw).transpose(2, 1, 0, 3))


def colvec(v):
    return np.ascontiguousarray(v.reshape(-1, 128).T)


def rep(v):
    v = np.asarray(v, np.float32).reshape(-1)
    return np.ascontiguousarray(np.broadcast_to(v[None, :], (128, v.size)))


def run_prog(phases, ins, outs, scratch, in_maps):
    nc = bass.Bass("TRN2", target_bir_lowering=False)
    with ExitStack() as top:
        fw = FW(nc, top)
        D = {}
        for name, shape in ins.items():
            D[name] = fw.dram(name, shape, kind="ExternalInput")
        for name, shape in outs.items():
            D[name] = fw.dram(name, shape, kind="ExternalOutput")
        for name, shape in scratch.items():
            D[name] = fw.dram(name, shape, kind="Internal")
        for ph in phases:
            with ExitStack() as st:
                fw.stack = st
                ph(fw, D)
                fw.flush()
    maps = [{k: np.ascontiguousarray(m[k], dtype=np.float32) for k in ins} for m in in_maps]
    for m in maps:
        for k, shape in ins.items():
            assert tuple(m[k].shape) == tuple(shape), (k, m[k].shape, shape)
    res = run_bass_kernel_spmd(nc, maps, core_ids=list(range(NCORE)))
    return res.results


def seg_inputs(lat, cx):
    out = []
    for r in range(NCORE):
        b, s = divmod(r, 4)
        x0 = np.zeros((2052, 1024), np.float32)
        lo, hi = s * 2048 - 2, s * 2048 + 2050
        a, e = max(lo, 0), min(hi, 8192)
        x0[a - lo:e - lo] = lat[b, a:e]
        x1 = np.zeros((132, 1024), np.float32)
        hm = np.zeros(4, np.float32)
        hm[0] = float(s > 0)
        hm[1] = float(s < 3)
        if s < 2:
            lo, hi = s * 128 - 2, s * 128 + 130
            a, e = max(lo, 0), min(hi, 256)
            x1[a - lo:e - lo] = cx[b, a:e]
            hm[2] = float(s > 0)
            hm[3] = float(s < 1)
        out.append({"xT0": fm(x0), "xT1": fm(x1), "hm": rep(hm)})
    return out


def rope_tables():
    r = np.repeat(np.arange(128, dtype=np.float32), 64)
    col = np.tile(np.arange(64, dtype=np.float32), 128)
    nf = 32
    inv = (np.float32(10000.0) ** (-np.arange(nf, dtype=np.float32) / nf)).astype(np.float32)
    ang = np.concatenate([r[:, None] * inv, col[:, None] * inv], -1).astype(np.float32)
    return np.concatenate([np.cos(ang), np.sin(ang)], -1).astype(np.float32)


def prefix_lists(tot, totA, nk, a_half):
    outF, outA = [], []
    for r in range(NCORE):
        b, s = divmod(r, 4)
        base = 4 * b
        F = np.zeros((2, 5) + tot[0].shape[1:], np.float32)
        A = np.ones((128, 2, 5, totA[0].shape[-1]), np.float32)
        fwd0 = [(base + 0, 1), (base + 1, 1)] + [(base + k, 0) for k in range(3)]
        use_f0 = [True, True] + [k < s for k in range(3)]
        bwd0 = [(base + 1, 1), (base + 0, 1)] + [(base + k, 0) for k in (3, 2, 1)]
        use_b0 = [True, True] + [k > s for k in (3, 2, 1)]
        fwd1 = [(base + 0, 1)]
        use_f1 = [s == 1]
        bwd1 = [(base + 1, 1)]
        use_b1 = [s == 0]
        for si, (fl, uf, bl, ub) in enumerate(((fwd0, use_f0, bwd0, use_b0), (fwd1, use_f1, bwd1, use_b1))):
            if si == 1 and s >= 2:
                continue
            for e, ((cr, cs_), u) in enumerate(zip(fl, uf)):
                if u:
                    for k in FWD_KINDS[nk]:
                        F[si, e, k] = tot[cr][cs_, k]
                    A[:, si, e, 0:a_half] = totA[cr][cs_][:, 0:a_half]
            for e, ((cr, cs_), u) in enumerate(zip(bl, ub)):
                if u:
                    for k in BWD_KINDS[nk]:
                        F[si, e, k] = tot[cr][cs_, k]
                    A[:, si, e, a_half:2 * a_half] = totA[cr][cs_][:, a_half:2 * a_half]
        outF.append(F)
        outA.append(A)
    return outF, outA


FWD_KINDS = {4: (0, 2)}
BWD_KINDS = {4: (1, 3)}

SCR0 = {"o_z": [NCH, 128, 1024], "o_g": [NCH, 128, 1024], "o_v": [NCH, 128, 1024], "o_qk": [NCH, 128, 16, 128],
        "o_kst": [NCH, 128, 1024], "o_dl": [NCH, 128, 64], "o_cb": [NCH, 128, 8, 128], "o_xs": [NCH, 128, 1024],
        "o_bm": [NCH, 128, 512]}


def layer0(inp, lat, cx, mods_only=False):
    consts = host_consts()
    j = np.arange(128, dtype=np.float32)
    iota = np.stack([j, 127 - j], 1).astype(np.float32)
    di = np.arange(128)[None, :] - np.arange(128)[:, None]
    diff = np.ascontiguousarray(np.stack([np.maximum(di, 0), np.maximum(-di, 0)], 1).astype(np.float32))
    W = inp["ab_in_w"][0]
    w_xbc = wblk(W[:, 1024:3072], 128)
    w_dt = np.ascontiguousarray(wblk(W[:, 3072:3088], 16)[0])
    w_tm = wblk(np.concatenate([W[:, 0:1024], W[:, 3088:]], 1), 256)
    cwl = np.ascontiguousarray(inp["ab_conv_w"][0].T.reshape(16, 128, 5).transpose(1, 0, 2))
    small = np.concatenate([rep(inp["ssd_dt_bias_f"][0]), rep(inp["ssd_dt_bias_b"][0]), rep(inp["ssd_a_log_f"][0]),
                            rep(inp["ssd_a_log_b"][0]), rep(inp["ret_logit_f"][0]), rep(inp["ret_logit_b"][0])], 1)
    rt = rope_tables()
    ones_cs = np.concatenate([np.ones((128, 64), np.float32), np.zeros((128, 64), np.float32)], 1)[None]
    segs = seg_inputs(lat, cx)
    shared = {"consts": consts, "adaw": wblk(inp["ada_w"][0], 128), "adab": colvec(inp["ada_b"][0]),
              "nw": colvec(inp["norm_mix_w"][0]), "cw": cwl, "cb": colvec(inp["ab_conv_b"][0]), "w_dt": w_dt,
              "small": small, "iota": iota, "w_xbc": w_xbc, "w_tm": w_tm, "cs1": ones_cs}
    maps = []
    for r in range(NCORE):
        b, s = divmod(r, 4)
        m = dict(shared)
        m.update(segs[r])
        m["cvec"] = np.ascontiguousarray(np.stack([colvec(inp["c"][b]), colvec(inp["c_ctx"])], -1))
        m["cs0"] = np.ascontiguousarray(rt[s * 2048:(s + 1) * 2048].reshape(16, 128, 128))
        maps.append(m)
    ins = {k: list(v.shape) for k, v in maps[0].items()}
    tot_shapes = {"mod_out": [128, 48, 2], "o_tot": [2, 4, 128, 1024], "o_totA": [2, 128, 40]}
    res1 = run_prog([l0p1], ins, dict(tot_shapes), dict(SCR0), maps)
    preF, preA = prefix_lists([r_["o_tot"] for r_ in res1], [r_["o_totA"] for r_ in res1], 4, 20)
    shared3 = {"dskip": rep(inp["ssd_d"][0]), "nws": rep(inp["ssd_norm_w"][0]),
               "diff": diff, "nwf": colvec(inp["norm_ffn_w"][0]), "w_out": wblk(inp["ab_out_w"][0], 128),
               "w1": wblk(inp["ffn_w1"][0], 128), "w3": wblk(inp["ffn_w3"][0], 128), "w2": wblk(inp["ffn_w2"][0], 128)}
    maps3 = []
    for r in range(NCORE):
        m = dict(maps[r])
        m.update(shared3)
        m["pre_F"] = preF[r]
        m["pre_A"] = preA[r]
        maps3.append(m)
    ins3 = {k: list(v.shape) for k, v in maps3[0].items()}
    outs3 = {"xo0": [128, 8, 2048], "xo1": [128, 8, 128]}
    scr3 = {"yf": [NCH, 128, 2048], "ycT": [128, 16, NCH * 128]}
    scr3.update(SCR0)
    scr3.update(tot_shapes)
    res3 = run_prog([l0p1, l0p3_sweeps, lambda fw, D: emit_tail(fw, D, 16, False)], ins3, outs3, scr3, maps3)
    lat1 = np.zeros_like(lat)
    cx1 = np.zeros_like(cx)
    for r in range(NCORE):
        b, s = divmod(r, 4)
        lat1[b, s * 2048:(s + 1) * 2048] = res3[r]["xo0"].transpose(2, 1, 0).reshape(2048, 1024)
        if s < 2:
            cx1[b, s * 128:(s + 1) * 128] = res3[r]["xo1"].transpose(2, 1, 0).reshape(128, 1024)
    return lat1, cx1


MS = float(512.0 ** -0.5)
SCR1 = {"o_qT": [NCH, 128, 16, 128], "o_kT": [NCH, 128, 16, 128], "o_ktok": [NCH, 128, 2048],
        "o_vtok": [NCH, 128, 2048], "o_gsc": [NCH, 128, 32], "o_xc": [NCH, 128, 16, 128], "o_sz": [NCH, 128, 16, 128]}


def l1p1a(fw, D):
    K = {"ps": Pool(fw, [128, 512], 8, psum=True, name="ps"), "t512": Pool(fw, [128, 512], 4, name="t512")}
    K["c"] = C = Consts(fw, D["consts"])
    mod = emit_mods(fw, K, D["cvec"], D["adaw"], D["adab"], None, None, D["mod_out"])
    gs = emit_gs(fw, mod, D["nw"], 8, "gs")
    sh = mod[:, 0:8, :]
    hm = fw.sb([128, 4], "hm"); fw.dma(hm[:], D["hm"][:])
    cw = fw.sb([128, 16, 5], "cw"); fw.dma(cw[:], D["cw"][:])
    cb = fw.sb([128, 16], "cb"); fw.dma(cb[:], D["cb"][:])
    wbd = fw.sb([128, 3, 16, 128], "wbd"); fw.dma(wbd[:], D["wbd"][:])
    gw = fw.sb([128, 48, 16], "gw"); fw.dma(gw[:], D["gw"][:])
    gb = fw.sb([128, 16], "gb"); fw.dma(gb[:], D["gb"][:])
    xg = fw.sb([128, 8, 132], "xg")
    hT = fw.sb([128, 8, 132], "hT")
    xmT = fw.sb([128, 16, 128], "xmT")
    xcT = fw.sb([128, 16, 128], "xcT")
    szT = fw.sb([128, 16, 128], "szT")
    wup = Pool(fw, [128, 8, 128], 3, name="wup")
    cacc = Pool(fw, [128, 128], 3, name="cacc")
    qkvp = Pool(fw, [128, 16, 384], 1, name="qkv")
    kTsp = Pool(fw, [128, 16, 128], 2, name="kTs")
    tk = Pool(fw, [128, 2048], 3, name="tk")
    smallp = Pool(fw, [128, 32], 10, name="sm")
    for si, (ch0, nch, ncol) in enumerate(SEGS):
        xT_d = D["xT%d" % si]
        for lc in range(nch):
            ch = ch0 + lc
            c0 = 128 * lc
            fw.dma(xg[:], xT_d[:, :, c0:c0 + 132])
            emit_norm(fw, K, xg, hT, 0, 132, gs, sh, si)
            if lc == 0:
                fw.ts("dve", hT[:, :, 0:2], hT[:, :, 0:2], hm[:, 2 * si:2 * si + 1], ALU.mult)
            if lc == nch - 1:
                fw.ts("dve", hT[:, :, 130:132], hT[:, :, 130:132], hm[:, 2 * si + 1:2 * si + 2], ALU.mult)
            for ft in range(16):
                w = wup.get()
                fw.dma(w[:], D["w_xm"][ft])
                p = K["ps"].get()
                for kt in range(8):
                    fw.mm(p[:, 0:132], w[:, kt, :], hT[:, kt, 0:132], kt == 0, kt == 7)
                fw.copy("act", xmT[:, ft, :], p[:, 2:130])
                acc = cacc.get()
                fw.ts("dve", acc[:], p[:, 0:128], cw[:, ft, 0:1], ALU.mult)
                for k in range(1, 5):
                    fw.stt("dve", acc[:], p[:, k:k + 128], cw[:, ft, k:k + 1], acc[:], ALU.mult, ALU.add)
                fw.act(xcT[:, ft, :], acc[:], AF.Silu, bias=cb[:, ft:ft + 1])
            for ft in range(16):
                w = wup.get()
                fw.dma(w[:], D["w_z"][ft])
                p = K["ps"].get()
                for kt in range(8):
                    fw.mm(p[:, 0:128], w[:, kt, :], hT[:, kt, 2:130], kt == 0, kt == 7)
                fw.act(szT[:, ft, :], p[:, 0:128], AF.Silu)
            fw.dma(D["o_xc"][ch], xcT[:])
            fw.dma(D["o_sz"][ch], szT[:])
            qkv = qkvp.get()
            for ft in range(16):
                p = K["ps"].get()
                fw.mm(p[:, 0:128], wbd[:, 0, ft, :], xcT[:, ft, :])
                fw.mm(p[:, 128:256], wbd[:, 1, ft, :], xcT[:, ft, :])
                fw.mm(p[:, 256:384], wbd[:, 2, ft, :], xmT[:, ft, :])
                fw.copy("act" if ft % 2 else "dve", qkv[:, ft, :], p[:, 0:384])
            kTs = kTsp.get()
            fw.act(kTs[:], qkv[:, :, 128:256], AF.Copy, scale=MS)
            fw.dma(D["o_qT"][ch], qkv[:, :, 0:128])
            fw.dma(D["o_kT"][ch], kTs[:])
            ktok = tk.get(); vtok = tk.get()
            for g4 in range(4):
                pk = K["ps"].get(); pv = K["ps"].get()
                for ff in range(4):
                    ft = 4 * g4 + ff
                    fw.mm(pk[:, 128 * ff:128 * ff + 128], xcT[:, ft, :], wbd[:, 1, ft, :])
                    fw.mm(pv[:, 128 * ff:128 * ff + 128], xmT[:, ft, :], wbd[:, 2, ft, :])
                fw.act(ktok[:, 512 * g4:512 * g4 + 512], pk[:], AF.Copy, scale=MS)
                fw.copy("dve", vtok[:, 512 * g4:512 * g4 + 512], pv[:])
            fw.dma(D["o_ktok"][ch], ktok[:])
            fw.dma(D["o_vtok"][ch], vtok[:])
            pg = K["ps"].get()
            for idx in range(48):
                t_, ft = divmod(idx, 16)
                fw.mm(pg[:, 0:16], qkv[:, ft, 128 * t_:128 * t_ + 128], gw[:, idx, :], idx == 0, idx == 47)
            gt = smallp.get()
            fw.tt("dve", gt[:, 0:16], pg[:, 0:16], gb[:], ALU.add)
            lf = smallp.get()
            fw.copy("dve", lf[:, 0:4], gt[:, 4:8])
            fw.copy("dve", lf[:, 4:8], gt[:, 12:16])
            fw.act(lf[:, 0:8], lf[:, 0:8], AF.Exp, scale=-1.0)
            fw.act(lf[:, 0:8], lf[:, 0:8], AF.Ln, bias=1.0)
            fw.ts("dve", lf[:, 0:8], lf[:, 0:8], -1.0, ALU.mult)
            pf = K["ps"].get()
            fw.mm(pf[:, 0:4], C.m_le, lf[:, 0:4])
            fw.mm(pf[:, 4:8], C.m_ge, lf[:, 4:8])
            fw.mm(pf[:, 8:16], C.ones, lf[:, 0:8])
            gsc = smallp.get()
            fw.copy("dve", gsc[:, 8:16], pf[:, 0:8])
            fw.copy("dve", gsc[:, 24:32], pf[:, 8:16])
            fw.tt("dve", gsc[:, 0:4], gt[:, 0:4], gsc[:, 8:12], ALU.subtract)
            fw.tt("dve", gsc[:, 4:8], gt[:, 8:12], gsc[:, 12:16], ALU.subtract)
            emit_colmax(fw, K, C, gsc[:, 0:8], gsc[:, 16:24], smallp)
            fw.dma(D["o_gsc"][ch], gsc[:])


def emit_colmax(fw, K, C, a, out, smallp):
    pt = K["ps"].get()
    fw.tr(pt[0:8, 0:128], a, C.ident)
    am = smallp.get()
    fw.rmax("dve", am[0:8, 0:1], pt[0:8, 0:128])
    d8 = smallp.get()
    fw.ts("dve", d8[0:8, 0:8], C.ident[0:8, 0:8], am[0:8, 0:1], ALU.mult)
    pb = K["ps"].get()
    fw.mm(pb[:, 0:8], C.ones[0:8, :], d8[0:8, 0:8])
    fw.copy("dve", out, pb[:, 0:8])


def emit_mstate(fw, K, C, ktok, vtok, u, Cst, nst, h, ucol, kpp, pn):
    kp = kpp.get()
    fw.ts("dve", kp[:], ktok[:, 512 * h:512 * h + 512], u[:, ucol:ucol + 1], ALU.mult)
    for dt in range(4):
        pF = K["ps"].get()
        fw.mm(pF[:], kp[:, 128 * dt:128 * dt + 128], vtok[:, 512 * h:512 * h + 512])
        fw.mm(pn[:, dt:dt + 1], kp[:, 128 * dt:128 * dt + 128], C.ones[:, 0:1])
        fw.tt("dve", Cst[:, h, dt, :], Cst[:, h, dt, :], pF[:], ALU.add)
    fw.tt("dve", nst[:, h, :], nst[:, h, :], pn[:, 0:4], ALU.add)


def l1p1b(fw, D):
    K = {"ps": Pool(fw, [128, 512], 6, psum=True, name="ps")}
    K["c"] = C = Consts(fw, D["consts"])
    pn = fw.ps([128, 512], "pn")
    Cst = [fw.sb([128, 4, 4, 512], "Ctot%d" % d) for d in range(2)]
    nst = [fw.sb([128, 4, 4], "ntot%d" % d) for d in range(2)]
    sc = fw.sb([128, 16], "msc")
    tk = Pool(fw, [128, 2048], 4, name="tk")
    kpp = Pool(fw, [128, 512], 3, name="kp")
    smallp = Pool(fw, [128, 32], 10, name="sm")
    for si, (ch0, nch, ncol) in enumerate(SEGS):
        for d in range(2):
            fw.memset("pool", Cst[d][:], 0.0)
            fw.memset("pool", nst[d][:], 0.0)
        fw.memset("pool", sc[:], 0.0)
        for lc in range(nch):
            ch = ch0 + lc
            ktok = tk.get(); fw.dma(ktok[:], D["o_ktok"][ch])
            vtok = tk.get(); fw.dma(vtok[:], D["o_vtok"][ch])
            gsc = smallp.get(); fw.dma(gsc[:], D["o_gsc"][ch])
            w = smallp.get()
            fw.tt("dve", w[:, 0:4], sc[:, 0:4], gsc[:, 16:20], ALU.max)
            fw.tt("dve", w[:, 8:12], sc[:, 0:4], w[:, 0:4], ALU.subtract)
            fw.tt("dve", w[:, 16:20], gsc[:, 16:20], w[:, 0:4], ALU.subtract)
            fw.tt("dve", w[:, 24:28], sc[:, 12:16], gsc[:, 20:24], ALU.add)
            fw.tt("dve", w[:, 24:28], w[:, 24:28], gsc[:, 28:32], ALU.add)
            fw.tt("dve", w[:, 4:8], sc[:, 4:8], w[:, 24:28], ALU.max)
            fw.tt("dve", w[:, 12:16], sc[:, 4:8], w[:, 4:8], ALU.subtract)
            fw.tt("dve", w[:, 20:24], w[:, 24:28], w[:, 4:8], ALU.subtract)
            fw.act(w[:, 8:24], w[:, 8:24], AF.Exp)
            fw.tt("dve", sc[:, 0:4], gsc[:, 24:28], w[:, 0:4], ALU.add)
            fw.copy("dve", sc[:, 4:8], w[:, 4:8])
            fw.tt("dve", sc[:, 8:12], sc[:, 8:12], gsc[:, 24:28], ALU.add)
            fw.tt("dve", sc[:, 12:16], sc[:, 12:16], gsc[:, 28:32], ALU.add)
            u = smallp.get()
            fw.tt("dve", u[:, 0:8], gsc[:, 0:8], gsc[:, 16:24], ALU.subtract)
            fw.act(u[:, 0:8], u[:, 0:8], AF.Exp)
            fw.tt("dve", u[:, 0:8], u[:, 0:8], w[:, 16:24], ALU.mult)
            for d in range(2):
                for h in range(4):
                    fw.ts("pool", Cst[d][:, h].re("p a b -> p (a b)"), Cst[d][:, h].re("p a b -> p (a b)"),
                          w[:, 8 + 4 * d + h:9 + 4 * d + h], ALU.mult)
                    fw.ts("dve", nst[d][:, h, :], nst[d][:, h, :], w[:, 8 + 4 * d + h:9 + 4 * d + h], ALU.mult)
                    emit_mstate(fw, K, C, ktok, vtok, u, Cst[d], nst[d], h, 4 * d + h, kpp, pn)
        for d in range(2):
            for h in range(4):
                fw.dma(D["o_totC"][si, d, h], Cst[d][:, h].re("p a b -> p (a b)"))
            fw.dma(D["o_totn"][si, d], nst[d][:].re("p a b -> p (a b)"))
        fw.dma(D["o_totm"][si], sc[:])


def l1p3_sweeps(fw, D):
    K = {"ps": Pool(fw, [128, 512], 5, psum=True, name="ps")}
    K["c"] = C = Consts(fw, D["consts"])
    pnum = fw.ps([128, 512], "pnum")
    pden = fw.ps([128, 512], "pden")
    pn = fw.ps([128, 512], "pn")
    nwm = fw.sb([128, 16], "nwm"); fw.dma(nwm[:], D["nwm"][:])
    skp = fw.sb([128, 16], "skip"); fw.dma(skp[:], D["skip"][:])
    preM = fw.sb([128, 2, 5, 8], "preM"); fw.dma(preM[:], D["pre_M"][:])
    preN = fw.sb([128, 2, 5, 16], "preN"); fw.dma(preN[:], D["pre_n"][:])
    Cst = fw.sb([128, 4, 4, 512], "C")
    nst = fw.sb([128, 4, 4], "n")
    mst = fw.sb([128, 4], "m")
    ld = {n: Pool(fw, sh, 2, name="ld_" + n) for n, sh in
          (("qT", [128, 16, 128]), ("kT", [128, 16, 128]), ("ktok", [128, 2048]), ("vtok", [128, 2048]), ("gsc", [128, 32]))}
    tk = Pool(fw, [128, 2048], 3, name="tk")
    fmp = Pool(fw, [128, 16, 128], 3, name="fm")
    kpp = Pool(fw, [128, 512], 3, name="kp")
    t128 = Pool(fw, [128, 128], 4, name="t128")
    smallp = Pool(fw, [128, 32], 12, name="sm")
    si, (ch0, nch, ncol) = 0, SEGS[0]
    flat = lambda v: v.re("p a b -> p (a b)")
    for d in range(2):
        fw.memset("pool", Cst[:], 0.0)
        fw.memset("pool", nst[:], 0.0)
        fw.memset("pool", mst[:], 0.0)
        for e in range(5):
            w = smallp.get()
            fw.tt("dve", w[:, 0:4], mst[:], preM[:, d, e, 4:8], ALU.add)
            fw.tt("dve", w[:, 4:8], w[:, 0:4], preM[:, d, e, 0:4], ALU.max)
            fw.tt("dve", w[:, 8:12], w[:, 0:4], w[:, 4:8], ALU.subtract)
            fw.tt("dve", w[:, 12:16], preM[:, d, e, 0:4], w[:, 4:8], ALU.subtract)
            fw.act(w[:, 8:16], w[:, 8:16], AF.Exp)
            fw.copy("dve", mst[:], w[:, 4:8])
            for h in range(4):
                Fb = tk.get()
                fw.dma(Fb[:], D["pre_C"][d, e, h])
                fw.ts("dve", flat(Cst[:, h]), flat(Cst[:, h]), w[:, 8 + h:9 + h], ALU.mult)
                fw.stt("dve", flat(Cst[:, h]), Fb[:], w[:, 12 + h:13 + h], flat(Cst[:, h]), ALU.mult, ALU.add)
                fw.ts("dve", nst[:, h, :], nst[:, h, :], w[:, 8 + h:9 + h], ALU.mult)
                fw.stt("dve", nst[:, h, :], preN[:, d, e, 4 * h:4 * h + 4], w[:, 12 + h:13 + h], nst[:, h, :], ALU.mult, ALU.add)
        order = range(nch) if d == 0 else range(nch - 1, -1, -1)
        for lc in order:
            ch = ch0 + lc
            t = {}
            for n, src in (("qT", "o_qT"), ("kT", "o_kT"), ("ktok", "o_ktok"), ("vtok", "o_vtok"), ("gsc", "o_gsc")):
                t[n] = ld[n].get()
                fw.dma(t[n][:], D[src][ch])
            qT, kT, ktok, vtok, gsc = (t[n] for n in ("qT", "kT", "ktok", "vtok", "gsc"))
            w = smallp.get()
            fw.tt("dve", w[:, 0:4], mst[:], gsc[:, 16 + 4 * d:20 + 4 * d], ALU.max)
            fw.tt("dve", w[:, 4:8], mst[:], w[:, 0:4], ALU.subtract)
            fw.tt("dve", w[:, 8:12], gsc[:, 8 + 4 * d:12 + 4 * d], w[:, 0:4], ALU.add)
            fw.ts("dve", w[:, 8:12], w[:, 8:12], -1.0, ALU.mult)
            fw.tt("dve", w[:, 12:16], gsc[:, 4 * d:4 * d + 4], w[:, 0:4], ALU.subtract)
            fw.act(w[:, 4:16], w[:, 4:16], AF.Exp)
            fw.tt("dve", mst[:], gsc[:, 24 + 4 * d:28 + 4 * d], w[:, 0:4], ALU.add)
            hd = tk.get()
            for h in range(4):
                fw.ts("pool", flat(Cst[:, h]), flat(Cst[:, h]), w[:, 4 + h:5 + h], ALU.mult)
                fw.ts("dve", nst[:, h, :], nst[:, h, :], w[:, 4 + h:5 + h], ALU.mult)
                p = K["ps"].get()
                for dt in range(4):
                    fw.mm(p[:, 0:128], kT[:, 4 * h + dt, :], qT[:, 4 * h + dt, :], dt == 0, dt == 3)
                P = t128.get()
                fw.stt("dve", P[:], p[:, 0:128], w[:, 12 + h:13 + h], C.m_ge if d else C.m_le, ALU.mult, ALU.mult)
                fw.mm(pnum[:], P[:], vtok[:, 512 * h:512 * h + 512], True, False)
                for dt in range(4):
                    fw.mm(pnum[:], qT[:, 4 * h + dt, :], Cst[:, h, dt, :], False, dt == 3)
                fw.mm(pden[:, 0:1], P[:], C.ones[:, 0:1], True, False)
                for dt in range(4):
                    fw.mm(pden[:, 0:1], qT[:, 4 * h + dt, :], nst[:, h, dt:dt + 1], False, dt == 3)
                rr = smallp.get()
                fw.ts("dve", rr[:, 2:3], pden[:, 0:1], -1.0, ALU.mult)
                fw.tt("dve", rr[:, 0:1], rr[:, 2:3], pden[:, 0:1], ALU.max)
                fw.tt("dve", rr[:, 0:1], rr[:, 0:1], w[:, 8 + h:9 + h], ALU.max)
                fw.op("dve", (lambda o, i: (lambda e: e.reciprocal(o, i)))(rr[:, 1:2].ap, rr[:, 0:1].ap), [rr], [rr])
                fw.act(hd[:, 512 * h:512 * h + 512], pnum[:], AF.Copy, scale=rr[:, 1:2])
                emit_mstate(fw, K, C, ktok, vtok, w, Cst, nst, h, 12 + h, kpp, pn)
            if d == 0:
                fw.dma(D["hf"][ch], hd[:])
                continue
            hf = tk.get()
            fw.dma(hf[:], D["hf"][ch])
            fw.tt("pool", hd[:], hd[:], hf[:], ALU.add)
            h4 = lambda v: v.re("p (h e) -> p h e", h=4)
            st_ = smallp.get()
            fw.rsum("dve", st_[:, 0:4], h4(hd[:]))
            fw.ts("dve", st_[:, 0:4], st_[:, 0:4], -1.0 / 512, ALU.mult)
            fw.tt("dve", h4(hd[:]), h4(hd[:]), st_[:, 0:4].unsq(2).bc([128, 4, 512]), ALU.add)
            fw.act(hf[:], hd[:], AF.Square)
            fw.rsum("dve", st_[:, 4:8], h4(hf[:]))
            emit_rstd(fw, st_[:, 4:8], st_[:, 4:8], 1.0 / 512, LN_EPS)
            fw.tt("dve", h4(hd[:]), h4(hd[:]), st_[:, 4:8].unsq(2).bc([128, 4, 512]), ALU.mult)
            hnT = fmp.get()
            for ft in range(16):
                pt = K["ps"].get()
                fw.tr(pt[:, 0:128], hd[:, 128 * ft:128 * ft + 128], C.ident)
                fw.copy("act" if ft % 2 else "dve", hnT[:, ft, :], pt[:, 0:128])
            xc = fmp.get(); fw.dma(xc[:], D["o_xc"][ch])
            sz = fmp.get(); fw.dma(sz[:], D["o_sz"][ch])
            fw.tt("dve", hnT[:], hnT[:], nwm[:].unsq(2).bc([128, 16, 128]), ALU.mult)
            fw.tt("pool", xc[:], xc[:], skp[:].unsq(2).bc([128, 16, 128]), ALU.mult)
            fw.tt("pool", hnT[:], hnT[:], xc[:], ALU.add)
            fw.tt("dve", hnT[:], hnT[:], sz[:], ALU.mult)
            fw.dma(D["ycT"][:, :, 128 * ch:128 * ch + 128], hnT[:])


def layer1(inp, lat1, cx1):
    consts = host_consts()
    segs = seg_inputs(lat1, cx1)
    up = inp["ml_up_w"][0]
    bd = []
    for wn in ("ml_wq", "ml_wk", "ml_wv"):
        w = inp[wn][0]
        t = np.zeros((16, 128, 128), np.float32)
        for b in range(512):
            ft, o = divmod(4 * b, 128)
            t[ft, o:o + 4, o:o + 4] = w[b]
        bd.append(t.transpose(1, 0, 2))
    wbd = np.ascontiguousarray(np.stack(bd, 1))
    cwl = np.ascontiguousarray(inp["ml_conv_w"][0].T.reshape(16, 128, 5).transpose(1, 0, 2))
    shared = {"consts": consts, "adaw": wblk(inp["ada_w"][1], 128), "adab": colvec(inp["ada_b"][1]),
              "nw": colvec(inp["norm_mix_w"][1]), "cw": cwl, "cb": colvec(inp["ml_conv_b"][0]),
              "w_xm": wblk(up[:, 0:2048], 128), "w_z": wblk(up[:, 2048:4096], 128), "wbd": wbd,
              "gw": np.ascontiguousarray(inp["ml_gate_w"][0].reshape(48, 128, 16).transpose(1, 0, 2)),
              "gb": rep(inp["ml_gate_b"][0])}
    maps = []
    for r in range(NCORE):
        b, s = divmod(r, 4)
        m = dict(shared)
        m.update(segs[r])
        m["cvec"] = np.ascontiguousarray(np.stack([colvec(inp["c"][b]), colvec(inp["c_ctx"])], -1))
        maps.append(m)
    ins = {k: list(v.shape) for k, v in maps[0].items()}
    tot_shapes = {"mod_out": [128, 48, 2], "o_totC": [2, 2, 4, 128, 2048], "o_totn": [2, 2, 128, 16], "o_totm": [2, 128, 16]}
    res1 = run_prog([l1p1a, l1p1b], ins, dict(tot_shapes), dict(SCR1), maps)
    maps3 = []
    shared3 = {"nwm": colvec(inp["ml_norm_w"][0]), "skip": colvec(inp["ml_skip"][0]),
               "nwf": colvec(inp["norm_ffn_w"][1]), "w_out": wblk(inp["ml_down_w"][0], 128),
               "w1": wblk(inp["ffn_w1"][1], 128), "w3": wblk(inp["ffn_w3"][1], 128), "w2": wblk(inp["ffn_w2"][1], 128),
               "nwfin": colvec(inp["final_norm_w"])}
    for r in range(NCORE):
        b, s = divmod(r, 4)
        base = 4 * b
        pC = np.zeros((2, 5, 4, 128, 2048), np.float32)
        pn = np.zeros((128, 2, 5, 16), np.float32)
        pM = np.zeros((128, 2, 5, 8), np.float32)
        pM[:, :, :, 0:4] = -1e30
        fwd = [(base + 0, 1, True), (base + 1, 1, True)] + [(base + k, 0, k < s) for k in range(3)]
        bwd = [(base + 1, 1, True), (base + 0, 1, True)] + [(base + k, 0, k > s) for k in (3, 2, 1)]
        for d, lst in enumerate((fwd, bwd)):
            for e, (cr, cs_, u) in enumerate(lst):
                if not u:
                    continue
                pC[d, e] = res1[cr]["o_totC"][cs_, d]
                pn[:, d, e] = res1[cr]["o_totn"][cs_, d]
                tm = res1[cr]["o_totm"][cs_]
                pM[:, d, e, 0:4] = tm[:, 4 * d:4 * d + 4]
                pM[:, d, e, 4:8] = tm[:, 8 + 4 * d:12 + 4 * d]
        m = dict(maps[r])
        m.update(shared3)
        m["pre_C"] = pC
        m["pre_n"] = pn
        m["pre_M"] = pM
        maps3.append(m)
    ins3 = {k: list(v.shape) for k, v in maps3[0].items()}
    outs3 = {"xo0": [128, 8, 2048]}
    scr3 = {"hf": [NCH, 128, 2048], "ycT": [128, 16, NCH * 128]}
    scr3.update(SCR1)
    scr3.update(tot_shapes)
    res3 = run_prog([l1p1a, l1p3_sweeps, lambda fw, D: emit_tail(fw, D, 16, True)], ins3, outs3, scr3, maps3)
    out = np.zeros_like(lat1)
    for r in range(NCORE):
        b, s = divmod(r, 4)
        out[b, s * 2048:(s + 1) * 2048] = res3[r]["xo0"].transpose(2, 1, 0).reshape(2048, 1024)
    return out


def kernel(**inp):
    inp = {k: np.asarray(v, np.float32) for k, v in inp.items()}
    lat1, cx1 = layer0(inp, inp["x"], inp["ctx"])
    return layer1(inp, lat1, cx1)
```

```python
import numpy as np
from contextlib import ExitStack
import concourse.bass as bass
import concourse.mybir as mybir
from concourse.bass_utils import run_bass_kernel_spmd

F32 = mybir.dt.float32
F32R = mybir.dt.float32r
AF = mybir.ActivationFunctionType
ALU = mybir.AluOpType
AX = mybir.AxisListType

NCORE = 8
DEBUG = None
GC = 2
NCH = 17
L = 128
RMS_EPS = 1e-6
LN_EPS = 1e-5


class View:
    def __init__(self, b, ap):
        self.b = b
        self.ap = ap

    def __getitem__(self, k):
        return View(self.b, self.ap[k])

    def unsq(self, ax):
        return View(self.b, self.ap.unsqueeze(ax))

    def bc(self, shape):
        return View(self.b, self.ap.to_broadcast(list(shape)))

    def re(self, s, **kw):
        return View(self.b, self.ap.rearrange(s, **kw))


class Buf:
    def __init__(self, name, t, psum=False, r32=False):
        self.name = name
        self.t = t
        self.psum = psum
        self.r32 = r32
        self.last_w = None
        self.reads = {}

    def __getitem__(self, k):
        return View(self, self.t[k])


def _bufs(*vs):
    out = []
    for v in vs:
        if isinstance(v, View) and v.b not in out:
            out.append(v.b)
    return out


def _ap(v):
    return v.ap if isinstance(v, View) else v


def _o(v):
    return v.ap.bitcast(F32R) if v.b.r32 else v.ap


class FW:
    ENGS = ("pe", "act", "dve", "pool", "sp")
    EOBJ = {"pe": "tensor", "act": "scalar", "dve": "vector", "pool": "gpsimd", "sp": "sync"}
    NSLOT = 8

    def __init__(self, nc, stack):
        self.nc = nc
        self.sem = {e: stack.enter_context(nc.semaphore("s_" + e)) for e in self.ENGS}
        self.count = {e: 0 for e in self.ENGS}
        self.prog = {e: [] for e in self.ENGS}
        self.waited = {e: {} for e in self.ENGS}
        self.slots = {}
        for q in ("sp", "pool", "act"):
            self.slots[q] = [[stack.enter_context(nc.semaphore("d_%s%d" % (q, i))), 0] for i in range(self.NSLOT)]
        self.slot_i = {q: 0 for q in self.slots}
        self.nbuf = 0
        self.stack = None
        self.dq = 0

    def sb(self, shape, name=None, r32=False):
        self.nbuf += 1
        name = (name or "sb") + "_%d" % self.nbuf
        return Buf(name, self.stack.enter_context(self.nc.sbuf_tensor(name, list(shape), F32)), r32=r32)

    def ps(self, shape, name=None):
        self.nbuf += 1
        name = (name or "ps") + "_%d" % self.nbuf
        return Buf(name, self.stack.enter_context(self.nc.psum_tensor(name, list(shape), F32)), psum=True)

    def dram(self, name, shape, kind="Internal"):
        return Buf(name, self.nc.dram_tensor(name, list(shape), F32, kind=kind).ap())

    def _waits(self, eng, reads, writes):
        need = {}

        def add(tok):
            if tok is None:
                return
            s, v = tok
            if id(s) not in need or need[id(s)][1] < v:
                need[id(s)] = (s, v)

        for b in reads:
            add(b.last_w)
            if b.psum:
                for k_, tok in b.reads.items():
                    if k_ != eng:
                        add(tok)
        for b in writes:
            add(b.last_w)
            for tok in b.reads.values():
                add(tok)
        out = []
        for k, (s, v) in need.items():
            if eng == "pe" and s is self.sem["pe"]:
                continue
            if self.waited[eng].get(k, 0) >= v:
                continue
            self.waited[eng][k] = v
            out.append((s, v))
        return out

    def op(self, eng, fn, reads, writes):
        waits = self._waits(eng, reads, writes)
        self.count[eng] += 1
        tok = (self.sem[eng], self.count[eng])
        self.prog[eng].append((waits, fn, self.sem[eng], 1))
        for b in reads:
            b.reads[eng] = tok
        for b in writes:
            b.last_w = tok
            b.reads = {}
        return tok

    def dma(self, out, in_, q=None):
        if out.b.r32:
            q = "pool"
        elif q is None:
            q = ("sp", "pool")[self.dq % 2]
            self.dq += 1
        reads, writes = [in_.b], [out.b]
        sl = self.slots[q]
        i = self.slot_i[q]
        self.slot_i[q] = (i + 1) % len(sl)
        s, v = sl[i]
        waits = self._waits(q, reads, writes)
        if v > 0 and self.waited[q].get(id(s), 0) < v:
            self.waited[q][id(s)] = v
            waits.append((s, v))
        sl[i][1] = v + 16
        tok = (s, v + 16)
        oa, ia = out.ap, in_.ap
        if out.b.r32:
            oa, ia = oa.bitcast(F32R), ia.bitcast(F32R)
        self.prog[q].append((waits, lambda e: e.dma_start(out=oa, in_=ia), s, 16))
        for b in reads:
            b.reads[("dma", q, i)] = tok
        for b in writes:
            b.last_w = tok
            b.reads = {}
        return tok

    def allgather(self, out, in_, groups):
        q = "pool"
        reads, writes = [in_.b], [out.b]
        sl = self.slots[q]
        i = self.slot_i[q]
        self.slot_i[q] = (i + 1) % len(sl)
        s, v = sl[i]
        waits = self._waits(q, reads, writes)
        if v > 0 and self.waited[q].get(id(s), 0) < v:
            self.waited[q][id(s)] = v
            waits.append((s, v))
        sl[i][1] = v + 16
        tok = (s, v + 16)
        oa, ia = out.ap, in_.ap
        self.prog[q].append((waits, lambda e: e.collective_compute("AllGather", ALU.bypass, replica_groups=groups,
                                                                    ins=[ia], outs=[oa]), s, 16))
        for b in reads:
            b.reads[("dma", q, i)] = tok
        for b in writes:
            b.last_w = tok
            b.reads = {}
        return tok

    def flush(self, final=False):
        fin = {}
        for q in self.slots:
            for s, v in self.slots[q]:
                if v > 0:
                    fin[id(s)] = (s, v)
        for e in self.ENGS:
            if self.count[e] > 0:
                fin[id(self.sem[e])] = (self.sem[e], self.count[e])
        progs = self.prog
        self.prog = {e: [] for e in self.ENGS}
        with self.nc.Block() as block:
            for e in self.ENGS:
                extra = []
                for k, (s, v) in fin.items():
                    if self.waited[e].get(k, 0) < v:
                        self.waited[e][k] = v
                        extra.append((s, v))

                def body(eng, prog=progs[e], extra=extra):
                    for waits, fn, s, inc in prog:
                        for ws, wv in waits:
                            eng.wait_ge(ws, wv)
                        fn(eng).then_inc(s, inc)
                    for ws, wv in extra:
                        eng.wait_ge(ws, wv)

                getattr(block, self.EOBJ[e])(body)

    def mm(self, out, lhsT, rhs, start=True, stop=True):
        o, l, r = out.ap, lhsT.ap, rhs.ap
        if lhsT.b.r32 and rhs.b.r32:
            l, r = l.bitcast(F32R), r.bitcast(F32R)
        return self.op("pe", lambda e: e.matmul(o, l, r, start=start, stop=stop), _bufs(lhsT, rhs), _bufs(out))

    def tr(self, out, in_, ident):
        o, i, d = out.ap, in_.ap, ident.ap
        return self.op("pe", lambda e: e.transpose(o, i, d), _bufs(in_, ident), _bufs(out))

    def act(self, out, in_, func, bias=None, scale=None):
        kw = {}
        if bias is not None:
            kw["bias"] = _ap(bias)
        if scale is not None:
            kw["scale"] = _ap(scale)
        o, i = _o(out), in_.ap
        return self.op("act", lambda e: e.activation(o, i, func, **kw), _bufs(in_, bias, scale), _bufs(out))

    def tt(self, eng, out, a, b, op):
        o, x, y = _o(out), a.ap, b.ap
        return self.op(eng, lambda e: e.tensor_tensor(o, x, y, op), _bufs(a, b), _bufs(out))

    def ts(self, eng, out, a, s1, op0, s2=None, op1=None):
        o, x, p1, p2 = _o(out), a.ap, _ap(s1), _ap(s2)
        if op1 is None:
            fn = lambda e: e.tensor_scalar(o, x, p1, None, op0)
        else:
            fn = lambda e: e.tensor_scalar(o, x, p1, p2, op0, op1)
        return self.op(eng, fn, _bufs(a, s1, s2), _bufs(out))

    def stt(self, eng, out, a, sc, b, op0, op1):
        o, x, s, y = _o(out), a.ap, _ap(sc), b.ap
        return self.op(eng, lambda e: e.scalar_tensor_tensor(o, x, s, y, op0, op1), _bufs(a, sc, b), _bufs(out))

    def copy(self, eng, out, in_):
        o, i = _o(out), in_.ap
        if eng == "act":
            return self.op("act", lambda e: e.copy(o, i), _bufs(in_), _bufs(out))
        return self.op(eng, lambda e: e.tensor_copy(o, i), _bufs(in_), _bufs(out))

    def memset(self, eng, out, val):
        o = out.ap
        return self.op(eng, lambda e: e.memset(o, val), [], _bufs(out))

    def rsum(self, eng, out, in_):
        o, i = out.ap, in_.ap
        return self.op(eng, lambda e: e.reduce_sum(o, i, AX.X), _bufs(in_), _bufs(out))

    def rmax(self, eng, out, in_):
        o, i = out.ap, in_.ap
        return self.op(eng, lambda e: e.reduce_max(o, i, AX.X), _bufs(in_), _bufs(out))


class Pool:
    def __init__(self, fw, shape, n, psum=False, name=None, r32=False):
        self.bufs = [fw.ps(shape, name) if psum else fw.sb(shape, name, r32=r32) for _ in range(n)]
        self.i = 0

    def get(self):
        b = self.bufs[self.i]
        self.i = (self.i + 1) % len(self.bufs)
        return b


class Consts:
    def __init__(self, fw, cdram):
        self.t = fw.sb([128, 6, 128], "consts")
        fw.dma(self.t[:], cdram[:])
        self.ident = self.t[:, 0, :]
        self.m_le = self.t[:, 1, :]
        self.m_ge = self.t[:, 2, :]
        self.m_gt = self.t[:, 3, :]
        self.m_lt = self.t[:, 4, :]
        self.ones = self.t[:, 5, :]


def host_consts():
    j = np.arange(128)[:, None]
    i = np.arange(128)[None, :]
    c = np.stack([(j == i), (j <= i), (j >= i), (j > i), (j < i), np.ones((128, 128), bool)], 0).astype(np.float32)
    return np.ascontiguousarray(c.transpose(1, 0, 2))


def emit_norm(fw, K, xin, hout, c0, c1, gs, sh, col, nk=8, dim=1024.0):
    n = c1 - c0
    ss = K["ps"].get()
    for kt in range(nk):
        sq = K["t512"].get()
        fw.act(sq[:, :n], xin[:, kt, c0:c1], AF.Square)
        fw.mm(ss[:, :n], K["c"].ones, sq[:, :n], kt == 0, kt == nk - 1)
    if "nrm" not in K:
        K["nrm"] = Pool(fw, [128, 512], 2, name="nrm")
    t = K["nrm"].get()
    fw.ts("dve", t[:, :n], ss[:, :n], 1.0 / dim, ALU.mult, RMS_EPS, ALU.add)
    fw.act(t[:, :n], t[:, :n], AF.Ln)
    r = K["nrm"].get()
    fw.act(r[:, :n], t[:, :n], AF.Exp, scale=-0.5)
    for kt in range(nk):
        t2 = K["t512"].get()
        fw.stt("dve", t2[:, :n], xin[:, kt, c0:c1], gs[:, kt, col:col + 1], r[:, :n], ALU.mult, ALU.mult)
        if sh is None:
            fw.copy("act", hout[:, kt, c0:c1], t2[:, :n])
        else:
            fw.act(hout[:, kt, c0:c1], t2[:, :n], AF.Identity, bias=sh[:, kt, col:col + 1])


def emit_mods(fw, K, cvec_d, adaw_d, adab_d, nw_d, nwf_d, mod_out_d):
    cv = fw.sb([128, 8, 2], "cvec")
    fw.dma(cv[:], cvec_d[:])
    sc = fw.sb([128, 8, 2], "silu_c")
    fw.act(sc[:], cv[:], AF.Silu)
    ab = fw.sb([128, 48], "adab")
    fw.dma(ab[:], adab_d[:])
    mod = fw.sb([128, 48, 2], "mod")
    wp = Pool(fw, [128, 8, 128], 3, name="adaw")
    for ft in range(48):
        w = wp.get()
        fw.dma(w[:], adaw_d[ft])
        p = K["ps"].get()
        for kt in range(8):
            fw.mm(p[:, 0:2], w[:, kt, :], sc[:, kt, :], kt == 0, kt == 7)
        fw.ts("dve", mod[:, ft, :], p[:, 0:2], ab[:, ft:ft + 1], ALU.add)
    if mod_out_d is not None:
        fw.dma(mod_out_d[:], mod[:])
    return mod


def emit_gs(fw, mod, nw_d, scale_ft0, name):
    nw = fw.sb([128, 8], name + "_nw")
    fw.dma(nw[:], nw_d[:])
    gs = fw.sb([128, 8, 2], name)
    fw.ts("dve", gs[:], mod[:, scale_ft0:scale_ft0 + 8, :], 1.0, ALU.add)
    fw.tt("dve", gs[:], gs[:], nw[:].unsq(2).bc([128, 8, 2]), ALU.mult)
    return gs


SEGS = ((0, 16, 2052), (16, 1, 132))


def l0p1(fw, D):
    st = fw.stack
    K = {"ps": Pool(fw, [128, 512], 8, psum=True, name="ps"), "t512": Pool(fw, [128, 512], 6, name="t512")}
    K["c"] = C = Consts(fw, D["consts"])
    mod = emit_mods(fw, K, D["cvec"], D["adaw"], D["adab"], None, None, D["mod_out"])
    gs = emit_gs(fw, mod, D["nw"], 8, "gs")
    sh = mod[:, 0:8, :]
    hm = fw.sb([128, 4], "hm"); fw.dma(hm[:], D["hm"][:])
    cw = fw.sb([128, 16, 5], "cw"); fw.dma(cw[:], D["cw"][:])
    cb = fw.sb([128, 16], "cb"); fw.dma(cb[:], D["cb"][:])
    wdt = fw.sb([128, 8, 16], "wdt"); fw.dma(wdt[:], D["w_dt"][:])
    sm = fw.sb([128, 72], "small"); fw.dma(sm[:], D["small"][:])
    iota = fw.sb([128, 2], "iota"); fw.dma(iota[:], D["iota"][:])
    negA = fw.sb([128, 32], "negA")
    fw.act(negA[:], sm[:, 32:64], AF.Exp)
    fw.ts("dve", negA[:], negA[:], -1.0, ALU.mult)
    lg = fw.sb([128, 8], "lg")
    fw.act(lg[:], sm[:, 64:72], AF.Exp, scale=-1.0)
    fw.act(lg[:], lg[:], AF.Ln, bias=1.0)
    fw.ts("dve", lg[:], lg[:], -1.0, ALU.mult)
    rsc = fw.sb([128, 16], "rsc")
    ip1 = fw.sb([128, 4], "ip1")
    fw.ts("dve", ip1[:, 0:1], iota[:, 0:1], 1.0, ALU.add)
    fw.ts("dve", ip1[:, 1:2], iota[:, 1:2], 1.0, ALU.add)
    fw.copy("dve", ip1[:, 2:3], iota[:, 1:2])
    fw.copy("dve", ip1[:, 3:4], iota[:, 0:1])
    for n_, (lgc, ic) in enumerate(((0, 0), (4, 1), (0, 2), (4, 3))):
        fw.ts("dve", rsc[:, 4 * n_:4 * n_ + 4], lg[:, lgc:lgc + 4], ip1[:, ic:ic + 1], ALU.mult)
    fw.act(rsc[:], rsc[:], AF.Exp)
    Aret = fw.sb([128, 8], "Aret")
    fw.act(Aret[:], lg[:], AF.Exp, scale=128.0)

    xg = fw.sb([128, 8, 128 * GC + 4], "xg")
    hT = fw.sb([128, 8, 128 * GC + 4], "hT", r32=True)
    xbc = fw.sb([128, 16, 128 * GC], "xbcT")
    wxp = Pool(fw, [128, 8, 128], 3, name="wx")
    wtp = Pool(fw, [128, 8, 256], 2, name="wt", r32=True)
    cacc = Pool(fw, [128, 128], 3, name="cacc")
    tokp = Pool(fw, [128, 1024], 5, name="tok")
    tok5 = Pool(fw, [128, 512], 10, name="tok5")
    fmp = Pool(fw, [128, 16, 128], 2, name="fm")
    kstp = Pool(fw, [128, 1024], 3, name="kst")
    qkst = Pool(fw, [128, 512], 2 * GC, name="qkst")
    smallp = Pool(fw, [128, 64], 8, name="sm")
    Stot = fw.sb([128, 4, 1024], "Stot")
    Pb = fw.sb([128, 20], "Pb")
    Af = fw.sb([128, 20], "Af")
    zst = fw.sb([128, GC, 1024], "zstage")

    for si, (ch0, nch, ncol) in enumerate(SEGS):
        xT_d = D["xT%d" % si]
        cs_d = D["cs%d" % si]
        fw.memset("pool", Stot[:], 0.0)
        fw.memset("pool", Pb[:], 1.0)
        fw.memset("pool", Af[:], 1.0)
        ngrp = (nch + GC - 1) // GC
        for g in range(ngrp):
            gch = min(GC, nch - GC * g)
            W = gch * 128
            c0 = 128 * GC * g
            fw.dma(xg[:, :, 0:W + 4], xT_d[:, :, c0:c0 + W + 4])
            for a in range(0, W + 4, 512):
                b_ = min(a + 512, W + 4)
                emit_norm(fw, K, xg, hT, a, b_, gs, sh, si)
            if g == 0:
                fw.ts("dve", hT[:, :, 0:2], hT[:, :, 0:2], hm[:, 2 * si:2 * si + 1], ALU.mult)
            if g == ngrp - 1:
                fw.ts("dve", hT[:, :, W + 2:W + 4], hT[:, :, W + 2:W + 4], hm[:, 2 * si + 1:2 * si + 2], ALU.mult)
            for ft in range(16):
                w = wxp.get()
                fw.dma(w[:], D["w_xbc"][ft])
                for j in range(gch):
                    p = K["ps"].get()
                    for kt in range(8):
                        fw.mm(p[:, 0:132], w[:, kt, :], hT[:, kt, 128 * j:128 * j + 132], kt == 0, kt == 7)
                    acc = cacc.get()
                    fw.ts("dve", acc[:], p[:, 0:128], cw[:, ft, 0:1], ALU.mult)
                    for k in range(1, 5):
                        fw.stt("dve", acc[:], p[:, k:k + 128], cw[:, ft, k:k + 1], acc[:], ALU.mult, ALU.add)
                    fw.act(xbc[:, ft, 128 * j:128 * j + 128], acc[:], AF.Silu, bias=cb[:, ft:ft + 1])
            kstc = {}
            qkT = {}
            for name, b0, nb in (("z", 0, 4), ("q", 4, 2), ("k", 6, 2), ("v", 8, 4), ("g", 12, 4)):
                stg = {}
                for bi in range(nb):
                    w = wtp.get()
                    fw.dma(w[:], D["w_tm"][b0 + bi])
                    for j in range(gch):
                        ch = ch0 + GC * g + j
                        if bi == 0:
                            stg[j] = zst[:, j, :] if name == "v" else (tokp.get()[:] if name in "zg" else qkst.get()[:])
                        p = K["ps"].get()
                        for kt in range(8):
                            fw.mm(p[:, 0:256], hT[:, kt, 2 + 128 * j:2 + 128 * j + 128], w[:, kt, :], kt == 0, kt == 7)
                        o = stg[j][:, 256 * bi:256 * bi + 256]
                        if name in "zg":
                            fw.act(o, p[:, 0:256], AF.Silu)
                        elif name == "k":
                            fw.act(o, p[:, 0:256], AF.Copy, scale=float(128.0 ** -0.5))
                        else:
                            fw.copy("act", o, p[:, 0:256])
                        if bi == nb - 1:
                            if name == "z":
                                fw.dma(D["o_z"][ch], stg[j])
                            elif name == "g":
                                fw.dma(D["o_g"][ch], stg[j])
                            elif name == "v":
                                fw.dma(D["o_v"][ch], stg[j])
                            elif name == "q":
                                qkT[j] = fmp.get()
                                emit_rope_q(fw, K, C, stg[j], cs_d, GC * g + j, rsc, qkT[j], tok5)
                            else:
                                kstc[j] = emit_rope_k(fw, K, C, stg[j], cs_d, GC * g + j, rsc, qkT[j], tok5, kstp)
                                fw.dma(D["o_qk"][ch], qkT[j][:])
                                fw.dma(D["o_kst"][ch], kstc[j][:])
            for j in range(gch):
                ch = ch0 + GC * g + j
                p = K["ps"].get()
                for kt in range(8):
                    fw.mm(p[:, 0:16], hT[:, kt, 2 + 128 * j:2 + 128 * j + 128], wdt[:, kt, :], kt == 0, kt == 7)
                dl = smallp.get()
                fw.tt("dve", dl[:, 0:16], p[:, 0:16], sm[:, 0:16], ALU.add)
                fw.tt("dve", dl[:, 16:32], p[:, 0:16], sm[:, 16:32], ALU.add)
                fw.act(dl[:, 0:32], dl[:, 0:32], AF.Exp)
                fw.act(dl[:, 0:32], dl[:, 0:32], AF.Ln, bias=1.0)
                fw.tt("dve", dl[:, 32:64], dl[:, 0:32], negA[:], ALU.mult)
                fw.dma(D["o_dl"][ch], dl[:])
                fw.dma(D["o_cb"][ch], xbc[:, 8:16, 128 * j:128 * j + 128])
                xs = tokp.get()
                for ft in range(8):
                    pt = K["ps"].get()
                    fw.tr(pt[:, 0:128], xbc[:, ft, 128 * j:128 * j + 128], C.ident)
                    fw.copy("act" if ft % 2 else "dve", xs[:, 128 * ft:128 * ft + 128], pt[:, 0:128])
                fw.dma(D["o_xs"][ch], xs[:])
                bm = tok5.get()
                for ft in range(4):
                    pt = K["ps"].get()
                    fw.tr(pt[:, 0:128], xbc[:, 8 + ft, 128 * j:128 * j + 128], C.ident)
                    fw.copy("act" if ft % 2 else "dve", bm[:, 128 * ft:128 * ft + 128], pt[:, 0:128])
                fw.dma(D["o_bm"][ch], bm[:])
                emit_ssd_state(fw, K, C, dl, xs, bm, Stot[:, 0, :], Stot[:, 1, :], Af, Pb, smallp, tokp, "p1")
                emit_ret_state(fw, K, kstc[j], zst[:, j, :], Stot[:, 2, :], Stot[:, 3, :], Af, Pb, Aret, tokp, "p1")
        for k in range(4):
            fw.dma(D["o_tot"][si, k], Stot[:, k, :])
        ta = smallp.get()
        fw.copy("dve", ta[:, 0:20], Af[:])
        fw.copy("dve", ta[:, 20:40], Pb[:])
        fw.dma(D["o_totA"][si], ta[:, 0:40])


def emit_rope(fw, src, cs, out, tok5, scale):
    s3 = src.re("p (h d) -> p h d", h=4)
    o3 = out[:].re("p (h d) -> p h d", h=4)
    cos = cs[:, 0:64].unsq(1).bc([128, 4, 64])
    sin = cs[:, 64:128].unsq(1).bc([128, 4, 64])
    t1 = tok5.get(); t2 = tok5.get()
    a = t1[:, 0:256].re("p (h d) -> p h d", h=4)
    b = t1[:, 256:512].re("p (h d) -> p h d", h=4)
    c = t2[:, 0:256].re("p (h d) -> p h d", h=4)
    d = t2[:, 256:512].re("p (h d) -> p h d", h=4)
    fw.tt("dve", a, s3[:, :, 0:64], cos, ALU.mult)
    fw.tt("dve", b, s3[:, :, 64:128], sin, ALU.mult)
    fw.tt("dve", c, s3[:, :, 0:64], sin, ALU.mult)
    fw.tt("dve", d, s3[:, :, 64:128], cos, ALU.mult)
    fw.tt("pool", o3[:, :, 0:64], a, b, ALU.subtract)
    fw.tt("pool", o3[:, :, 64:128], c, d, ALU.add)


def emit_rope_q(fw, K, C, qs, cs_d, lc, rsc, qk, tok5):
    cs = tok5.get()
    fw.dma(cs[:, 0:128], cs_d[lc])
    qr = tok5.get()
    emit_rope(fw, qs, cs[:, 0:128], qr, tok5, 1.0)
    variants = [qr]
    for v in range(2):
        o = tok5.get()
        fw.tt("dve" if v else "pool", o[:].re("p (h d) -> p h d", h=4), qr[:].re("p (h d) -> p h d", h=4),
              rsc[:, 4 * v:4 * v + 4].unsq(2).bc([128, 4, 128]), ALU.mult)
        variants.append(o)
    for vi, src in enumerate(variants):
        for h in range(4):
            pt = K["ps"].get()
            fw.tr(pt[:, 0:128], src[:, 128 * h:128 * h + 128], C.ident)
            fw.copy("act" if h % 2 else "dve", qk[:, 4 * vi + h, :], pt[:, 0:128])


def emit_rope_k(fw, K, C, ks, cs_d, lc, rsc, qk, tok5, kstp):
    cs = tok5.get()
    fw.dma(cs[:, 0:128], cs_d[lc])
    kr = tok5.get()
    emit_rope(fw, ks, cs[:, 0:128], kr, tok5, 1.0)
    for h in range(4):
        pt = K["ps"].get()
        fw.tr(pt[:, 0:128], kr[:, 128 * h:128 * h + 128], C.ident)
        fw.copy("act" if h % 2 else "dve", qk[:, 12 + h, :], pt[:, 0:128])
    kst = kstp.get()
    for v in range(2):
        fw.tt("dve" if v else "pool", kst[:, 512 * v:512 * v + 512].re("p (h d) -> p h d", h=4),
              kr[:].re("p (h d) -> p h d", h=4), rsc[:, 8 + 4 * v:12 + 4 * v].unsq(2).bc([128, 4, 128]), ALU.mult)
    return kst


def emit_ssd_state(fw, K, C, dl, xs, bm, Sf, Sb, Af, Pb, smallp, tokp, mode):
    dirs = {"p1": (0, 1), "f": (0,), "b": (1,)}[mode]
    for d in dirs:
        la = dl[:, 32 + 16 * d:48 + 16 * d]
        pr = K["ps"].get()
        fw.mm(pr[:, 0:16], C.m_lt if d else C.m_gt, la, True, True)
        fw.mm(pr[:, 16:32], C.ones, la, True, True)
        wv = smallp.get()
        fw.act(wv[:, 0:32], pr[:, 0:32], AF.Exp)
        fw.tt("dve", wv[:, 0:16], wv[:, 0:16], dl[:, 16 * d:16 * d + 16], ALU.mult)
        vs = tokp.get()
        fw.tt("pool", vs[:].re("p (h e) -> p h e", h=16), xs[:].re("p (h e) -> p h e", h=16),
              wv[:, 0:16].unsq(2).bc([128, 16, 64]), ALU.mult)
        S = Sb if d else Sf
        for half in range(2):
            pf = K["ps"].get()
            for gg in range(2):
                g4 = 2 * half + gg
                fw.mm(pf[:, 256 * gg:256 * gg + 256], bm[:, 128 * g4:128 * g4 + 128], vs[:, 256 * g4:256 * g4 + 256], True, True)
            Sh = S[:, 512 * half:512 * half + 512].re("p (h e) -> p h e", h=8)
            Abc = wv[:, 16 + 8 * half:24 + 8 * half].unsq(2).bc([128, 8, 64])
            pf3 = pf[:].re("p (h e) -> p h e", h=8)
            if mode == "p1" and d == 1:
                t = K["t512"].get()
                fw.tt("dve", t[:].re("p (h e) -> p h e", h=8), pf3, Pb[:, 8 * half:8 * half + 8].unsq(2).bc([128, 8, 64]), ALU.mult)
                fw.tt("pool", S[:, 512 * half:512 * half + 512], S[:, 512 * half:512 * half + 512], t[:], ALU.add)
            else:
                fw.tt("dve", Sh, Sh, Abc, ALU.mult)
                fw.tt("dve", Sh, Sh, pf3, ALU.add)
        if mode == "p1":
            if d == 0:
                fw.tt("dve", Af[:, 0:16], Af[:, 0:16], wv[:, 16:32], ALU.mult)
            else:
                fw.tt("dve", Pb[:, 0:16], Pb[:, 0:16], wv[:, 16:32], ALU.mult)


def emit_ret_state(fw, K, kst, v, Sf, Sb, Af, Pb, Aret, tokp, mode):
    dirs = {"p1": (0, 1), "f": (0,), "b": (1,)}[mode]
    for d in dirs:
        S = Sb if d else Sf
        for half in range(2):
            pf = K["ps"].get()
            for hh in range(2):
                h = 2 * half + hh
                fw.mm(pf[:, 256 * hh:256 * hh + 256], kst[:, 512 * d + 128 * h:512 * d + 128 * h + 128],
                      v[:, 256 * h:256 * h + 256], True, True)
            Sh = S[:, 512 * half:512 * half + 512].re("p (h e) -> p h e", h=2)
            pf3 = pf[:].re("p (h e) -> p h e", h=2)
            if mode == "p1" and d == 1:
                t = K["t512"].get()
                fw.tt("dve", t[:].re("p (h e) -> p h e", h=2), pf3, Pb[:, 16 + 2 * half:18 + 2 * half].unsq(2).bc([128, 2, 256]), ALU.mult)
                fw.tt("pool", S[:, 512 * half:512 * half + 512], S[:, 512 * half:512 * half + 512], t[:], ALU.add)
            else:
                fw.tt("dve", Sh, Sh, Aret[:, 4 * d + 2 * half:4 * d + 2 * half + 2].unsq(2).bc([128, 2, 256]), ALU.mult)
                fw.tt("dve", Sh, Sh, pf3, ALU.add)
        if mode == "p1":
            if d == 0:
                fw.tt("dve", Af[:, 16:20], Af[:, 16:20], Aret[:, 0:4], ALU.mult)
            else:
                fw.tt("dve", Pb[:, 16:20], Pb[:, 16:20], Aret[:, 4:8], ALU.mult)


def emit_rstd(fw, out, in_, scale, eps):
    fw.ts("dve", out, in_, scale, ALU.mult, eps, ALU.add)
    fw.act(out, out, AF.Ln)
    fw.act(out, out, AF.Exp, scale=-0.5)


def l0p3_sweeps(fw, D):
    K = {"ps": Pool(fw, [128, 512], 4, psum=True, name="ps"), "t512": Pool(fw, [128, 512], 4, name="t512")}
    K["c"] = C = Consts(fw, D["consts"])
    py = [fw.ps([128, 512], "py") for _ in range(2)]
    pr = [fw.ps([128, 512], "pr") for _ in range(2)]
    sm = fw.sb([128, 72], "small"); fw.dma(sm[:], D["small"][:])
    dsk = fw.sb([128, 16], "dskip"); fw.dma(dsk[:], D["dskip"][:])
    nws = fw.sb([128, 1024], "nws"); fw.dma(nws[:], D["nws"][:])
    diff = fw.sb([128, 2, 128], "diff"); fw.dma(diff[:], D["diff"][:])
    lg = fw.sb([128, 8], "lg")
    fw.act(lg[:], sm[:, 64:72], AF.Exp, scale=-1.0)
    fw.act(lg[:], lg[:], AF.Ln, bias=1.0)
    fw.ts("dve", lg[:], lg[:], -1.0, ALU.mult)
    Aret = fw.sb([128, 8], "Aret")
    fw.act(Aret[:], lg[:], AF.Exp, scale=128.0)
    DT = fw.sb([128, 8, 128], "DT")
    for d in range(2):
        for h in range(4):
            fw.ts("dve", DT[:, 4 * d + h, :], diff[:, d, :], lg[:, 4 * d + h:4 * d + h + 1], ALU.mult)
            fw.act(DT[:, 4 * d + h, :], DT[:, 4 * d + h, :], AF.Exp)
            fw.tt("dve", DT[:, 4 * d + h, :], DT[:, 4 * d + h, :], C.m_ge if d else C.m_le, ALU.mult)
    preA = fw.sb([128, 2, 5, 40], "preA"); fw.dma(preA[:], D["pre_A"][:])
    S = fw.sb([128, 4, 1024], "S")
    tokp = Pool(fw, [128, 1024], 7, name="tok")
    t128 = Pool(fw, [128, 128], 8, name="t128")
    smallp = Pool(fw, [128, 64], 8, name="sm")
    ld = {n: Pool(fw, sh, 2, name="ld_" + n) for n, sh in
          (("cb", [128, 8, 128]), ("dl", [128, 64]), ("xs", [128, 1024]), ("bm", [128, 512]),
           ("qk", [128, 16, 128]), ("v", [128, 1024]), ("kst", [128, 1024]))}
    yfp = Pool(fw, [128, 2048], 1, name="yf")
    SCb = fw.sb([128, 4, 128], "SC")
    ycp = Pool(fw, [128, 16, 128], 2, name="ycT")
    h16 = lambda v: v.re("p (h e) -> p h e", h=16)
    h4 = lambda v: v.re("p (h e) -> p h e", h=4)

    for si, (ch0, nch, ncol) in enumerate(SEGS):
        fw.memset("pool", S[:], 0.0)
        for e in range(5):
            for k in range(4):
                Fb = tokp.get()
                fw.dma(Fb[:], D["pre_F"][si, e, k])
                if k < 2:
                    A = preA[:, si, e, 20 * k:20 * k + 16].unsq(2).bc([128, 16, 64])
                    Sv = h16(S[:, k, :])
                else:
                    A = preA[:, si, e, 20 * (k - 2) + 16:20 * (k - 2) + 20].unsq(2).bc([128, 4, 256])
                    Sv = h4(S[:, k, :])
                fw.tt("dve", Sv, Sv, A, ALU.mult)
                fw.tt("pool", S[:, k, :], S[:, k, :], Fb[:], ALU.add)
        for d in range(2):
            order = range(nch) if d == 0 else range(nch - 1, -1, -1)
            for lc in order:
                ch = ch0 + lc
                t = {}
                for n, src in (("cb", "o_cb"), ("dl", "o_dl"), ("xs", "o_xs"), ("bm", "o_bm"),
                               ("qk", "o_qk"), ("v", "o_v"), ("kst", "o_kst")):
                    t[n] = ld[n].get()
                    fw.dma(t[n][:], D[src][ch])
                cbm, dl, xs, bm, qk, v, kst = (t[n] for n in ("cb", "dl", "xs", "bm", "qk", "v", "kst"))
                for g4 in range(4):
                    p = K["ps"].get()
                    fw.mm(p[:, 0:128], cbm[:, g4, :], cbm[:, 4 + g4, :])
                    fw.tt("dve", SCb[:, g4, :], p[:, 0:128], C.m_ge if d else C.m_le, ALU.mult)
                vdt = tokp.get()
                fw.tt("pool", h16(vdt[:]), h16(xs[:]), dl[:, 16 * d:16 * d + 16].unsq(2).bc([128, 16, 64]), ALU.mult)
                la = dl[:, 32 + 16 * d:48 + 16 * d]
                for h in range(16):
                    Lm = t128.get()
                    fw.ts("pool" if h % 2 else "dve", Lm[:], C.m_lt if d else C.m_gt, la[:, h:h + 1], ALU.mult)
                    pd = K["ps"].get()
                    fw.mm(pd[:, 0:128], Lm[:], C.m_ge if d else C.m_le)
                    E = t128.get()
                    fw.act(E[:], pd[:, 0:128], AF.Exp)
                    P = t128.get()
                    fw.tt("dve" if h % 2 else "pool", P[:], E[:], SCb[:, h // 4, :], ALU.mult)
                    fw.mm(py[h // 8][:, 64 * (h % 8):64 * (h % 8) + 64], P[:], vdt[:, 64 * h:64 * h + 64])
                pu = [K["ps"].get(), K["ps"].get()]
                for g4 in range(4):
                    fw.mm(pu[g4 // 2][:, 256 * (g4 % 2):256 * (g4 % 2) + 256], cbm[:, 4 + g4, :], S[:, d, 256 * g4:256 * g4 + 256])
                pc = K["ps"].get()
                fw.mm(pc[:, 0:16], C.m_ge if d else C.m_le, la)
                ecs = smallp.get()
                fw.act(ecs[:, 0:16], pc[:, 0:16], AF.Exp)
                y = tokp.get()
                for half in range(2):
                    tq = K["t512"].get()
                    fw.tt("dve", tq[:].re("p (h e) -> p h e", h=8), pu[half][:].re("p (h e) -> p h e", h=8),
                          ecs[:, 8 * half:8 * half + 8].unsq(2).bc([128, 8, 64]), ALU.mult)
                    fw.tt("dve", y[:, 512 * half:512 * half + 512], tq[:], py[half][:], ALU.add)
                emit_ssd_state(fw, K, C, dl, xs, bm, S[:, 0, :], S[:, 1, :], None, None, smallp, tokp, "b" if d else "f")
                for h in range(4):
                    p = K["ps"].get()
                    fw.mm(p[:, 0:128], qk[:, 12 + h, :], qk[:, h, :])
                    P = t128.get()
                    fw.tt("dve", P[:], p[:, 0:128], DT[:, 4 * d + h, :], ALU.mult)
                    o = pr[h // 2][:, 256 * (h % 2):256 * (h % 2) + 256]
                    fw.mm(o, P[:], v[:, 256 * h:256 * h + 256], True, False)
                    fw.mm(o, qk[:, 4 * (1 + d) + h, :], S[:, 2 + d, 256 * h:256 * h + 256], False, True)
                yr = tokp.get()
                fw.copy("act", yr[:, 0:512], pr[0][:])
                fw.copy("act", yr[:, 512:1024], pr[1][:])
                emit_ret_state(fw, K, kst, v[:], S[:, 2, :], S[:, 3, :], None, None, Aret, tokp, "b" if d else "f")
                if d == 0:
                    fw.dma(D["yf"][ch, :, 0:1024], y[:])
                    fw.dma(D["yf"][ch, :, 1024:2048], yr[:])
                    continue
                yf = yfp.get()
                fw.dma(yf[:], D["yf"][ch])
                sz = tokp.get(); fw.dma(sz[:], D["o_z"][ch])
                sg = tokp.get(); fw.dma(sg[:], D["o_g"][ch])
                fw.tt("dve", y[:], y[:], yf[:, 0:1024], ALU.add)
                tq = tokp.get()
                fw.tt("pool", h16(tq[:]), h16(xs[:]), dsk[:].unsq(2).bc([128, 16, 64]), ALU.mult)
                fw.tt("pool", y[:], y[:], tq[:], ALU.add)
                fw.tt("dve", y[:], y[:], sz[:], ALU.mult)
                fw.act(tq[:], y[:], AF.Square)
                st_ = smallp.get()
                fw.rsum("dve", st_[:, 0:4], h4(tq[:]))
                emit_rstd(fw, st_[:, 0:4], st_[:, 0:4], 1.0 / 256, RMS_EPS)
                fw.tt("dve", h4(y[:]), h4(y[:]), st_[:, 0:4].unsq(2).bc([128, 4, 256]), ALU.mult)
                fw.tt("pool", y[:], y[:], nws[:], ALU.mult)
                fw.tt("dve", yr[:], yr[:], yf[:, 1024:2048], ALU.add)
                fw.rsum("dve", st_[:, 8:12], h4(yr[:]))
                fw.ts("dve", st_[:, 8:12], st_[:, 8:12], -1.0 / 256, ALU.mult)
                fw.tt("dve", h4(yr[:]), h4(yr[:]), st_[:, 8:12].unsq(2).bc([128, 4, 256]), ALU.add)
                fw.act(tq[:], yr[:], AF.Square)
                fw.rsum("dve", st_[:, 12:16], h4(tq[:]))
                emit_rstd(fw, st_[:, 12:16], st_[:, 12:16], 1.0 / 256, LN_EPS)
                fw.tt("dve", h4(yr[:]), h4(yr[:]), st_[:, 12:16].unsq(2).bc([128, 4, 256]), ALU.mult)
                fw.tt("pool", yr[:], yr[:], sg[:], ALU.mult)
                ycT = ycp.get()
                for kt in range(16):
                    src = y if kt < 8 else yr
                    pt = K["ps"].get()
                    fw.tr(pt[:, 0:128], src[:, 128 * (kt % 8):128 * (kt % 8) + 128], C.ident)
                    fw.copy("act" if kt % 2 else "dve", ycT[:, kt, :], pt[:, 0:128])
                fw.dma(D["ycT"][:, :, 128 * ch:128 * ch + 128], ycT[:])


def emit_tail(fw, D, nk_in, last):
    K = {"ps": Pool(fw, [128, 512], 8, psum=True, name="ps"), "t512": Pool(fw, [128, 512], 4, name="t512")}
    K["c"] = Consts(fw, D["consts"])
    mod = fw.sb([128, 48, 2], "mod"); fw.dma(mod[:], D["mod_out"][:])
    gsf = emit_gs(fw, mod, D["nwf"], 32, "gsf")
    shf = mod[:, 24:32, :]
    if last:
        gfin = fw.sb([128, 8, 1], "gfin"); fw.dma(gfin[:, :, 0], D["nwfin"][:])
    ycat = fw.sb([128, nk_in, 512], "ycat", r32=True)
    x1 = fw.sb([128, 8, 512], "x1")
    h2 = fw.sb([128, 8, 512], "h2", r32=True)
    x2 = fw.sb([128, 8, 512], "x2")
    uT = fw.sb([128, 22, 512], "uT", r32=True)
    wop = Pool(fw, [128, nk_in, 128], 1, name="wo", r32=True)
    w13 = Pool(fw, [128, 8, 128], 4, name="w13", r32=True)
    w2p = Pool(fw, [128, 22, 128], 1, name="w2", r32=True)
    for si, (ch0, nch, ncol) in enumerate(SEGS):
        if last and si == 1:
            continue
        for c0 in range(0, nch * 128, 512):
            W = min(512, nch * 128 - c0)
            fw.dma(ycat[:, :, 0:W], D["ycT"][:, :, 128 * ch0 + c0:128 * ch0 + c0 + W])
            fw.dma(x1[:, :, 0:W], D["xT%d" % si][:, :, 2 + c0:2 + c0 + W])
            for ft in range(8):
                w = wop.get()
                fw.dma(w[:], D["w_out"][ft])
                p = K["ps"].get()
                for kt in range(nk_in):
                    fw.mm(p[:, 0:W], w[:, kt, :], ycat[:, kt, 0:W], kt == 0, kt == nk_in - 1)
                fw.stt("dve", x1[:, ft, 0:W], p[:, 0:W], mod[:, 16 + ft, si:si + 1], x1[:, ft, 0:W], ALU.mult, ALU.add)
            emit_norm(fw, K, x1, h2, 0, W, gsf, shf, si)
            for ft in range(22):
                wa = w13.get(); fw.dma(wa[:], D["w1"][ft])
                wb = w13.get(); fw.dma(wb[:], D["w3"][ft])
                pa = K["ps"].get(); pb = K["ps"].get()
                for kt in range(8):
                    fw.mm(pa[:, 0:W], wa[:, kt, :], h2[:, kt, 0:W], kt == 0, kt == 7)
                for kt in range(8):
                    fw.mm(pb[:, 0:W], wb[:, kt, :], h2[:, kt, 0:W], kt == 0, kt == 7)
                sa = K["t512"].get()
                fw.act(sa[:, 0:W], pa[:, 0:W], AF.Silu)
                fw.tt("dve", uT[:, ft, 0:W], sa[:, 0:W], pb[:, 0:W], ALU.mult)
            for f2 in range(8):
                w = w2p.get()
                fw.dma(w[:], D["w2"][f2])
                p = K["ps"].get()
                for ft in range(22):
                    fw.mm(p[:, 0:W], w[:, ft, :], uT[:, ft, 0:W], ft == 0, ft == 21)
                fw.stt("dve", x2[:, f2, 0:W], p[:, 0:W], mod[:, 40 + f2, si:si + 1], x1[:, f2, 0:W], ALU.mult, ALU.add)
            if last:
                emit_norm(fw, K, x2, x1, 0, W, gfin, None, 0)
                fw.dma(D["xo%d" % si][:, :, c0:c0 + W], x1[:, :, 0:W])
            else:
                fw.dma(D["xo%d" % si][:, :, c0:c0 + W], x2[:, :, 0:W])


def fm(x2d):
    T, F = x2d.shape
    return np.ascontiguousarray(x2d.T.reshape(F // 128, 128, T).transpose(1, 0, 2))


def wblk(W, bw):
    K_, N = W.shape
    return np.ascontiguousarray(W.reshape(K_ // 128, 128, N // bw, bw).transpose(2, 1, 0, 3))


def colvec(v):
    return np.ascontiguousarray(v.reshape(-1, 128).T)


def rep(v):
    v = np.asarray(v, np.float32).reshape(-1)
    return np.ascontiguousarray(np.broadcast_to(v[None, :], (128, v.size)))


def run_prog(phases, ins, outs, scratch, in_maps):
    nc = bass.Bass("TRN2", target_bir_lowering=False)
    with ExitStack() as top:
        fw = FW(nc, top)
        D = {}
        for name, shape in ins.items():
            D[name] = fw.dram(name, shape, kind="ExternalInput")
        for name, shape in outs.items():
            D[name] = fw.dram(name, shape, kind="ExternalOutput")
        for name, shape in scratch.items():
            D[name] = fw.dram(name, shape, kind="Internal")
        for ph in phases:
            with ExitStack() as st:
                fw.stack = st
                ph(fw, D)
                fw.flush()
    maps = [{k: np.ascontiguousarray(m[k], dtype=np.float32) for k in ins} for m in in_maps]
    for m in maps:
        for k, shape in ins.items():
            assert tuple(m[k].shape) == tuple(shape), (k, m[k].shape, shape)
    res = run_bass_kernel_spmd(nc, maps, core_ids=list(range(NCORE)))
    return res.results


def seg_inputs(lat, cx):
    out = []
    for r in range(NCORE):
        b, s = divmod(r, 4)
        x0 = np.zeros((2052, 1024), np.float32)
        lo, hi = s * 2048 - 2, s * 2048 + 2050
        a, e = max(lo, 0), min(hi, 8192)
        x0[a - lo:e - lo] = lat[b, a:e]
        x1 = np.zeros((132, 1024), np.float32)
        hm = np.zeros(4, np.float32)
        hm[0] = float(s > 0)
        hm[1] = float(s < 3)
        if s < 2:
            lo, hi = s * 128 - 2, s * 128 + 130
            a, e = max(lo, 0), min(hi, 256)
            x1[a - lo:e - lo] = cx[b, a:e]
            hm[2] = float(s > 0)
            hm[3] = float(s < 1)
        out.append({"xT0": fm(x0), "xT1": fm(x1), "hm": rep(hm)})
    return out


def rope_tables():
    r = np.repeat(np.arange(128, dtype=np.float32), 64)
    col = np.tile(np.arange(64, dtype=np.float32), 128)
    nf = 32
    inv = (np.float32(10000.0) ** (-np.arange(nf, dtype=np.float32) / nf)).astype(np.float32)
    ang = np.concatenate([r[:, None] * inv, col[:, None] * inv], -1).astype(np.float32)
    return np.concatenate([np.cos(ang), np.sin(ang)], -1).astype(np.float32)


def prefix_lists(tot, totA, nk, a_half):
    outF, outA = [], []
    for r in range(NCORE):
        b, s = divmod(r, 4)
        base = 4 * b
        F = np.zeros((2, 5) + tot[0].shape[1:], np.float32)
        A = np.ones((128, 2, 5, totA[0].shape[-1]), np.float32)
        fwd0 = [(base + 0, 1), (base + 1, 1)] + [(base + k, 0) for k in range(3)]
        use_f0 = [True, True] + [k < s for k in range(3)]
        bwd0 = [(base + 1, 1), (base + 0, 1)] + [(base + k, 0) for k in (3, 2, 1)]
        use_b0 = [True, True] + [k > s for k in (3, 2, 1)]
        fwd1 = [(base + 0, 1)]
        use_f1 = [s == 1]
        bwd1 = [(base + 1, 1)]
        use_b1 = [s == 0]
        for si, (fl, uf, bl, ub) in enumerate(((fwd0, use_f0, bwd0, use_b0), (fwd1, use_f1, bwd1, use_b1))):
            if si == 1 and s >= 2:
                continue
            for e, ((cr, cs_), u) in enumerate(zip(fl, uf)):
                if u:
                    for k in FWD_KINDS[nk]:
                        F[si, e, k] = tot[cr][cs_, k]
                    A[:, si, e, 0:a_half] = totA[cr][cs_][:, 0:a_half]
            for e, ((cr, cs_), u) in enumerate(zip(bl, ub)):
                if u:
                    for k in BWD_KINDS[nk]:
                        F[si, e, k] = tot[cr][cs_, k]
                    A[:, si, e, a_half:2 * a_half] = totA[cr][cs_][:, a_half:2 * a_half]
        outF.append(F)
        outA.append(A)
    return outF, outA


FWD_KINDS = {4: (0, 2)}
BWD_KINDS = {4: (1, 3)}

SCR0 = {"o_z": [NCH, 128, 1024], "o_g": [NCH, 128, 1024], "o_v": [NCH, 128, 1024], "o_qk": [NCH, 128, 16, 128],
        "o_kst": [NCH, 128, 1024], "o_dl": [NCH, 128, 64], "o_cb": [NCH, 128, 8, 128], "o_xs": [NCH, 128, 1024],
        "o_bm": [NCH, 128, 512]}


def layer0(inp, lat, cx, mods_only=False):
    consts = host_consts()
    j = np.arange(128, dtype=np.float32)
    iota = np.stack([j, 127 - j], 1).astype(np.float32)
    di = np.arange(128)[None, :] - np.arange(128)[:, None]
    diff = np.ascontiguousarray(np.stack([np.maximum(di, 0), np.maximum(-di, 0)], 1).astype(np.float32))
    W = inp["ab_in_w"][0]
    w_xbc = wblk(W[:, 1024:3072], 128)
    w_dt = np.ascontiguousarray(wblk(W[:, 3072:3088], 16)[0])
    w_tm = wblk(np.concatenate([W[:, 0:1024], W[:, 3088:]], 1), 256)
    cwl = np.ascontiguousarray(inp["ab_conv_w"][0].T.reshape(16, 128, 5).transpose(1, 0, 2))
    small = np.concatenate([rep(inp["ssd_dt_bias_f"][0]), rep(inp["ssd_dt_bias_b"][0]), rep(inp["ssd_a_log_f"][0]),
                            rep(inp["ssd_a_log_b"][0]), rep(inp["ret_logit_f"][0]), rep(inp["ret_logit_b"][0])], 1)
    rt = rope_tables()
    ones_cs = np.concatenate([np.ones((128, 64), np.float32), np.zeros((128, 64), np.float32)], 1)[None]
    segs = seg_inputs(lat, cx)
    shared = {"consts": consts, "adaw": wblk(inp["ada_w"][0], 128), "adab": colvec(inp["ada_b"][0]),
              "nw": colvec(inp["norm_mix_w"][0]), "cw": cwl, "cb": colvec(inp["ab_conv_b"][0]), "w_dt": w_dt,
              "small": small, "iota": iota, "w_xbc": w_xbc, "w_tm": w_tm, "cs1": ones_cs}
    maps = []
    for r in range(NCORE):
        b, s = divmod(r, 4)
        m = dict(shared)
        m.update(segs[r])
        m["cvec"] = np.ascontiguousarray(np.stack([colvec(inp["c"][b]), colvec(inp["c_ctx"])], -1))
        m["cs0"] = np.ascontiguousarray(rt[s * 2048:(s + 1) * 2048].reshape(16, 128, 128))
        maps.append(m)
    ins = {k: list(v.shape) for k, v in maps[0].items()}
    tot_shapes = {"mod_out": [128, 48, 2], "o_tot": [2, 4, 128, 1024], "o_totA": [2, 128, 40]}
    res1 = run_prog([l0p1], ins, dict(tot_shapes), dict(SCR0), maps)
    preF, preA = prefix_lists([r_["o_tot"] for r_ in res1], [r_["o_totA"] for r_ in res1], 4, 20)
    shared3 = {"dskip": rep(inp["ssd_d"][0]), "nws": rep(inp["ssd_norm_w"][0]),
               "diff": diff, "nwf": colvec(inp["norm_ffn_w"][0]), "w_out": wblk(inp["ab_out_w"][0], 128),
               "w1": wblk(inp["ffn_w1"][0], 128), "w3": wblk(inp["ffn_w3"][0], 128), "w2": wblk(inp["ffn_w2"][0], 128)}
    maps3 = []
    for r in range(NCORE):
        m = dict(maps[r])
        m.update(shared3)
        m["pre_F"] = preF[r]
        m["pre_A"] = preA[r]
        maps3.append(m)
    ins3 = {k: list(v.shape) for k, v in maps3[0].items()}
    outs3 = {"xo0": [128, 8, 2048], "xo1": [128, 8, 128]}
    scr3 = {"yf": [NCH, 128, 2048], "ycT": [128, 16, NCH * 128]}
    scr3.update(SCR0)
    scr3.update(tot_shapes)
    res3 = run_prog([l0p1, l0p3_sweeps, lambda fw, D: emit_tail(fw, D, 16, False)], ins3, outs3, scr3, maps3)
    lat1 = np.zeros_like(lat)
    cx1 = np.zeros_like(cx)
    for r in range(NCORE):
        b, s = divmod(r, 4)
        lat1[b, s * 2048:(s + 1) * 2048] = res3[r]["xo0"].transpose(2, 1, 0).reshape(2048, 1024)
        if s < 2:
            cx1[b, s * 128:(s + 1) * 128] = res3[r]["xo1"].transpose(2, 1, 0).reshape(128, 1024)
    return lat1, cx1


MS = float(512.0 ** -0.5)
SCR1 = {"o_qT": [NCH, 128, 16, 128], "o_kT": [NCH, 128, 16, 128], "o_ktok": [NCH, 128, 2048],
        "o_vtok": [NCH, 128, 2048], "o_gsc": [NCH, 128, 32], "o_xc": [NCH, 128, 16, 128], "o_sz": [NCH, 128, 16, 128]}


def l1p1a(fw, D):
    K = {"ps": Pool(fw, [128, 512], 8, psum=True, name="ps"), "t512": Pool(fw, [128, 512], 4, name="t512")}
    K["c"] = C = Consts(fw, D["consts"])
    mod = emit_mods(fw, K, D["cvec"], D["adaw"], D["adab"], None, None, D["mod_out"])
    gs = emit_gs(fw, mod, D["nw"], 8, "gs")
    sh = mod[:, 0:8, :]
    hm = fw.sb([128, 4], "hm"); fw.dma(hm[:], D["hm"][:])
    cw = fw.sb([128, 16, 5], "cw"); fw.dma(cw[:], D["cw"][:])
    cb = fw.sb([128, 16], "cb"); fw.dma(cb[:], D["cb"][:])
    wbd = fw.sb([128, 3, 16, 128], "wbd"); fw.dma(wbd[:], D["wbd"][:])
    gw = fw.sb([128, 48, 16], "gw"); fw.dma(gw[:], D["gw"][:])
    gb = fw.sb([128, 16], "gb"); fw.dma(gb[:], D["gb"][:])
    xg = fw.sb([128, 8, 132], "xg")
    hT = fw.sb([128, 8, 132], "hT")
    xmT = fw.sb([128, 16, 128], "xmT")
    xcT = fw.sb([128, 16, 128], "xcT")
    szT = fw.sb([128, 16, 128], "szT")
    wup = Pool(fw, [128, 8, 128], 3, name="wup")
    cacc = Pool(fw, [128, 128], 3, name="cacc")
    qkvp = Pool(fw, [128, 16, 384], 1, name="qkv")
    kTsp = Pool(fw, [128, 16, 128], 2, name="kTs")
    tk = Pool(fw, [128, 2048], 3, name="tk")
    smallp = Pool(fw, [128, 32], 10, name="sm")
    for si, (ch0, nch, ncol) in enumerate(SEGS):
        xT_d = D["xT%d" % si]
        for lc in range(nch):
            ch = ch0 + lc
            c0 = 128 * lc
            fw.dma(xg[:], xT_d[:, :, c0:c0 + 132])
            emit_norm(fw, K, xg, hT, 0, 132, gs, sh, si)
            if lc == 0:
                fw.ts("dve", hT[:, :, 0:2], hT[:, :, 0:2], hm[:, 2 * si:2 * si + 1], ALU.mult)
            if lc == nch - 1:
                fw.ts("dve", hT[:, :, 130:132], hT[:, :, 130:132], hm[:, 2 * si + 1:2 * si + 2], ALU.mult)
            for ft in range(16):
                w = wup.get()
                fw.dma(w[:], D["w_xm"][ft])
                p = K["ps"].get()
                for kt in range(8):
                    fw.mm(p[:, 0:132], w[:, kt, :], hT[:, kt, 0:132], kt == 0, kt == 7)
                fw.copy("act", xmT[:, ft, :], p[:, 2:130])
                acc = cacc.get()
                fw.ts("dve", acc[:], p[:, 0:128], cw[:, ft, 0:1], ALU.mult)
                for k in range(1, 5):
                    fw.stt("dve", acc[:], p[:, k:k + 128], cw[:, ft, k:k + 1], acc[:], ALU.mult, ALU.add)
                fw.act(xcT[:, ft, :], acc[:], AF.Silu, bias=cb[:, ft:ft + 1])
            for ft in range(16):
                w = wup.get()
                fw.dma(w[:], D["w_z"][ft])
                p = K["ps"].get()
                for kt in range(8):
                    fw.mm(p[:, 0:128], w[:, kt, :], hT[:, kt, 2:130], kt == 0, kt == 7)
                fw.act(szT[:, ft, :], p[:, 0:128], AF.Silu)
            fw.dma(D["o_xc"][ch], xcT[:])
            fw.dma(D["o_sz"][ch], szT[:])
            qkv = qkvp.get()
            for ft in range(16):
                p = K["ps"].get()
                fw.mm(p[:, 0:128], wbd[:, 0, ft, :], xcT[:, ft, :])
                fw.mm(p[:, 128:256], wbd[:, 1, ft, :], xcT[:, ft, :])
                fw.mm(p[:, 256:384], wbd[:, 2, ft, :], xmT[:, ft, :])
                fw.copy("act" if ft % 2 else "dve", qkv[:, ft, :], p[:, 0:384])
            kTs = kTsp.get()
            fw.act(kTs[:], qkv[:, :, 128:256], AF.Copy, scale=MS)
            fw.dma(D["o_qT"][ch], qkv[:, :, 0:128])
            fw.dma(D["o_kT"][ch], kTs[:])
            ktok = tk.get(); vtok = tk.get()
            for g4 in range(4):
                pk = K["ps"].get(); pv = K["ps"].get()
                for ff in range(4):
                    ft = 4 * g4 + ff
                    fw.mm(pk[:, 128 * ff:128 * ff + 128], xcT[:, ft, :], wbd[:, 1, ft, :])
                    fw.mm(pv[:, 128 * ff:128 * ff + 128], xmT[:, ft, :], wbd[:, 2, ft, :])
                fw.act(ktok[:, 512 * g4:512 * g4 + 512], pk[:], AF.Copy, scale=MS)
                fw.copy("dve", vtok[:, 512 * g4:512 * g4 + 512], pv[:])
            fw.dma(D["o_ktok"][ch], ktok[:])
            fw.dma(D["o_vtok"][ch], vtok[:])
            pg = K["ps"].get()
            for idx in range(48):
                t_, ft = divmod(idx, 16)
                fw.mm(pg[:, 0:16], qkv[:, ft, 128 * t_:128 * t_ + 128], gw[:, idx, :], idx == 0, idx == 47)
            gt = smallp.get()
            fw.tt("dve", gt[:, 0:16], pg[:, 0:16], gb[:], ALU.add)
            lf = smallp.get()
            fw.copy("dve", lf[:, 0:4], gt[:, 4:8])
            fw.copy("dve", lf[:, 4:8], gt[:, 12:16])
            fw.act(lf[:, 0:8], lf[:, 0:8], AF.Exp, scale=-1.0)
            fw.act(lf[:, 0:8], lf[:, 0:8], AF.Ln, bias=1.0)
            fw.ts("dve", lf[:, 0:8], lf[:, 0:8], -1.0, ALU.mult)
            pf = K["ps"].get()
            fw.mm(pf[:, 0:4], C.m_le, lf[:, 0:4])
            fw.mm(pf[:, 4:8], C.m_ge, lf[:, 4:8])
            fw.mm(pf[:, 8:16], C.ones, lf[:, 0:8])
            gsc = smallp.get()
            fw.copy("dve", gsc[:, 8:16], pf[:, 0:8])
            fw.copy("dve", gsc[:, 24:32], pf[:, 8:16])
            fw.tt("dve", gsc[:, 0:4], gt[:, 0:4], gsc[:, 8:12], ALU.subtract)
            fw.tt("dve", gsc[:, 4:8], gt[:, 8:12], gsc[:, 12:16], ALU.subtract)
            emit_colmax(fw, K, C, gsc[:, 0:8], gsc[:, 16:24], smallp)
            fw.dma(D["o_gsc"][ch], gsc[:])


def emit_colmax(fw, K, C, a, out, smallp):
    pt = K["ps"].get()
    fw.tr(pt[0:8, 0:128], a, C.ident)
    am = smallp.get()
    fw.rmax("dve", am[0:8, 0:1], pt[0:8, 0:128])
    d8 = smallp.get()
    fw.ts("dve", d8[0:8, 0:8], C.ident[0:8, 0:8], am[0:8, 0:1], ALU.mult)
    pb = K["ps"].get()
    fw.mm(pb[:, 0:8], C.ones[0:8, :], d8[0:8, 0:8])
    fw.copy("dve", out, pb[:, 0:8])


def emit_mstate(fw, K, C, ktok, vtok, u, Cst, nst, h, ucol, kpp, pn):
    kp = kpp.get()
    fw.ts("dve", kp[:], ktok[:, 512 * h:512 * h + 512], u[:, ucol:ucol + 1], ALU.mult)
    for dt in range(4):
        pF = K["ps"].get()
        fw.mm(pF[:], kp[:, 128 * dt:128 * dt + 128], vtok[:, 512 * h:512 * h + 512])
        fw.mm(pn[:, dt:dt + 1], kp[:, 128 * dt:128 * dt + 128], C.ones[:, 0:1])
        fw.tt("dve", Cst[:, h, dt, :], Cst[:, h, dt, :], pF[:], ALU.add)
    fw.tt("dve", nst[:, h, :], nst[:, h, :], pn[:, 0:4], ALU.add)


def l1p1b(fw, D):
    K = {"ps": Pool(fw, [128, 512], 6, psum=True, name="ps")}
    K["c"] = C = Consts(fw, D["consts"])
    pn = fw.ps([128, 512], "pn")
    Cst = [fw.sb([128, 4, 4, 512], "Ctot%d" % d) for d in range(2)]
    nst = [fw.sb([128, 4, 4], "ntot%d" % d) for d in range(2)]
    sc = fw.sb([128, 16], "msc")
    tk = Pool(fw, [128, 2048], 4, name="tk", r32=True)
    kpp = Pool(fw, [128, 512], 3, name="kp", r32=True)
    smallp = Pool(fw, [128, 32], 10, name="sm")
    for si, (ch0, nch, ncol) in enumerate(SEGS):
        for d in range(2):
            fw.memset("pool", Cst[d][:], 0.0)
            fw.memset("pool", nst[d][:], 0.0)
        fw.memset("pool", sc[:], 0.0)
        for lc in range(nch):
            ch = ch0 + lc
            ktok = tk.get(); fw.dma(ktok[:], D["o_ktok"][ch])
            vtok = tk.get(); fw.dma(vtok[:], D["o_vtok"][ch])
            gsc = smallp.get(); fw.dma(gsc[:], D["o_gsc"][ch])
            w = smallp.get()
            fw.tt("dve", w[:, 0:4], sc[:, 0:4], gsc[:, 16:20], ALU.max)
            fw.tt("dve", w[:, 8:12], sc[:, 0:4], w[:, 0:4], ALU.subtract)
            fw.tt("dve", w[:, 16:20], gsc[:, 16:20], w[:, 0:4], ALU.subtract)
            fw.tt("dve", w[:, 24:28], sc[:, 12:16], gsc[:, 20:24], ALU.add)
            fw.tt("dve", w[:, 24:28], w[:, 24:28], gsc[:, 28:32], ALU.add)
            fw.tt("dve", w[:, 4:8], sc[:, 4:8], w[:, 24:28], ALU.max)
            fw.tt("dve", w[:, 12:16], sc[:, 4:8], w[:, 4:8], ALU.subtract)
            fw.tt("dve", w[:, 20:24], w[:, 24:28], w[:, 4:8], ALU.subtract)
            fw.act(w[:, 8:24], w[:, 8:24], AF.Exp)
            fw.tt("dve", sc[:, 0:4], gsc[:, 24:28], w[:, 0:4], ALU.add)
            fw.copy("dve", sc[:, 4:8], w[:, 4:8])
            fw.tt("dve", sc[:, 8:12], sc[:, 8:12], gsc[:, 24:28], ALU.add)
            fw.tt("dve", sc[:, 12:16], sc[:, 12:16], gsc[:, 28:32], ALU.add)
            u = smallp.get()
            fw.tt("dve", u[:, 0:8], gsc[:, 0:8], gsc[:, 16:24], ALU.subtract)
            fw.act(u[:, 0:8], u[:, 0:8], AF.Exp)
            fw.tt("dve", u[:, 0:8], u[:, 0:8], w[:, 16:24], ALU.mult)
            for d in range(2):
                for h in range(4):
                    fw.ts("pool", Cst[d][:, h].re("p a b -> p (a b)"), Cst[d][:, h].re("p a b -> p (a b)"),
                          w[:, 8 + 4 * d + h:9 + 4 * d + h], ALU.mult)
                    fw.ts("dve", nst[d][:, h, :], nst[d][:, h, :], w[:, 8 + 4 * d + h:9 + 4 * d + h], ALU.mult)
                    emit_mstate(fw, K, C, ktok, vtok, u, Cst[d], nst[d], h, 4 * d + h, kpp, pn)
        for d in range(2):
            for h in range(4):
                fw.dma(D["o_totC"][si, d, h], Cst[d][:, h].re("p a b -> p (a b)"))
            fw.dma(D["o_totn"][si, d], nst[d][:].re("p a b -> p (a b)"))
        fw.dma(D["o_totm"][si], sc[:])


def l1p3_sweeps(fw, D):
    K = {"ps": Pool(fw, [128, 512], 5, psum=True, name="ps")}
    K["c"] = C = Consts(fw, D["consts"])
    pnum = fw.ps([128, 512], "pnum")
    pden = fw.ps([128, 512], "pden")
    pn = fw.ps([128, 512], "pn")
    nwm = fw.sb([128, 16], "nwm"); fw.dma(nwm[:], D["nwm"][:])
    skp = fw.sb([128, 16], "skip"); fw.dma(skp[:], D["skip"][:])
    preM = fw.sb([128, 2, 5, 8], "preM"); fw.dma(preM[:], D["pre_M"][:])
    preN = fw.sb([128, 2, 5, 16], "preN"); fw.dma(preN[:], D["pre_n"][:])
    Cst = fw.sb([128, 4, 4, 512], "C")
    nst = fw.sb([128, 4, 4], "n")
    mst = fw.sb([128, 4], "m")
    ld = {n: Pool(fw, sh, 2, name="ld_" + n, r32=(n == "vtok")) for n, sh in
          (("qT", [128, 16, 128]), ("kT", [128, 16, 128]), ("ktok", [128, 2048]), ("vtok", [128, 2048]), ("gsc", [128, 32]))}
    tk = Pool(fw, [128, 2048], 3, name="tk")
    fmp = Pool(fw, [128, 16, 128], 3, name="fm")
    kpp = Pool(fw, [128, 512], 3, name="kp", r32=True)
    t128 = Pool(fw, [128, 128], 4, name="t128", r32=True)
    smallp = Pool(fw, [128, 32], 12, name="sm")
    si, (ch0, nch, ncol) = 0, SEGS[0]
    flat = lambda v: v.re("p a b -> p (a b)")
    for d in range(2):
        fw.memset("pool", Cst[:], 0.0)
        fw.memset("pool", nst[:], 0.0)
        fw.memset("pool", mst[:], 0.0)
        for e in range(5):
            w = smallp.get()
            fw.tt("dve", w[:, 0:4], mst[:], preM[:, d, e, 4:8], ALU.add)
            fw.tt("dve", w[:, 4:8], w[:, 0:4], preM[:, d, e, 0:4], ALU.max)
            fw.tt("dve", w[:, 8:12], w[:, 0:4], w[:, 4:8], ALU.subtract)
            fw.tt("dve", w[:, 12:16], preM[:, d, e, 0:4], w[:, 4:8], ALU.subtract)
            fw.act(w[:, 8:16], w[:, 8:16], AF.Exp)
            fw.copy("dve", mst[:], w[:, 4:8])
            for h in range(4):
                Fb = tk.get()
                fw.dma(Fb[:], D["pre_C"][d, e, h])
                fw.ts("dve", flat(Cst[:, h]), flat(Cst[:, h]), w[:, 8 + h:9 + h], ALU.mult)
                fw.stt("dve", flat(Cst[:, h]), Fb[:], w[:, 12 + h:13 + h], flat(Cst[:, h]), ALU.mult, ALU.add)
                fw.ts("dve", nst[:, h, :], nst[:, h, :], w[:, 8 + h:9 + h], ALU.mult)
                fw.stt("dve", nst[:, h, :], preN[:, d, e, 4 * h:4 * h + 4], w[:, 12 + h:13 + h], nst[:, h, :], ALU.mult, ALU.add)
        order = range(nch) if d == 0 else range(nch - 1, -1, -1)
        for lc in order:
            ch = ch0 + lc
            t = {}
            for n, src in (("qT", "o_qT"), ("kT", "o_kT"), ("ktok", "o_ktok"), ("vtok", "o_vtok"), ("gsc", "o_gsc")):
                t[n] = ld[n].get()
                fw.dma(t[n][:], D[src][ch])
            qT, kT, ktok, vtok, gsc = (t[n] for n in ("qT", "kT", "ktok", "vtok", "gsc"))
            w = smallp.get()
            fw.tt("dve", w[:, 0:4], mst[:], gsc[:, 16 + 4 * d:20 + 4 * d], ALU.max)
            fw.tt("dve", w[:, 4:8], mst[:], w[:, 0:4], ALU.subtract)
            fw.tt("dve", w[:, 8:12], gsc[:, 8 + 4 * d:12 + 4 * d], w[:, 0:4], ALU.add)
            fw.ts("dve", w[:, 8:12], w[:, 8:12], -1.0, ALU.mult)
            fw.tt("dve", w[:, 12:16], gsc[:, 4 * d:4 * d + 4], w[:, 0:4], ALU.subtract)
            fw.act(w[:, 4:16], w[:, 4:16], AF.Exp)
            fw.tt("dve", mst[:], gsc[:, 24 + 4 * d:28 + 4 * d], w[:, 0:4], ALU.add)
            hd = tk.get()
            for h in range(4):
                fw.ts("pool", flat(Cst[:, h]), flat(Cst[:, h]), w[:, 4 + h:5 + h], ALU.mult)
                fw.ts("dve", nst[:, h, :], nst[:, h, :], w[:, 4 + h:5 + h], ALU.mult)
                p = K["ps"].get()
                for dt in range(4):
                    fw.mm(p[:, 0:128], kT[:, 4 * h + dt, :], qT[:, 4 * h + dt, :], dt == 0, dt == 3)
                P = t128.get()
                fw.stt("dve", P[:], p[:, 0:128], w[:, 12 + h:13 + h], C.m_ge if d else C.m_le, ALU.mult, ALU.mult)
                fw.mm(pnum[:], P[:], vtok[:, 512 * h:512 * h + 512], True, False)
                for dt in range(4):
                    fw.mm(pnum[:], qT[:, 4 * h + dt, :], Cst[:, h, dt, :], False, dt == 3)
                fw.mm(pden[:, 0:1], P[:], C.ones[:, 0:1], True, False)
                for dt in range(4):
                    fw.mm(pden[:, 0:1], qT[:, 4 * h + dt, :], nst[:, h, dt:dt + 1], False, dt == 3)
                rr = smallp.get()
                fw.ts("dve", rr[:, 2:3], pden[:, 0:1], -1.0, ALU.mult)
                fw.tt("dve", rr[:, 0:1], rr[:, 2:3], pden[:, 0:1], ALU.max)
                fw.tt("dve", rr[:, 0:1], rr[:, 0:1], w[:, 8 + h:9 + h], ALU.max)
                fw.op("dve", (lambda o, i: (lambda e: e.reciprocal(o, i)))(rr[:, 1:2].ap, rr[:, 0:1].ap), [rr], [rr])
                fw.act(hd[:, 512 * h:512 * h + 512], pnum[:], AF.Copy, scale=rr[:, 1:2])
                emit_mstate(fw, K, C, ktok, vtok, w, Cst, nst, h, 12 + h, kpp, pn)
            if d == 0:
                fw.dma(D["hf"][ch], hd[:])
                continue
            hf = tk.get()
            fw.dma(hf[:], D["hf"][ch])
            fw.tt("pool", hd[:], hd[:], hf[:], ALU.add)
            h4 = lambda v: v.re("p (h e) -> p h e", h=4)
            st_ = smallp.get()
            fw.rsum("dve", st_[:, 0:4], h4(hd[:]))
            fw.ts("dve", st_[:, 0:4], st_[:, 0:4], -1.0 / 512, ALU.mult)
            fw.tt("dve", h4(hd[:]), h4(hd[:]), st_[:, 0:4].unsq(2).bc([128, 4, 512]), ALU.add)
            fw.act(hf[:], hd[:], AF.Square)
            fw.rsum("dve", st_[:, 4:8], h4(hf[:]))
            emit_rstd(fw, st_[:, 4:8], st_[:, 4:8], 1.0 / 512, LN_EPS)
            fw.tt("dve", h4(hd[:]), h4(hd[:]), st_[:, 4:8].unsq(2).bc([128, 4, 512]), ALU.mult)
            hnT = fmp.get()
            for ft in range(16):
                pt = K["ps"].get()
                fw.tr(pt[:, 0:128], hd[:, 128 * ft:128 * ft + 128], C.ident)
                fw.copy("act" if ft % 2 else "dve", hnT[:, ft, :], pt[:, 0:128])
            xc = fmp.get(); fw.dma(xc[:], D["o_xc"][ch])
            sz = fmp.get(); fw.dma(sz[:], D["o_sz"][ch])
            fw.tt("dve", hnT[:], hnT[:], nwm[:].unsq(2).bc([128, 16, 128]), ALU.mult)
            fw.tt("pool", xc[:], xc[:], skp[:].unsq(2).bc([128, 16, 128]), ALU.mult)
            fw.tt("pool", hnT[:], hnT[:], xc[:], ALU.add)
            fw.tt("dve", hnT[:], hnT[:], sz[:], ALU.mult)
            fw.dma(D["ycT"][:, :, 128 * ch:128 * ch + 128], hnT[:])


def layer1(inp, lat1, cx1):
    consts = host_consts()
    segs = seg_inputs(lat1, cx1)
    up = inp["ml_up_w"][0]
    bd = []
    for wn in ("ml_wq", "ml_wk", "ml_wv"):
        w = inp[wn][0]
        t = np.zeros((16, 128, 128), np.float32)
        for b in range(512):
            ft, o = divmod(4 * b, 128)
            t[ft, o:o + 4, o:o + 4] = w[b]
        bd.append(t.transpose(1, 0, 2))
    wbd = np.ascontiguousarray(np.stack(bd, 1))
    cwl = np.ascontiguousarray(inp["ml_conv_w"][0].T.reshape(16, 128, 5).transpose(1, 0, 2))
    shared = {"consts": consts, "adaw": wblk(inp["ada_w"][1], 128), "adab": colvec(inp["ada_b"][1]),
              "nw": colvec(inp["norm_mix_w"][1]), "cw": cwl, "cb": colvec(inp["ml_conv_b"][0]),
              "w_xm": wblk(up[:, 0:2048], 128), "w_z": wblk(up[:, 2048:4096], 128), "wbd": wbd,
              "gw": np.ascontiguousarray(inp["ml_gate_w"][0].reshape(48, 128, 16).transpose(1, 0, 2)),
              "gb": rep(inp["ml_gate_b"][0])}
    maps = []
    for r in range(NCORE):
        b, s = divmod(r, 4)
        m = dict(shared)
        m.update(segs[r])
        m["cvec"] = np.ascontiguousarray(np.stack([colvec(inp["c"][b]), colvec(inp["c_ctx"])], -1))
        maps.append(m)
    ins = {k: list(v.shape) for k, v in maps[0].items()}
    tot_shapes = {"mod_out": [128, 48, 2], "o_totC": [2, 2, 4, 128, 2048], "o_totn": [2, 2, 128, 16], "o_totm": [2, 128, 16]}
    res1 = run_prog([l1p1a, l1p1b], ins, dict(tot_shapes), dict(SCR1), maps)
    maps3 = []
    shared3 = {"nwm": colvec(inp["ml_norm_w"][0]), "skip": colvec(inp["ml_skip"][0]),
               "nwf": colvec(inp["norm_ffn_w"][1]), "w_out": wblk(inp["ml_down_w"][0], 128),
               "w1": wblk(inp["ffn_w1"][1], 128), "w3": wblk(inp["ffn_w3"][1], 128), "w2": wblk(inp["ffn_w2"][1], 128),
               "nwfin": colvec(inp["final_norm_w"])}
    for r in range(NCORE):
        b, s = divmod(r, 4)
        base = 4 * b
        pC = np.zeros((2, 5, 4, 128, 2048), np.float32)
        pn = np.zeros((128, 2, 5, 16), np.float32)
        pM = np.zeros((128, 2, 5, 8), np.float32)
        pM[:, :, :, 0:4] = -1e30
        fwd = [(base + 0, 1, True), (base + 1, 1, True)] + [(base + k, 0, k < s) for k in range(3)]
        bwd = [(base + 1, 1, True), (base + 0, 1, True)] + [(base + k, 0, k > s) for k in (3, 2, 1)]
        for d, lst in enumerate((fwd, bwd)):
            for e, (cr, cs_, u) in enumerate(lst):
                if not u:
                    continue
                pC[d, e] = res1[cr]["o_totC"][cs_, d]
                pn[:, d, e] = res1[cr]["o_totn"][cs_, d]
                tm = res1[cr]["o_totm"][cs_]
                pM[:, d, e, 0:4] = tm[:, 4 * d:4 * d + 4]
                pM[:, d, e, 4:8] = tm[:, 8 + 4 * d:12 + 4 * d]
        m = dict(maps[r])
        m.update(shared3)
        m["pre_C"] = pC
        m["pre_n"] = pn
        m["pre_M"] = pM
        maps3.append(m)
    ins3 = {k: list(v.shape) for k, v in maps3[0].items()}
    outs3 = {"xo0": [128, 8, 2048]}
    scr3 = {"hf": [NCH, 128, 2048], "ycT": [128, 16, NCH * 128]}
    scr3.update(SCR1)
    scr3.update(tot_shapes)
    res3 = run_prog([l1p1a, l1p3_sweeps, lambda fw, D: emit_tail(fw, D, 16, True)], ins3, outs3, scr3, maps3)
    out = np.zeros_like(lat1)
    for r in range(NCORE):
        b, s = divmod(r, 4)
        out[b, s * 2048:(s + 1) * 2048] = res3[r]["xo0"].transpose(2, 1, 0).reshape(2048, 1024)
    return out


def kernel(**inp):
    inp = {k: np.asarray(v, np.float32) for k, v in inp.items()}
    lat1, cx1 = layer0(inp, inp["x"], inp["ctx"])
    return layer1(inp, lat1, cx1)
```

```python
import numpy as np
from contextlib import ExitStack
import concourse.bass as bass
import concourse.mybir as mybir
from concourse.bass_utils import run_bass_kernel_spmd

F32 = mybir.dt.float32
F32R = mybir.dt.float32r
AF = mybir.ActivationFunctionType
ALU = mybir.AluOpType
AX = mybir.AxisListType

NCORE = 8
DEBUG = None
GC = 2
GC1 = 2
NCH = 17
L = 128
RMS_EPS = 1e-6
LN_EPS = 1e-5


class View:
    def __init__(self, b, ap):
        self.b = b
        self.ap = ap

    def __getitem__(self, k):
        return View(self.b, self.ap[k])

    def unsq(self, ax):
        return View(self.b, self.ap.unsqueeze(ax))

    def bc(self, shape):
        return View(self.b, self.ap.to_broadcast(list(shape)))

    def re(self, s, **kw):
        return View(self.b, self.ap.rearrange(s, **kw))


class Buf:
    def __init__(self, name, t, psum=False, r32=False):
        self.name = name
        self.t = t
        self.psum = psum
        self.r32 = r32
        self.last_w = None
        self.reads = {}

    def __getitem__(self, k):
        return View(self, self.t[k])


def _bufs(*vs):
    out = []
    for v in vs:
        if isinstance(v, View) and v.b not in out:
            out.append(v.b)
    return out


def _ap(v):
    return v.ap if isinstance(v, View) else v


def _o(v):
    return v.ap.bitcast(F32R) if v.b.r32 else v.ap


class FW:
    ENGS = ("pe", "act", "dve", "pool", "sp")
    EOBJ = {"pe": "tensor", "act": "scalar", "dve": "vector", "pool": "gpsimd", "sp": "sync"}
    NSLOT = 8

    def __init__(self, nc, stack):
        self.nc = nc
        self.sem = {e: stack.enter_context(nc.semaphore("s_" + e)) for e in self.ENGS}
        self.count = {e: 0 for e in self.ENGS}
        self.prog = {e: [] for e in self.ENGS}
        self.waited = {e: {} for e in self.ENGS}
        self.slots = {}
        for q in ("sp", "pool", "act"):
            self.slots[q] = [[stack.enter_context(nc.semaphore("d_%s%d" % (q, i))), 0] for i in range(self.NSLOT)]
        self.slot_i = {q: 0 for q in self.slots}
        self.nbuf = 0
        self.stack = None
        self.dq = 0

    def sb(self, shape, name=None, r32=False):
        self.nbuf += 1
        name = (name or "sb") + "_%d" % self.nbuf
        return Buf(name, self.stack.enter_context(self.nc.sbuf_tensor(name, list(shape), F32)), r32=r32)

    def ps(self, shape, name=None):
        self.nbuf += 1
        name = (name or "ps") + "_%d" % self.nbuf
        return Buf(name, self.stack.enter_context(self.nc.psum_tensor(name, list(shape), F32)), psum=True)

    def dram(self, name, shape, kind="Internal"):
        return Buf(name, self.nc.dram_tensor(name, list(shape), F32, kind=kind).ap())

    def _waits(self, eng, reads, writes):
        need = {}

        def add(tok):
            if tok is None:
                return
            s, v = tok
            if id(s) not in need or need[id(s)][1] < v:
                need[id(s)] = (s, v)

        for b in reads:
            add(b.last_w)
            if b.psum:
                for k_, tok in b.reads.items():
                    if k_ != eng:
                        add(tok)
        for b in writes:
            add(b.last_w)
            for tok in b.reads.values():
                add(tok)
        out = []
        for k, (s, v) in need.items():
            if eng == "pe" and s is self.sem["pe"]:
                continue
            if self.waited[eng].get(k, 0) >= v:
                continue
            self.waited[eng][k] = v
            out.append((s, v))
        return out

    def op(self, eng, fn, reads, writes):
        waits = self._waits(eng, reads, writes)
        self.count[eng] += 1
        tok = (self.sem[eng], self.count[eng])
        self.prog[eng].append((waits, fn, self.sem[eng], 1))
        for b in reads:
            b.reads[eng] = tok
        for b in writes:
            b.last_w = tok
            b.reads = {}
        return tok

    def dma(self, out, in_, q=None):
        if out.b.r32:
            q = "pool"
        elif q is None:
            q = ("sp", "pool")[self.dq % 2]
            self.dq += 1
        reads, writes = [in_.b], [out.b]
        sl = self.slots[q]
        i = self.slot_i[q]
        self.slot_i[q] = (i + 1) % len(sl)
        s, v = sl[i]
        waits = self._waits(q, reads, writes)
        if v > 0 and self.waited[q].get(id(s), 0) < v:
            self.waited[q][id(s)] = v
            waits.append((s, v))
        sl[i][1] = v + 16
        tok = (s, v + 16)
        oa, ia = out.ap, in_.ap
        if out.b.r32:
            oa, ia = oa.bitcast(F32R), ia.bitcast(F32R)
        self.prog[q].append((waits, lambda e: e.dma_start(out=oa, in_=ia), s, 16))
        for b in reads:
            b.reads[("dma", q, i)] = tok
        for b in writes:
            b.last_w = tok
            b.reads = {}
        return tok

    def allgather(self, out, in_, groups):
        q = "pool"
        reads, writes = [in_.b], [out.b]
        sl = self.slots[q]
        i = self.slot_i[q]
        self.slot_i[q] = (i + 1) % len(sl)
        s, v = sl[i]
        waits = self._waits(q, reads, writes)
        if v > 0 and self.waited[q].get(id(s), 0) < v:
            self.waited[q][id(s)] = v
            waits.append((s, v))
        sl[i][1] = v + 16
        tok = (s, v + 16)
        oa, ia = out.ap, in_.ap
        self.prog[q].append((waits, lambda e: e.collective_compute("AllGather", ALU.bypass, replica_groups=groups,
                                                                    ins=[ia], outs=[oa]), s, 16))
        for b in reads:
            b.reads[("dma", q, i)] = tok
        for b in writes:
            b.last_w = tok
            b.reads = {}
        return tok

    def flush(self, final=False):
        fin = {}
        for q in self.slots:
            for s, v in self.slots[q]:
                if v > 0:
                    fin[id(s)] = (s, v)
        for e in self.ENGS:
            if self.count[e] > 0:
                fin[id(self.sem[e])] = (self.sem[e], self.count[e])
        progs = self.prog
        self.prog = {e: [] for e in self.ENGS}
        with self.nc.Block() as block:
            for e in self.ENGS:
                extra = []
                for k, (s, v) in fin.items():
                    if self.waited[e].get(k, 0) < v:
                        self.waited[e][k] = v
                        extra.append((s, v))

                def body(eng, prog=progs[e], extra=extra):
                    for waits, fn, s, inc in prog:
                        for ws, wv in waits:
                            eng.wait_ge(ws, wv)
                        fn(eng).then_inc(s, inc)
                    for ws, wv in extra:
                        eng.wait_ge(ws, wv)

                getattr(block, self.EOBJ[e])(body)

    def mm(self, out, lhsT, rhs, start=True, stop=True):
        o, l, r = out.ap, lhsT.ap, rhs.ap
        if lhsT.b.r32 and rhs.b.r32:
            l, r = l.bitcast(F32R), r.bitcast(F32R)
        return self.op("pe", lambda e: e.matmul(o, l, r, start=start, stop=stop), _bufs(lhsT, rhs), _bufs(out))

    def tr(self, out, in_, ident):
        o, i, d = out.ap, in_.ap, ident.ap
        return self.op("pe", lambda e: e.transpose(o, i, d), _bufs(in_, ident), _bufs(out))

    def act(self, out, in_, func, bias=None, scale=None):
        kw = {}
        if bias is not None:
            kw["bias"] = _ap(bias)
        if scale is not None:
            kw["scale"] = _ap(scale)
        o, i = _o(out), in_.ap
        return self.op("act", lambda e: e.activation(o, i, func, **kw), _bufs(in_, bias, scale), _bufs(out))

    def tt(self, eng, out, a, b, op):
        o, x, y = _o(out), a.ap, b.ap
        return self.op(eng, lambda e: e.tensor_tensor(o, x, y, op), _bufs(a, b), _bufs(out))

    def ts(self, eng, out, a, s1, op0, s2=None, op1=None):
        o, x, p1, p2 = _o(out), a.ap, _ap(s1), _ap(s2)
        if op1 is None:
            fn = lambda e: e.tensor_scalar(o, x, p1, None, op0)
        else:
            fn = lambda e: e.tensor_scalar(o, x, p1, p2, op0, op1)
        return self.op(eng, fn, _bufs(a, s1, s2), _bufs(out))

    def stt(self, eng, out, a, sc, b, op0, op1):
        o, x, s, y = _o(out), a.ap, _ap(sc), b.ap
        return self.op(eng, lambda e: e.scalar_tensor_tensor(o, x, s, y, op0, op1), _bufs(a, sc, b), _bufs(out))

    def copy(self, eng, out, in_):
        o, i = _o(out), in_.ap
        if eng == "act":
            return self.op("act", lambda e: e.copy(o, i), _bufs(in_), _bufs(out))
        return self.op(eng, lambda e: e.tensor_copy(o, i), _bufs(in_), _bufs(out))

    def memset(self, eng, out, val):
        o = out.ap
        return self.op(eng, lambda e: e.memset(o, val), [], _bufs(out))

    def rsum(self, eng, out, in_):
        o, i = out.ap, in_.ap
        return self.op(eng, lambda e: e.reduce_sum(o, i, AX.X), _bufs(in_), _bufs(out))

    def rmax(self, eng, out, in_):
        o, i = out.ap, in_.ap
        return self.op(eng, lambda e: e.reduce_max(o, i, AX.X), _bufs(in_), _bufs(out))


class Pool:
    def __init__(self, fw, shape, n, psum=False, name=None, r32=False):
        self.bufs = [fw.ps(shape, name) if psum else fw.sb(shape, name, r32=r32) for _ in range(n)]
        self.i = 0

    def get(self):
        b = self.bufs[self.i]
        self.i = (self.i + 1) % len(self.bufs)
        return b


class Consts:
    def __init__(self, fw, cdram):
        self.t = fw.sb([128, 6, 128], "consts")
        fw.dma(self.t[:], cdram[:])
        self.ident = self.t[:, 0, :]
        self.m_le = self.t[:, 1, :]
        self.m_ge = self.t[:, 2, :]
        self.m_gt = self.t[:, 3, :]
        self.m_lt = self.t[:, 4, :]
        self.ones = self.t[:, 5, :]


def host_consts():
    j = np.arange(128)[:, None]
    i = np.arange(128)[None, :]
    c = np.stack([(j == i), (j <= i), (j >= i), (j > i), (j < i), np.ones((128, 128), bool)], 0).astype(np.float32)
    return np.ascontiguousarray(c.transpose(1, 0, 2))


def emit_norm(fw, K, xin, hout, c0, c1, gs, sh, col, nk=8, dim=1024.0):
    n = c1 - c0
    ss = K["ps"].get()
    for kt in range(nk):
        sq = K["t512"].get()
        fw.act(sq[:, :n], xin[:, kt, c0:c1], AF.Square)
        fw.mm(ss[:, :n], K["c"].ones, sq[:, :n], kt == 0, kt == nk - 1)
    if "nrm" not in K:
        K["nrm"] = Pool(fw, [128, 512], 2, name="nrm")
    t = K["nrm"].get()
    fw.ts("dve", t[:, :n], ss[:, :n], 1.0 / dim, ALU.mult, RMS_EPS, ALU.add)
    fw.act(t[:, :n], t[:, :n], AF.Ln)
    r = K["nrm"].get()
    fw.act(r[:, :n], t[:, :n], AF.Exp, scale=-0.5)
    for kt in range(nk):
        t2 = K["t512"].get()
        fw.stt("dve", t2[:, :n], xin[:, kt, c0:c1], gs[:, kt, col:col + 1], r[:, :n], ALU.mult, ALU.mult)
        if sh is None:
            fw.copy("act", hout[:, kt, c0:c1], t2[:, :n])
        else:
            fw.act(hout[:, kt, c0:c1], t2[:, :n], AF.Identity, bias=sh[:, kt, col:col + 1])


def emit_mods(fw, K, cvec_d, adaw_d, adab_d, nw_d, nwf_d, mod_out_d):
    cv = fw.sb([128, 8, 2], "cvec")
    fw.dma(cv[:], cvec_d[:])
    sc = fw.sb([128, 8, 2], "silu_c")
    fw.act(sc[:], cv[:], AF.Silu)
    ab = fw.sb([128, 48], "adab")
    fw.dma(ab[:], adab_d[:])
    mod = fw.sb([128, 48, 2], "mod")
    wp = Pool(fw, [128, 8, 128], 3, name="adaw")
    for ft in range(48):
        w = wp.get()
        fw.dma(w[:], adaw_d[ft])
        p = K["ps"].get()
        for kt in range(8):
            fw.mm(p[:, 0:2], w[:, kt, :], sc[:, kt, :], kt == 0, kt == 7)
        fw.ts("dve", mod[:, ft, :], p[:, 0:2], ab[:, ft:ft + 1], ALU.add)
    if mod_out_d is not None:
        fw.dma(mod_out_d[:], mod[:])
    return mod


def emit_gs(fw, mod, nw_d, scale_ft0, name):
    nw = fw.sb([128, 8], name + "_nw")
    fw.dma(nw[:], nw_d[:])
    gs = fw.sb([128, 8, 2], name)
    fw.ts("dve", gs[:], mod[:, scale_ft0:scale_ft0 + 8, :], 1.0, ALU.add)
    fw.tt("dve", gs[:], gs[:], nw[:].unsq(2).bc([128, 8, 2]), ALU.mult)
    return gs


SEGS = ((0, 16, 2052), (16, 1, 132))


def l0p1(fw, D, totals=True):
    st = fw.stack
    K = {"ps": Pool(fw, [128, 512], 8, psum=True, name="ps"), "t512": Pool(fw, [128, 512], 6, name="t512")}
    K["c"] = C = Consts(fw, D["consts"])
    mod = emit_mods(fw, K, D["cvec"], D["adaw"], D["adab"], None, None, D["mod_out"])
    gs = emit_gs(fw, mod, D["nw"], 8, "gs")
    sh = mod[:, 0:8, :]
    hm = fw.sb([128, 4], "hm"); fw.dma(hm[:], D["hm"][:])
    cw = fw.sb([128, 16, 5], "cw"); fw.dma(cw[:], D["cw"][:])
    cb = fw.sb([128, 16], "cb"); fw.dma(cb[:], D["cb"][:])
    wdt = fw.sb([128, 8, 16], "wdt"); fw.dma(wdt[:], D["w_dt"][:])
    sm = fw.sb([128, 72], "small"); fw.dma(sm[:], D["small"][:])
    iota = fw.sb([128, 2], "iota"); fw.dma(iota[:], D["iota"][:])
    negA = fw.sb([128, 32], "negA")
    fw.act(negA[:], sm[:, 32:64], AF.Exp)
    fw.ts("dve", negA[:], negA[:], -1.0, ALU.mult)
    lg = fw.sb([128, 8], "lg")
    fw.act(lg[:], sm[:, 64:72], AF.Exp, scale=-1.0)
    fw.act(lg[:], lg[:], AF.Ln, bias=1.0)
    fw.ts("dve", lg[:], lg[:], -1.0, ALU.mult)
    rsc = fw.sb([128, 16], "rsc")
    ip1 = fw.sb([128, 4], "ip1")
    fw.ts("dve", ip1[:, 0:1], iota[:, 0:1], 1.0, ALU.add)
    fw.ts("dve", ip1[:, 1:2], iota[:, 1:2], 1.0, ALU.add)
    fw.copy("dve", ip1[:, 2:3], iota[:, 1:2])
    fw.copy("dve", ip1[:, 3:4], iota[:, 0:1])
    for n_, (lgc, ic) in enumerate(((0, 0), (4, 1), (0, 2), (4, 3))):
        fw.ts("dve", rsc[:, 4 * n_:4 * n_ + 4], lg[:, lgc:lgc + 4], ip1[:, ic:ic + 1], ALU.mult)
    fw.act(rsc[:], rsc[:], AF.Exp)
    Aret = fw.sb([128, 8], "Aret")
    fw.act(Aret[:], lg[:], AF.Exp, scale=128.0)

    xg = fw.sb([128, 8, 128 * GC + 4], "xg")
    hT = fw.sb([128, 8, 128 * GC + 4], "hT", r32=True)
    xbc = fw.sb([128, 16, 128 * GC], "xbcT")
    wxp = Pool(fw, [128, 8, 128], 3, name="wx", r32=True)
    wtp = Pool(fw, [128, 8, 256], 2, name="wt", r32=True)
    cacc = Pool(fw, [128, 128], 3, name="cacc")
    tokp = Pool(fw, [128, 1024], 5, name="tok")
    tok5 = Pool(fw, [128, 512], 10, name="tok5")
    fmp = Pool(fw, [128, 16, 128], 2, name="fm")
    kstp = Pool(fw, [128, 1024], 3, name="kst")
    qkst = Pool(fw, [128, 512], 2 * GC, name="qkst")
    smallp = Pool(fw, [128, 64], 8, name="sm")
    Stot = fw.sb([128, 4, 1024], "Stot")
    Pb = fw.sb([128, 20], "Pb")
    Af = fw.sb([128, 20], "Af")
    zst = fw.sb([128, GC, 1024], "zstage")

    for si, (ch0, nch, ncol) in enumerate(SEGS):
        xT_d = D["xT%d" % si]
        cs_d = D["cs%d" % si]
        fw.memset("pool", Stot[:], 0.0)
        fw.memset("pool", Pb[:], 1.0)
        fw.memset("pool", Af[:], 1.0)
        ngrp = (nch + GC - 1) // GC
        for g in range(ngrp):
            gch = min(GC, nch - GC * g)
            W = gch * 128
            c0 = 128 * GC * g
            fw.dma(xg[:, :, 0:W + 4], xT_d[:, :, c0:c0 + W + 4])
            for a in range(0, W + 4, 512):
                b_ = min(a + 512, W + 4)
                emit_norm(fw, K, xg, hT, a, b_, gs, sh, si)
            if g == 0:
                fw.ts("dve", hT[:, :, 0:2], hT[:, :, 0:2], hm[:, 2 * si:2 * si + 1], ALU.mult)
            if g == ngrp - 1:
                fw.ts("dve", hT[:, :, W + 2:W + 4], hT[:, :, W + 2:W + 4], hm[:, 2 * si + 1:2 * si + 2], ALU.mult)
            for ft in range(16):
                w = wxp.get()
                fw.dma(w[:], D["w_xbc"][ft])
                p = K["ps"].get()
                for kt in range(8):
                    fw.mm(p[:, 0:W + 4], w[:, kt, :], hT[:, kt, 0:W + 4], kt == 0, kt == 7)
                for j in range(gch):
                    acc = cacc.get()
                    fw.ts("dve", acc[:], p[:, 128 * j:128 * j + 128], cw[:, ft, 0:1], ALU.mult)
                    for k in range(1, 5):
                        fw.stt("dve", acc[:], p[:, 128 * j + k:128 * j + k + 128], cw[:, ft, k:k + 1], acc[:], ALU.mult, ALU.add)
                    fw.act(xbc[:, ft, 128 * j:128 * j + 128], acc[:], AF.Silu, bias=cb[:, ft:ft + 1])
            kstc = {}
            qkT = {}
            for name, b0, nb in (("z", 0, 4), ("q", 4, 2), ("k", 6, 2), ("v", 8, 4), ("g", 12, 4)):
                stg = {}
                for bi in range(nb):
                    w = wtp.get()
                    fw.dma(w[:], D["w_tm"][b0 + bi])
                    for j in range(gch):
                        ch = ch0 + GC * g + j
                        if bi == 0:
                            stg[j] = zst[:, j, :] if name == "v" else (tokp.get()[:] if name in "zg" else qkst.get()[:])
                        p = K["ps"].get()
                        for kt in range(8):
                            fw.mm(p[:, 0:256], hT[:, kt, 2 + 128 * j:2 + 128 * j + 128], w[:, kt, :], kt == 0, kt == 7)
                        o = stg[j][:, 256 * bi:256 * bi + 256]
                        if name in "zg":
                            fw.act(o, p[:, 0:256], AF.Silu)
                        elif name == "k":
                            fw.act(o, p[:, 0:256], AF.Copy, scale=float(128.0 ** -0.5))
                        else:
                            fw.copy("act", o, p[:, 0:256])
                        if bi == nb - 1:
                            if name == "z":
                                fw.dma(D["o_z"][ch], stg[j])
                            elif name == "g":
                                fw.dma(D["o_g"][ch], stg[j])
                            elif name == "v":
                                fw.dma(D["o_v"][ch], stg[j])
                            elif name == "q":
                                qkT[j] = fmp.get()
                                emit_rope_q(fw, K, C, stg[j], cs_d, GC * g + j, rsc, qkT[j], tok5)
                            else:
                                kstc[j] = emit_rope_k(fw, K, C, stg[j], cs_d, GC * g + j, rsc, qkT[j], tok5, kstp)
                                fw.dma(D["o_qk"][ch], qkT[j][:])
                                fw.dma(D["o_kst"][ch], kstc[j][:])
            for j in range(gch):
                ch = ch0 + GC * g + j
                p = K["ps"].get()
                for kt in range(8):
                    fw.mm(p[:, 0:16], hT[:, kt, 2 + 128 * j:2 + 128 * j + 128], wdt[:, kt, :], kt == 0, kt == 7)
                dl = smallp.get()
                fw.tt("dve", dl[:, 0:16], p[:, 0:16], sm[:, 0:16], ALU.add)
                fw.tt("dve", dl[:, 16:32], p[:, 0:16], sm[:, 16:32], ALU.add)
                fw.act(dl[:, 0:32], dl[:, 0:32], AF.Exp)
                fw.act(dl[:, 0:32], dl[:, 0:32], AF.Ln, bias=1.0)
                fw.tt("dve", dl[:, 32:64], dl[:, 0:32], negA[:], ALU.mult)
                fw.dma(D["o_dl"][ch], dl[:])
                fw.dma(D["o_cb"][ch], xbc[:, 8:16, 128 * j:128 * j + 128])
                xs = tokp.get()
                for ft in range(8):
                    pt = K["ps"].get()
                    fw.tr(pt[:, 0:128], xbc[:, ft, 128 * j:128 * j + 128], C.ident)
                    fw.copy("act" if ft % 2 else "dve", xs[:, 128 * ft:128 * ft + 128], pt[:, 0:128])
                fw.dma(D["o_xs"][ch], xs[:])
                bm = tok5.get()
                for ft in range(4):
                    pt = K["ps"].get()
                    fw.tr(pt[:, 0:128], xbc[:, 8 + ft, 128 * j:128 * j + 128], C.ident)
                    fw.copy("act" if ft % 2 else "dve", bm[:, 128 * ft:128 * ft + 128], pt[:, 0:128])
                fw.dma(D["o_bm"][ch], bm[:])
                if totals:
                    emit_ssd_state(fw, K, C, dl, xs, bm, Stot[:, 0, :], Stot[:, 1, :], Af, Pb, smallp, tokp, "p1")
                    emit_ret_state(fw, K, kstc[j], zst[:, j, :], Stot[:, 2, :], Stot[:, 3, :], Af, Pb, Aret, tokp, "p1")
        if not totals:
            continue
        for k in range(4):
            fw.dma(D["o_tot"][si, k], Stot[:, k, :])
        ta = smallp.get()
        fw.copy("dve", ta[:, 0:20], Af[:])
        fw.copy("dve", ta[:, 20:40], Pb[:])
        fw.dma(D["o_totA"][si], ta[:, 0:40])


def emit_rope(fw, src, cs, out, tok5, scale):
    s3 = src.re("p (h d) -> p h d", h=4)
    o3 = out[:].re("p (h d) -> p h d", h=4)
    cos = cs[:, 0:64].unsq(1).bc([128, 4, 64])
    sin = cs[:, 64:128].unsq(1).bc([128, 4, 64])
    t1 = tok5.get(); t2 = tok5.get()
    a = t1[:, 0:256].re("p (h d) -> p h d", h=4)
    b = t1[:, 256:512].re("p (h d) -> p h d", h=4)
    c = t2[:, 0:256].re("p (h d) -> p h d", h=4)
    d = t2[:, 256:512].re("p (h d) -> p h d", h=4)
    fw.tt("dve", a, s3[:, :, 0:64], cos, ALU.mult)
    fw.tt("dve", b, s3[:, :, 64:128], sin, ALU.mult)
    fw.tt("dve", c, s3[:, :, 0:64], sin, ALU.mult)
    fw.tt("dve", d, s3[:, :, 64:128], cos, ALU.mult)
    fw.tt("pool", o3[:, :, 0:64], a, b, ALU.subtract)
    fw.tt("pool", o3[:, :, 64:128], c, d, ALU.add)


def emit_rope_q(fw, K, C, qs, cs_d, lc, rsc, qk, tok5):
    cs = tok5.get()
    fw.dma(cs[:, 0:128], cs_d[lc])
    qr = tok5.get()
    emit_rope(fw, qs, cs[:, 0:128], qr, tok5, 1.0)
    variants = [qr]
    for v in range(2):
        o = tok5.get()
        fw.tt("dve" if v else "pool", o[:].re("p (h d) -> p h d", h=4), qr[:].re("p (h d) -> p h d", h=4),
              rsc[:, 4 * v:4 * v + 4].unsq(2).bc([128, 4, 128]), ALU.mult)
        variants.append(o)
    for vi, src in enumerate(variants):
        for h in range(4):
            pt = K["ps"].get()
            fw.tr(pt[:, 0:128], src[:, 128 * h:128 * h + 128], C.ident)
            fw.copy("act" if h % 2 else "dve", qk[:, 4 * vi + h, :], pt[:, 0:128])


def emit_rope_k(fw, K, C, ks, cs_d, lc, rsc, qk, tok5, kstp):
    cs = tok5.get()
    fw.dma(cs[:, 0:128], cs_d[lc])
    kr = tok5.get()
    emit_rope(fw, ks, cs[:, 0:128], kr, tok5, 1.0)
    for h in range(4):
        pt = K["ps"].get()
        fw.tr(pt[:, 0:128], kr[:, 128 * h:128 * h + 128], C.ident)
        fw.copy("act" if h % 2 else "dve", qk[:, 12 + h, :], pt[:, 0:128])
    kst = kstp.get()
    for v in range(2):
        fw.tt("dve" if v else "pool", kst[:, 512 * v:512 * v + 512].re("p (h d) -> p h d", h=4),
              kr[:].re("p (h d) -> p h d", h=4), rsc[:, 8 + 4 * v:12 + 4 * v].unsq(2).bc([128, 4, 128]), ALU.mult)
    return kst


def emit_ssd_state(fw, K, C, dl, xs, bm, Sf, Sb, Af, Pb, smallp, tokp, mode):
    dirs = {"p1": (0, 1), "f": (0,), "b": (1,)}[mode]
    for d in dirs:
        la = dl[:, 32 + 16 * d:48 + 16 * d]
        pr = K["ps"].get()
        fw.mm(pr[:, 0:16], C.m_lt if d else C.m_gt, la, True, True)
        fw.mm(pr[:, 16:32], C.ones, la, True, True)
        wv = smallp.get()
        fw.act(wv[:, 0:32], pr[:, 0:32], AF.Exp)
        fw.tt("dve", wv[:, 0:16], wv[:, 0:16], dl[:, 16 * d:16 * d + 16], ALU.mult)
        vs = tokp.get()
        fw.tt("pool", vs[:].re("p (h e) -> p h e", h=16), xs[:].re("p (h e) -> p h e", h=16),
              wv[:, 0:16].unsq(2).bc([128, 16, 64]), ALU.mult)
        S = Sb if d else Sf
        for half in range(2):
            pf = K["ps"].get()
            for gg in range(2):
                g4 = 2 * half + gg
                fw.mm(pf[:, 256 * gg:256 * gg + 256], bm[:, 128 * g4:128 * g4 + 128], vs[:, 256 * g4:256 * g4 + 256], True, True)
            Sh = S[:, 512 * half:512 * half + 512].re("p (h e) -> p h e", h=8)
            Abc = wv[:, 16 + 8 * half:24 + 8 * half].unsq(2).bc([128, 8, 64])
            pf3 = pf[:].re("p (h e) -> p h e", h=8)
            if mode == "p1" and d == 1:
                t = K["t512"].get()
                fw.tt("dve", t[:].re("p (h e) -> p h e", h=8), pf3, Pb[:, 8 * half:8 * half + 8].unsq(2).bc([128, 8, 64]), ALU.mult)
                fw.tt("pool", S[:, 512 * half:512 * half + 512], S[:, 512 * half:512 * half + 512], t[:], ALU.add)
            else:
                fw.tt("dve", Sh, Sh, Abc, ALU.mult)
                fw.tt("dve", Sh, Sh, pf3, ALU.add)
        if mode == "p1":
            if d == 0:
                fw.tt("dve", Af[:, 0:16], Af[:, 0:16], wv[:, 16:32], ALU.mult)
            else:
                fw.tt("dve", Pb[:, 0:16], Pb[:, 0:16], wv[:, 16:32], ALU.mult)


def emit_ret_state(fw, K, kst, v, Sf, Sb, Af, Pb, Aret, tokp, mode):
    dirs = {"p1": (0, 1), "f": (0,), "b": (1,)}[mode]
    for d in dirs:
        S = Sb if d else Sf
        for half in range(2):
            pf = K["ps"].get()
            for hh in range(2):
                h = 2 * half + hh
                fw.mm(pf[:, 256 * hh:256 * hh + 256], kst[:, 512 * d + 128 * h:512 * d + 128 * h + 128],
                      v[:, 256 * h:256 * h + 256], True, True)
            Sh = S[:, 512 * half:512 * half + 512].re("p (h e) -> p h e", h=2)
            pf3 = pf[:].re("p (h e) -> p h e", h=2)
            if mode == "p1" and d == 1:
                t = K["t512"].get()
                fw.tt("dve", t[:].re("p (h e) -> p h e", h=2), pf3, Pb[:, 16 + 2 * half:18 + 2 * half].unsq(2).bc([128, 2, 256]), ALU.mult)
                fw.tt("pool", S[:, 512 * half:512 * half + 512], S[:, 512 * half:512 * half + 512], t[:], ALU.add)
            else:
                fw.tt("dve", Sh, Sh, Aret[:, 4 * d + 2 * half:4 * d + 2 * half + 2].unsq(2).bc([128, 2, 256]), ALU.mult)
                fw.tt("dve", Sh, Sh, pf3, ALU.add)
        if mode == "p1":
            if d == 0:
                fw.tt("dve", Af[:, 16:20], Af[:, 16:20], Aret[:, 0:4], ALU.mult)
            else:
                fw.tt("dve", Pb[:, 16:20], Pb[:, 16:20], Aret[:, 4:8], ALU.mult)


def emit_rstd(fw, out, in_, scale, eps):
    fw.ts("dve", out, in_, scale, ALU.mult, eps, ALU.add)
    fw.act(out, out, AF.Ln)
    fw.act(out, out, AF.Exp, scale=-0.5)


def l0p3_sweeps(fw, D):
    K = {"ps": Pool(fw, [128, 512], 4, psum=True, name="ps"), "t512": Pool(fw, [128, 512], 4, name="t512")}
    K["c"] = C = Consts(fw, D["consts"])
    py = [fw.ps([128, 512], "py") for _ in range(2)]
    pr = [fw.ps([128, 512], "pr") for _ in range(2)]
    sm = fw.sb([128, 72], "small"); fw.dma(sm[:], D["small"][:])
    dsk = fw.sb([128, 16], "dskip"); fw.dma(dsk[:], D["dskip"][:])
    nws = fw.sb([128, 1024], "nws"); fw.dma(nws[:], D["nws"][:])
    diff = fw.sb([128, 2, 128], "diff"); fw.dma(diff[:], D["diff"][:])
    lg = fw.sb([128, 8], "lg")
    fw.act(lg[:], sm[:, 64:72], AF.Exp, scale=-1.0)
    fw.act(lg[:], lg[:], AF.Ln, bias=1.0)
    fw.ts("dve", lg[:], lg[:], -1.0, ALU.mult)
    Aret = fw.sb([128, 8], "Aret")
    fw.act(Aret[:], lg[:], AF.Exp, scale=128.0)
    DT = fw.sb([128, 8, 128], "DT")
    for d in range(2):
        for h in range(4):
            fw.ts("dve", DT[:, 4 * d + h, :], diff[:, d, :], lg[:, 4 * d + h:4 * d + h + 1], ALU.mult)
            fw.act(DT[:, 4 * d + h, :], DT[:, 4 * d + h, :], AF.Exp)
            fw.tt("dve", DT[:, 4 * d + h, :], DT[:, 4 * d + h, :], C.m_ge if d else C.m_le, ALU.mult)
    preA = fw.sb([128, 2, 5, 40], "preA"); fw.dma(preA[:], D["pre_A"][:])
    S = fw.sb([128, 4, 1024], "S")
    tokp = Pool(fw, [128, 1024], 7, name="tok")
    t128 = Pool(fw, [128, 128], 8, name="t128")
    smallp = Pool(fw, [128, 64], 8, name="sm")
    ld = {n: Pool(fw, sh, 2, name="ld_" + n) for n, sh in
          (("cb", [128, 8, 128]), ("dl", [128, 64]), ("xs", [128, 1024]), ("bm", [128, 512]),
           ("qk", [128, 16, 128]), ("v", [128, 1024]), ("kst", [128, 1024]))}
    yfp = Pool(fw, [128, 2048], 1, name="yf")
    SCb = fw.sb([128, 4, 128], "SC")
    ycp = Pool(fw, [128, 16, 128], 2, name="ycT")
    h16 = lambda v: v.re("p (h e) -> p h e", h=16)
    h4 = lambda v: v.re("p (h e) -> p h e", h=4)

    for si, (ch0, nch, ncol) in enumerate(SEGS):
        fw.memset("pool", S[:], 0.0)
        for e in range(5):
            for k in range(4):
                Fb = tokp.get()
                fw.dma(Fb[:], D["pre_F"][si, e, k])
                if k < 2:
                    A = preA[:, si, e, 20 * k:20 * k + 16].unsq(2).bc([128, 16, 64])
                    Sv = h16(S[:, k, :])
                else:
                    A = preA[:, si, e, 20 * (k - 2) + 16:20 * (k - 2) + 20].unsq(2).bc([128, 4, 256])
                    Sv = h4(S[:, k, :])
                fw.tt("dve", Sv, Sv, A, ALU.mult)
                fw.tt("pool", S[:, k, :], S[:, k, :], Fb[:], ALU.add)
        for d in range(2):
            order = range(nch) if d == 0 else range(nch - 1, -1, -1)
            for lc in order:
                ch = ch0 + lc
                t = {}
                for n, src in (("cb", "o_cb"), ("dl", "o_dl"), ("xs", "o_xs"), ("bm", "o_bm"),
                               ("qk", "o_qk"), ("v", "o_v"), ("kst", "o_kst")):
                    t[n] = ld[n].get()
                    fw.dma(t[n][:], D[src][ch])
                cbm, dl, xs, bm, qk, v, kst = (t[n] for n in ("cb", "dl", "xs", "bm", "qk", "v", "kst"))
                for g4 in range(4):
                    p = K["ps"].get()
                    fw.mm(p[:, 0:128], cbm[:, g4, :], cbm[:, 4 + g4, :])
                    fw.tt("dve", SCb[:, g4, :], p[:, 0:128], C.m_ge if d else C.m_le, ALU.mult)
                vdt = tokp.get()
                fw.tt("pool", h16(vdt[:]), h16(xs[:]), dl[:, 16 * d:16 * d + 16].unsq(2).bc([128, 16, 64]), ALU.mult)
                la = dl[:, 32 + 16 * d:48 + 16 * d]
                for h in range(16):
                    Lm = t128.get()
                    fw.ts("pool" if h % 2 else "dve", Lm[:], C.m_lt if d else C.m_gt, la[:, h:h + 1], ALU.mult)
                    pd = K["ps"].get()
                    fw.mm(pd[:, 0:128], Lm[:], C.m_ge if d else C.m_le)
                    E = t128.get()
                    fw.act(E[:], pd[:, 0:128], AF.Exp)
                    P = t128.get()
                    fw.tt("dve" if h % 2 else "pool", P[:], E[:], SCb[:, h // 4, :], ALU.mult)
                    fw.mm(py[h // 8][:, 64 * (h % 8):64 * (h % 8) + 64], P[:], vdt[:, 64 * h:64 * h + 64])
                pu = [K["ps"].get(), K["ps"].get()]
                for g4 in range(4):
                    fw.mm(pu[g4 // 2][:, 256 * (g4 % 2):256 * (g4 % 2) + 256], cbm[:, 4 + g4, :], S[:, d, 256 * g4:256 * g4 + 256])
                pc = K["ps"].get()
                fw.mm(pc[:, 0:16], C.m_ge if d else C.m_le, la)
                ecs = smallp.get()
                fw.act(ecs[:, 0:16], pc[:, 0:16], AF.Exp)
                y = tokp.get()
                for half in range(2):
                    tq = K["t512"].get()
                    fw.tt("dve", tq[:].re("p (h e) -> p h e", h=8), pu[half][:].re("p (h e) -> p h e", h=8),
                          ecs[:, 8 * half:8 * half + 8].unsq(2).bc([128, 8, 64]), ALU.mult)
                    fw.tt("dve", y[:, 512 * half:512 * half + 512], tq[:], py[half][:], ALU.add)
                emit_ssd_state(fw, K, C, dl, xs, bm, S[:, 0, :], S[:, 1, :], None, None, smallp, tokp, "b" if d else "f")
                for h in range(4):
                    p = K["ps"].get()
                    fw.mm(p[:, 0:128], qk[:, 12 + h, :], qk[:, h, :])
                    P = t128.get()
                    fw.tt("dve", P[:], p[:, 0:128], DT[:, 4 * d + h, :], ALU.mult)
                    o = pr[h // 2][:, 256 * (h % 2):256 * (h % 2) + 256]
                    fw.mm(o, P[:], v[:, 256 * h:256 * h + 256], True, False)
                    fw.mm(o, qk[:, 4 * (1 + d) + h, :], S[:, 2 + d, 256 * h:256 * h + 256], False, True)
                yr = tokp.get()
                fw.copy("act", yr[:, 0:512], pr[0][:])
                fw.copy("act", yr[:, 512:1024], pr[1][:])
                emit_ret_state(fw, K, kst, v[:], S[:, 2, :], S[:, 3, :], None, None, Aret, tokp, "b" if d else "f")
                if d == 0:
                    fw.dma(D["yf"][ch, :, 0:1024], y[:])
                    fw.dma(D["yf"][ch, :, 1024:2048], yr[:])
                    continue
                yf = yfp.get()
                fw.dma(yf[:], D["yf"][ch])
                sz = tokp.get(); fw.dma(sz[:], D["o_z"][ch])
                sg = tokp.get(); fw.dma(sg[:], D["o_g"][ch])
                fw.tt("dve", y[:], y[:], yf[:, 0:1024], ALU.add)
                tq = tokp.get()
                fw.tt("pool", h16(tq[:]), h16(xs[:]), dsk[:].unsq(2).bc([128, 16, 64]), ALU.mult)
                fw.tt("pool", y[:], y[:], tq[:], ALU.add)
                fw.tt("dve", y[:], y[:], sz[:], ALU.mult)
                fw.act(tq[:], y[:], AF.Square)
                st_ = smallp.get()
                fw.rsum("dve", st_[:, 0:4], h4(tq[:]))
                emit_rstd(fw, st_[:, 0:4], st_[:, 0:4], 1.0 / 256, RMS_EPS)
                fw.tt("dve", h4(y[:]), h4(y[:]), st_[:, 0:4].unsq(2).bc([128, 4, 256]), ALU.mult)
                fw.tt("pool", y[:], y[:], nws[:], ALU.mult)
                fw.tt("dve", yr[:], yr[:], yf[:, 1024:2048], ALU.add)
                fw.rsum("dve", st_[:, 8:12], h4(yr[:]))
                fw.ts("dve", st_[:, 8:12], st_[:, 8:12], -1.0 / 256, ALU.mult)
                fw.tt("dve", h4(yr[:]), h4(yr[:]), st_[:, 8:12].unsq(2).bc([128, 4, 256]), ALU.add)
                fw.act(tq[:], yr[:], AF.Square)
                fw.rsum("dve", st_[:, 12:16], h4(tq[:]))
                emit_rstd(fw, st_[:, 12:16], st_[:, 12:16], 1.0 / 256, LN_EPS)
                fw.tt("dve", h4(yr[:]), h4(yr[:]), st_[:, 12:16].unsq(2).bc([128, 4, 256]), ALU.mult)
                fw.tt("pool", yr[:], yr[:], sg[:], ALU.mult)
                ycT = ycp.get()
                for kt in range(16):
                    src = y if kt < 8 else yr
                    pt = K["ps"].get()
                    fw.tr(pt[:, 0:128], src[:, 128 * (kt % 8):128 * (kt % 8) + 128], C.ident)
                    fw.copy("act" if kt % 2 else "dve", ycT[:, kt, :], pt[:, 0:128])
                fw.dma(D["ycT"][:, :, 128 * ch:128 * ch + 128], ycT[:])


def emit_tail(fw, D, nk_in, last):
    K = {"ps": Pool(fw, [128, 512], 8, psum=True, name="ps"), "t512": Pool(fw, [128, 512], 4, name="t512")}
    K["c"] = Consts(fw, D["consts"])
    mod = fw.sb([128, 48, 2], "mod"); fw.dma(mod[:], D["mod_out"][:])
    gsf = emit_gs(fw, mod, D["nwf"], 32, "gsf")
    shf = mod[:, 24:32, :]
    if last:
        gfin = fw.sb([128, 8, 1], "gfin"); fw.dma(gfin[:, :, 0], D["nwfin"][:])
    ycat = fw.sb([128, nk_in, 512], "ycat", r32=True)
    x1 = fw.sb([128, 8, 512], "x1")
    h2 = fw.sb([128, 8, 512], "h2", r32=True)
    x2 = fw.sb([128, 8, 512], "x2")
    uT = fw.sb([128, 22, 512], "uT", r32=True)
    wop = Pool(fw, [128, nk_in, 128], 1, name="wo", r32=True)
    w13 = Pool(fw, [128, 8, 128], 4, name="w13", r32=True)
    w2p = Pool(fw, [128, 22, 128], 1, name="w2", r32=True)
    for si, (ch0, nch, ncol) in enumerate(SEGS):
        if last and si == 1:
            continue
        for c0 in range(0, nch * 128, 512):
            W = min(512, nch * 128 - c0)
            fw.dma(ycat[:, :, 0:W], D["ycT"][:, :, 128 * ch0 + c0:128 * ch0 + c0 + W])
            fw.dma(x1[:, :, 0:W], D["xT%d" % si][:, :, 2 + c0:2 + c0 + W])
            for ft in range(8):
                w = wop.get()
                fw.dma(w[:], D["w_out"][ft])
                p = K["ps"].get()
                for kt in range(nk_in):
                    fw.mm(p[:, 0:W], w[:, kt, :], ycat[:, kt, 0:W], kt == 0, kt == nk_in - 1)
                fw.stt("dve", x1[:, ft, 0:W], p[:, 0:W], mod[:, 16 + ft, si:si + 1], x1[:, ft, 0:W], ALU.mult, ALU.add)
            emit_norm(fw, K, x1, h2, 0, W, gsf, shf, si)
            for ft in range(22):
                wa = w13.get(); fw.dma(wa[:], D["w1"][ft])
                wb = w13.get(); fw.dma(wb[:], D["w3"][ft])
                pa = K["ps"].get(); pb = K["ps"].get()
                for kt in range(8):
                    fw.mm(pa[:, 0:W], wa[:, kt, :], h2[:, kt, 0:W], kt == 0, kt == 7)
                for kt in range(8):
                    fw.mm(pb[:, 0:W], wb[:, kt, :], h2[:, kt, 0:W], kt == 0, kt == 7)
                sa = K["t512"].get()
                fw.act(sa[:, 0:W], pa[:, 0:W], AF.Silu)
                fw.tt("dve", uT[:, ft, 0:W], sa[:, 0:W], pb[:, 0:W], ALU.mult)
            for f2 in range(8):
                w = w2p.get()
                fw.dma(w[:], D["w2"][f2])
                p = K["ps"].get()
                for ft in range(22):
                    fw.mm(p[:, 0:W], w[:, ft, :], uT[:, ft, 0:W], ft == 0, ft == 21)
                fw.stt("dve", x2[:, f2, 0:W], p[:, 0:W], mod[:, 40 + f2, si:si + 1], x1[:, f2, 0:W], ALU.mult, ALU.add)
            if last:
                emit_norm(fw, K, x2, x1, 0, W, gfin, None, 0)
                fw.dma(D["xo%d" % si][:, :, c0:c0 + W], x1[:, :, 0:W])
            else:
                fw.dma(D["xo%d" % si][:, :, c0:c0 + W], x2[:, :, 0:W])


def fm(x2d):
    T, F = x2d.shape
    return np.ascontiguousarray(x2d.T.reshape(F // 128, 128, T).transpose(1, 0, 2))


def wblk(W, bw):
    K_, N = W.shape
    return np.ascontiguousarray(W.reshape(K_ // 128, 128, N // bw, bw).transpose(2, 1, 0, 3))


def colvec(v):
    return np.ascontiguousarray(v.reshape(-1, 128).T)


def rep(v):
    v = np.asarray(v, np.float32).reshape(-1)
    return np.ascontiguousarray(np.broadcast_to(v[None, :], (128, v.size)))


def run_prog(phases, ins, outs, scratch, in_maps):
    nc = bass.Bass("TRN2", target_bir_lowering=False)
    with ExitStack() as top:
        fw = FW(nc, top)
        D = {}
        for name, shape in ins.items():
            D[name] = fw.dram(name, shape, kind="ExternalInput")
        for name, shape in outs.items():
            D[name] = fw.dram(name, shape, kind="ExternalOutput")
        for name, shape in scratch.items():
            D[name] = fw.dram(name, shape, kind="Internal")
        for ph in phases:
            with ExitStack() as st:
                fw.stack = st
                ph(fw, D)
                fw.flush()
    maps = [{k: np.ascontiguousarray(m[k], dtype=np.float32) for k in ins} for m in in_maps]
    for m in maps:
        for k, shape in ins.items():
            assert tuple(m[k].shape) == tuple(shape), (k, m[k].shape, shape)
    res = run_bass_kernel_spmd(nc, maps, core_ids=list(range(NCORE)))
    return res.results


def seg_inputs(lat, cx):
    out = []
    for r in range(NCORE):
        b, s = divmod(r, 4)
        x0 = np.zeros((2052, 1024), np.float32)
        lo, hi = s * 2048 - 2, s * 2048 + 2050
        a, e = max(lo, 0), min(hi, 8192)
        x0[a - lo:e - lo] = lat[b, a:e]
        x1 = np.zeros((132, 1024), np.float32)
        hm = np.zeros(4, np.float32)
        hm[0] = float(s > 0)
        hm[1] = float(s < 3)
        if s < 2:
            lo, hi = s * 128 - 2, s * 128 + 130
            a, e = max(lo, 0), min(hi, 256)
            x1[a - lo:e - lo] = cx[b, a:e]
            hm[2] = float(s > 0)
            hm[3] = float(s < 1)
        out.append({"xT0": fm(x0), "xT1": fm(x1), "hm": rep(hm)})
    return out


def rope_tables():
    r = np.repeat(np.arange(128, dtype=np.float32), 64)
    col = np.tile(np.arange(64, dtype=np.float32), 128)
    nf = 32
    inv = (np.float32(10000.0) ** (-np.arange(nf, dtype=np.float32) / nf)).astype(np.float32)
    ang = np.concatenate([r[:, None] * inv, col[:, None] * inv], -1).astype(np.float32)
    return np.concatenate([np.cos(ang), np.sin(ang)], -1).astype(np.float32)


def prefix_lists(tot, totA, nk, a_half):
    outF, outA = [], []
    for r in range(NCORE):
        b, s = divmod(r, 4)
        base = 4 * b
        F = np.zeros((2, 5) + tot[0].shape[1:], np.float32)
        A = np.ones((128, 2, 5, totA[0].shape[-1]), np.float32)
        fwd0 = [(base + 0, 1), (base + 1, 1)] + [(base + k, 0) for k in range(3)]
        use_f0 = [True, True] + [k < s for k in range(3)]
        bwd0 = [(base + 1, 1), (base + 0, 1)] + [(base + k, 0) for k in (3, 2, 1)]
        use_b0 = [True, True] + [k > s for k in (3, 2, 1)]
        fwd1 = [(base + 0, 1)]
        use_f1 = [s == 1]
        bwd1 = [(base + 1, 1)]
        use_b1 = [s == 0]
        for si, (fl, uf, bl, ub) in enumerate(((fwd0, use_f0, bwd0, use_b0), (fwd1, use_f1, bwd1, use_b1))):
            if si == 1 and s >= 2:
                continue
            for e, ((cr, cs_), u) in enumerate(zip(fl, uf)):
                if u:
                    for k in FWD_KINDS[nk]:
                        F[si, e, k] = tot[cr][cs_, k]
                    A[:, si, e, 0:a_half] = totA[cr][cs_][:, 0:a_half]
            for e, ((cr, cs_), u) in enumerate(zip(bl, ub)):
                if u:
                    for k in BWD_KINDS[nk]:
                        F[si, e, k] = tot[cr][cs_, k]
                    A[:, si, e, a_half:2 * a_half] = totA[cr][cs_][:, a_half:2 * a_half]
        outF.append(F)
        outA.append(A)
    return outF, outA


FWD_KINDS = {4: (0, 2)}
BWD_KINDS = {4: (1, 3)}

SCR0 = {"o_z": [NCH, 128, 1024], "o_g": [NCH, 128, 1024], "o_v": [NCH, 128, 1024], "o_qk": [NCH, 128, 16, 128],
        "o_kst": [NCH, 128, 1024], "o_dl": [NCH, 128, 64], "o_cb": [NCH, 128, 8, 128], "o_xs": [NCH, 128, 1024],
        "o_bm": [NCH, 128, 512]}


def layer0(inp, lat, cx, mods_only=False):
    consts = host_consts()
    j = np.arange(128, dtype=np.float32)
    iota = np.stack([j, 127 - j], 1).astype(np.float32)
    di = np.arange(128)[None, :] - np.arange(128)[:, None]
    diff = np.ascontiguousarray(np.stack([np.maximum(di, 0), np.maximum(-di, 0)], 1).astype(np.float32))
    W = inp["ab_in_w"][0]
    w_xbc = wblk(W[:, 1024:3072], 128)
    w_dt = np.ascontiguousarray(wblk(W[:, 3072:3088], 16)[0])
    w_tm = wblk(np.concatenate([W[:, 0:1024], W[:, 3088:]], 1), 256)
    cwl = np.ascontiguousarray(inp["ab_conv_w"][0].T.reshape(16, 128, 5).transpose(1, 0, 2))
    small = np.concatenate([rep(inp["ssd_dt_bias_f"][0]), rep(inp["ssd_dt_bias_b"][0]), rep(inp["ssd_a_log_f"][0]),
                            rep(inp["ssd_a_log_b"][0]), rep(inp["ret_logit_f"][0]), rep(inp["ret_logit_b"][0])], 1)
    rt = rope_tables()
    ones_cs = np.concatenate([np.ones((128, 64), np.float32), np.zeros((128, 64), np.float32)], 1)[None]
    segs = seg_inputs(lat, cx)
    shared = {"consts": consts, "adaw": wblk(inp["ada_w"][0], 128), "adab": colvec(inp["ada_b"][0]),
              "nw": colvec(inp["norm_mix_w"][0]), "cw": cwl, "cb": colvec(inp["ab_conv_b"][0]), "w_dt": w_dt,
              "small": small, "iota": iota, "w_xbc": w_xbc, "w_tm": w_tm, "cs1": ones_cs}
    maps = []
    for r in range(NCORE):
        b, s = divmod(r, 4)
        m = dict(shared)
        m.update(segs[r])
        m["cvec"] = np.ascontiguousarray(np.stack([colvec(inp["c"][b]), colvec(inp["c_ctx"])], -1))
        m["cs0"] = np.ascontiguousarray(rt[s * 2048:(s + 1) * 2048].reshape(16, 128, 128))
        maps.append(m)
    ins = {k: list(v.shape) for k, v in maps[0].items()}
    tot_shapes = {"mod_out": [128, 48, 2], "o_tot": [2, 4, 128, 1024], "o_totA": [2, 128, 40]}
    res1 = run_prog([l0p1], ins, dict(tot_shapes), dict(SCR0), maps)
    preF, preA = prefix_lists([r_["o_tot"] for r_ in res1], [r_["o_totA"] for r_ in res1], 4, 20)
    shared3 = {"dskip": rep(inp["ssd_d"][0]), "nws": rep(inp["ssd_norm_w"][0]),
               "diff": diff, "nwf": colvec(inp["norm_ffn_w"][0]), "w_out": wblk(inp["ab_out_w"][0], 128),
               "w1": wblk(inp["ffn_w1"][0], 128), "w3": wblk(inp["ffn_w3"][0], 128), "w2": wblk(inp["ffn_w2"][0], 128)}
    maps3 = []
    for r in range(NCORE):
        m = dict(maps[r])
        m.update(shared3)
        m["pre_F"] = preF[r]
        m["pre_A"] = preA[r]
        maps3.append(m)
    ins3 = {k: list(v.shape) for k, v in maps3[0].items()}
    outs3 = {"xo0": [128, 8, 2048], "xo1": [128, 8, 128]}
    scr3 = {"yf": [NCH, 128, 2048], "ycT": [128, 16, NCH * 128]}
    scr3.update(SCR0)
    scr3.update(tot_shapes)
    res3 = run_prog([lambda fw, D: l0p1(fw, D, False), l0p3_sweeps, lambda fw, D: emit_tail(fw, D, 16, False)], ins3, outs3, scr3, maps3)
    lat1 = np.zeros_like(lat)
    cx1 = np.zeros_like(cx)
    for r in range(NCORE):
        b, s = divmod(r, 4)
        lat1[b, s * 2048:(s + 1) * 2048] = res3[r]["xo0"].transpose(2, 1, 0).reshape(2048, 1024)
        if s < 2:
            cx1[b, s * 128:(s + 1) * 128] = res3[r]["xo1"].transpose(2, 1, 0).reshape(128, 1024)
    return lat1, cx1


MS = float(512.0 ** -0.5)
SCR1 = {"o_qT": [NCH, 128, 16, 128], "o_kT": [NCH, 128, 16, 128], "o_ktok": [NCH, 128, 2048],
        "o_vtok": [NCH, 128, 2048], "o_gsc": [NCH, 128, 32], "o_xc": [NCH, 128, 16, 128], "o_sz": [NCH, 128, 16, 128]}


def l1p1a(fw, D):
    K = {"ps": Pool(fw, [128, 512], 8, psum=True, name="ps"), "t512": Pool(fw, [128, 512], 4, name="t512")}
    K["c"] = C = Consts(fw, D["consts"])
    mod = emit_mods(fw, K, D["cvec"], D["adaw"], D["adab"], None, None, D["mod_out"])
    gs = emit_gs(fw, mod, D["nw"], 8, "gs")
    sh = mod[:, 0:8, :]
    hm = fw.sb([128, 4], "hm"); fw.dma(hm[:], D["hm"][:])
    cw = fw.sb([128, 16, 5], "cw"); fw.dma(cw[:], D["cw"][:])
    cb = fw.sb([128, 16], "cb"); fw.dma(cb[:], D["cb"][:])
    wbd = fw.sb([128, 3, 16, 128], "wbd"); fw.dma(wbd[:], D["wbd"][:])
    gw = fw.sb([128, 48, 16], "gw"); fw.dma(gw[:], D["gw"][:])
    gb = fw.sb([128, 16], "gb"); fw.dma(gb[:], D["gb"][:])
    xg = fw.sb([128, 8, 128 * GC1 + 4], "xg")
    hT = fw.sb([128, 8, 128 * GC1 + 4], "hT", r32=True)
    xmT_g = fw.sb([128, 16, 128 * GC1], "xmT")
    xcT_g = fw.sb([128, 16, 128 * GC1], "xcT")
    szT_g = fw.sb([128, 16, 128 * GC1], "szT")
    wup = Pool(fw, [128, 8, 128], 3, name="wup", r32=True)
    cacc = Pool(fw, [128, 128], 3, name="cacc")
    qkvp = Pool(fw, [128, 16, 384], 1, name="qkv")
    kTsp = Pool(fw, [128, 16, 128], 2, name="kTs")
    tk = Pool(fw, [128, 2048], 3, name="tk")
    smallp = Pool(fw, [128, 32], 10, name="sm")
    for si, (ch0, nch, ncol) in enumerate(SEGS):
        xT_d = D["xT%d" % si]
        ngrp = (nch + GC1 - 1) // GC1
        for g in range(ngrp):
          gch = min(GC1, nch - GC1 * g)
          W = 128 * gch
          c0 = 128 * GC1 * g
          fw.dma(xg[:, :, 0:W + 4], xT_d[:, :, c0:c0 + W + 4])
          emit_norm(fw, K, xg, hT, 0, W + 4, gs, sh, si)
          if g == 0:
              fw.ts("dve", hT[:, :, 0:2], hT[:, :, 0:2], hm[:, 2 * si:2 * si + 1], ALU.mult)
          if g == ngrp - 1:
              fw.ts("dve", hT[:, :, W + 2:W + 4], hT[:, :, W + 2:W + 4], hm[:, 2 * si + 1:2 * si + 2], ALU.mult)
          for ft in range(16):
              w = wup.get()
              fw.dma(w[:], D["w_xm"][ft])
              p = K["ps"].get()
              for kt in range(8):
                  fw.mm(p[:, 0:W + 4], w[:, kt, :], hT[:, kt, 0:W + 4], kt == 0, kt == 7)
              fw.copy("act", xmT_g[:, ft, 0:W], p[:, 2:W + 2])
              for j in range(gch):
                  acc = cacc.get()
                  fw.ts("dve", acc[:], p[:, 128 * j:128 * j + 128], cw[:, ft, 0:1], ALU.mult)
                  for k in range(1, 5):
                      fw.stt("dve", acc[:], p[:, 128 * j + k:128 * j + k + 128], cw[:, ft, k:k + 1], acc[:], ALU.mult, ALU.add)
                  fw.act(xcT_g[:, ft, 128 * j:128 * j + 128], acc[:], AF.Silu, bias=cb[:, ft:ft + 1])
          for ft in range(16):
              w = wup.get()
              fw.dma(w[:], D["w_z"][ft])
              p = K["ps"].get()
              for kt in range(8):
                  fw.mm(p[:, 0:W], w[:, kt, :], hT[:, kt, 2:W + 2], kt == 0, kt == 7)
              fw.act(szT_g[:, ft, 0:W], p[:, 0:W], AF.Silu)
          for j in range(gch):
            ch = ch0 + GC1 * g + j
            xmT = xmT_g[:, :, 128 * j:128 * j + 128]
            xcT = xcT_g[:, :, 128 * j:128 * j + 128]
            szT = szT_g[:, :, 128 * j:128 * j + 128]
            fw.dma(D["o_xc"][ch], xcT)
            fw.dma(D["o_sz"][ch], szT)
            qkv = qkvp.get()
            for ft in range(16):
                p = K["ps"].get()
                fw.mm(p[:, 0:128], wbd[:, 0, ft, :], xcT[:, ft, :])
                fw.mm(p[:, 128:256], wbd[:, 1, ft, :], xcT[:, ft, :])
                fw.mm(p[:, 256:384], wbd[:, 2, ft, :], xmT[:, ft, :])
                fw.copy("act" if ft % 2 else "dve", qkv[:, ft, :], p[:, 0:384])
            kTs = kTsp.get()
            fw.act(kTs[:], qkv[:, :, 128:256], AF.Copy, scale=MS)
            fw.dma(D["o_qT"][ch], qkv[:, :, 0:128])
            fw.dma(D["o_kT"][ch], kTs[:])
            ktok = tk.get(); vtok = tk.get()
            for g4 in range(4):
                pk = K["ps"].get(); pv = K["ps"].get()
                for ff in range(4):
                    ft = 4 * g4 + ff
                    fw.mm(pk[:, 128 * ff:128 * ff + 128], xcT[:, ft, :], wbd[:, 1, ft, :])
                    fw.mm(pv[:, 128 * ff:128 * ff + 128], xmT[:, ft, :], wbd[:, 2, ft, :])
                fw.act(ktok[:, 512 * g4:512 * g4 + 512], pk[:], AF.Copy, scale=MS)
                fw.copy("dve", vtok[:, 512 * g4:512 * g4 + 512], pv[:])
            fw.dma(D["o_ktok"][ch], ktok[:])
            fw.dma(D["o_vtok"][ch], vtok[:])
            pg = K["ps"].get()
            for idx in range(48):
                t_, ft = divmod(idx, 16)
                fw.mm(pg[:, 0:16], qkv[:, ft, 128 * t_:128 * t_ + 128], gw[:, idx, :], idx == 0, idx == 47)
            gt = smallp.get()
            fw.tt("dve", gt[:, 0:16], pg[:, 0:16], gb[:], ALU.add)
            lf = smallp.get()
            fw.copy("dve", lf[:, 0:4], gt[:, 4:8])
            fw.copy("dve", lf[:, 4:8], gt[:, 12:16])
            fw.act(lf[:, 0:8], lf[:, 0:8], AF.Exp, scale=-1.0)
            fw.act(lf[:, 0:8], lf[:, 0:8], AF.Ln, bias=1.0)
            fw.ts("dve", lf[:, 0:8], lf[:, 0:8], -1.0, ALU.mult)
            pf = K["ps"].get()
            fw.mm(pf[:, 0:4], C.m_le, lf[:, 0:4])
            fw.mm(pf[:, 4:8], C.m_ge, lf[:, 4:8])
            fw.mm(pf[:, 8:16], C.ones, lf[:, 0:8])
            gsc = smallp.get()
            fw.copy("dve", gsc[:, 8:16], pf[:, 0:8])
            fw.copy("dve", gsc[:, 24:32], pf[:, 8:16])
            fw.tt("dve", gsc[:, 0:4], gt[:, 0:4], gsc[:, 8:12], ALU.subtract)
            fw.tt("dve", gsc[:, 4:8], gt[:, 8:12], gsc[:, 12:16], ALU.subtract)
            emit_colmax(fw, K, C, gsc[:, 0:8], gsc[:, 16:24], smallp)
            fw.dma(D["o_gsc"][ch], gsc[:])


def emit_colmax(fw, K, C, a, out, smallp):
    pt = K["ps"].get()
    fw.tr(pt[0:8, 0:128], a, C.ident)
    am = smallp.get()
    fw.rmax("dve", am[0:8, 0:1], pt[0:8, 0:128])
    d8 = smallp.get()
    fw.ts("dve", d8[0:8, 0:8], C.ident[0:8, 0:8], am[0:8, 0:1], ALU.mult)
    pb = K["ps"].get()
    fw.mm(pb[:, 0:8], C.ones[0:8, :], d8[0:8, 0:8])
    fw.copy("dve", out, pb[:, 0:8])


def emit_mstate(fw, K, C, ktok, vtok, u, Cst, nst, h, ucol, kpp, pn):
    kp = kpp.get()
    fw.ts("dve", kp[:], ktok[:, 512 * h:512 * h + 512], u[:, ucol:ucol + 1], ALU.mult)
    for dt in range(4):
        pF = K["ps"].get()
        fw.mm(pF[:], kp[:, 128 * dt:128 * dt + 128], vtok[:, 512 * h:512 * h + 512])
        fw.mm(pn[:, dt:dt + 1], kp[:, 128 * dt:128 * dt + 128], C.ones[:, 0:1])
        fw.tt("dve", Cst[:, h, dt, :], Cst[:, h, dt, :], pF[:], ALU.add)
    fw.tt("dve", nst[:, h, :], nst[:, h, :], pn[:, 0:4], ALU.add)


def l1p1b(fw, D):
    K = {"ps": Pool(fw, [128, 512], 6, psum=True, name="ps")}
    K["c"] = C = Consts(fw, D["consts"])
    pn = fw.ps([128, 512], "pn")
    Cst = [fw.sb([128, 4, 4, 512], "Ctot%d" % d) for d in range(2)]
    nst = [fw.sb([128, 4, 4], "ntot%d" % d) for d in range(2)]
    sc = fw.sb([128, 16], "msc")
    tk = Pool(fw, [128, 2048], 4, name="tk", r32=True)
    kpp = Pool(fw, [128, 512], 3, name="kp", r32=True)
    smallp = Pool(fw, [128, 32], 10, name="sm")
    for si, (ch0, nch, ncol) in enumerate(SEGS):
        for d in range(2):
            fw.memset("pool", Cst[d][:], 0.0)
            fw.memset("pool", nst[d][:], 0.0)
        fw.memset("pool", sc[:], 0.0)
        for lc in range(nch):
            ch = ch0 + lc
            ktok = tk.get(); fw.dma(ktok[:], D["o_ktok"][ch])
            vtok = tk.get(); fw.dma(vtok[:], D["o_vtok"][ch])
            gsc = smallp.get(); fw.dma(gsc[:], D["o_gsc"][ch])
            w = smallp.get()
            fw.tt("dve", w[:, 0:4], sc[:, 0:4], gsc[:, 16:20], ALU.max)
            fw.tt("dve", w[:, 8:12], sc[:, 0:4], w[:, 0:4], ALU.subtract)
            fw.tt("dve", w[:, 16:20], gsc[:, 16:20], w[:, 0:4], ALU.subtract)
            fw.tt("dve", w[:, 24:28], sc[:, 12:16], gsc[:, 20:24], ALU.add)
            fw.tt("dve", w[:, 24:28], w[:, 24:28], gsc[:, 28:32], ALU.add)
            fw.tt("dve", w[:, 4:8], sc[:, 4:8], w[:, 24:28], ALU.max)
            fw.tt("dve", w[:, 12:16], sc[:, 4:8], w[:, 4:8], ALU.subtract)
            fw.tt("dve", w[:, 20:24], w[:, 24:28], w[:, 4:8], ALU.subtract)
            fw.act(w[:, 8:24], w[:, 8:24], AF.Exp)
            fw.tt("dve", sc[:, 0:4], gsc[:, 24:28], w[:, 0:4], ALU.add)
            fw.copy("dve", sc[:, 4:8], w[:, 4:8])
            fw.tt("dve", sc[:, 8:12], sc[:, 8:12], gsc[:, 24:28], ALU.add)
            fw.tt("dve", sc[:, 12:16], sc[:, 12:16], gsc[:, 28:32], ALU.add)
            u = smallp.get()
            fw.tt("dve", u[:, 0:8], gsc[:, 0:8], gsc[:, 16:24], ALU.subtract)
            fw.act(u[:, 0:8], u[:, 0:8], AF.Exp)
            fw.tt("dve", u[:, 0:8], u[:, 0:8], w[:, 16:24], ALU.mult)
            for d in range(2):
                for h in range(4):
                    fw.ts("pool", Cst[d][:, h].re("p a b -> p (a b)"), Cst[d][:, h].re("p a b -> p (a b)"),
                          w[:, 8 + 4 * d + h:9 + 4 * d + h], ALU.mult)
                    fw.ts("dve", nst[d][:, h, :], nst[d][:, h, :], w[:, 8 + 4 * d + h:9 + 4 * d + h], ALU.mult)
                    emit_mstate(fw, K, C, ktok, vtok, u, Cst[d], nst[d], h, 4 * d + h, kpp, pn)
        for d in range(2):
            for h in range(4):
                fw.dma(D["o_totC"][si, d, h], Cst[d][:, h].re("p a b -> p (a b)"))
            fw.dma(D["o_totn"][si, d], nst[d][:].re("p a b -> p (a b)"))
        fw.dma(D["o_totm"][si], sc[:])


def l1p3_sweeps(fw, D):
    K = {"ps": Pool(fw, [128, 512], 5, psum=True, name="ps")}
    K["c"] = C = Consts(fw, D["consts"])
    pnum = fw.ps([128, 512], "pnum")
    pden = fw.ps([128, 512], "pden")
    pn = fw.ps([128, 512], "pn")
    nwm = fw.sb([128, 16], "nwm"); fw.dma(nwm[:], D["nwm"][:])
    skp = fw.sb([128, 16], "skip"); fw.dma(skp[:], D["skip"][:])
    preM = fw.sb([128, 2, 5, 8], "preM"); fw.dma(preM[:], D["pre_M"][:])
    preN = fw.sb([128, 2, 5, 16], "preN"); fw.dma(preN[:], D["pre_n"][:])
    Cst = fw.sb([128, 4, 4, 512], "C")
    nst = fw.sb([128, 4, 4], "n")
    mst = fw.sb([128, 4], "m")
    ld = {n: Pool(fw, sh, 2, name="ld_" + n, r32=(n == "vtok")) for n, sh in
          (("qT", [128, 16, 128]), ("kT", [128, 16, 128]), ("ktok", [128, 2048]), ("vtok", [128, 2048]), ("gsc", [128, 32]))}
    tk = Pool(fw, [128, 2048], 3, name="tk")
    fmp = Pool(fw, [128, 16, 128], 3, name="fm")
    kpp = Pool(fw, [128, 512], 3, name="kp", r32=True)
    t128 = Pool(fw, [128, 128], 4, name="t128", r32=True)
    smallp = Pool(fw, [128, 32], 12, name="sm")
    si, (ch0, nch, ncol) = 0, SEGS[0]
    flat = lambda v: v.re("p a b -> p (a b)")
    for d in range(2):
        fw.memset("pool", Cst[:], 0.0)
        fw.memset("pool", nst[:], 0.0)
        fw.memset("pool", mst[:], 0.0)
        for e in range(5):
            w = smallp.get()
            fw.tt("dve", w[:, 0:4], mst[:], preM[:, d, e, 4:8], ALU.add)
            fw.tt("dve", w[:, 4:8], w[:, 0:4], preM[:, d, e, 0:4], ALU.max)
            fw.tt("dve", w[:, 8:12], w[:, 0:4], w[:, 4:8], ALU.subtract)
            fw.tt("dve", w[:, 12:16], preM[:, d, e, 0:4], w[:, 4:8], ALU.subtract)
            fw.act(w[:, 8:16], w[:, 8:16], AF.Exp)
            fw.copy("dve", mst[:], w[:, 4:8])
            for h in range(4):
                Fb = tk.get()
                fw.dma(Fb[:], D["pre_C"][d, e, h])
                fw.ts("dve", flat(Cst[:, h]), flat(Cst[:, h]), w[:, 8 + h:9 + h], ALU.mult)
                fw.stt("dve", flat(Cst[:, h]), Fb[:], w[:, 12 + h:13 + h], flat(Cst[:, h]), ALU.mult, ALU.add)
                fw.ts("dve", nst[:, h, :], nst[:, h, :], w[:, 8 + h:9 + h], ALU.mult)
                fw.stt("dve", nst[:, h, :], preN[:, d, e, 4 * h:4 * h + 4], w[:, 12 + h:13 + h], nst[:, h, :], ALU.mult, ALU.add)
        order = range(nch) if d == 0 else range(nch - 1, -1, -1)
        for lc in order:
            ch = ch0 + lc
            t = {}
            for n, src in (("qT", "o_qT"), ("kT", "o_kT"), ("ktok", "o_ktok"), ("vtok", "o_vtok"), ("gsc", "o_gsc")):
                t[n] = ld[n].get()
                fw.dma(t[n][:], D[src][ch])
            qT, kT, ktok, vtok, gsc = (t[n] for n in ("qT", "kT", "ktok", "vtok", "gsc"))
            w = smallp.get()
            fw.tt("dve", w[:, 0:4], mst[:], gsc[:, 16 + 4 * d:20 + 4 * d], ALU.max)
            fw.tt("dve", w[:, 4:8], mst[:], w[:, 0:4], ALU.subtract)
            fw.tt("dve", w[:, 8:12], gsc[:, 8 + 4 * d:12 + 4 * d], w[:, 0:4], ALU.add)
            fw.ts("dve", w[:, 8:12], w[:, 8:12], -1.0, ALU.mult)
            fw.tt("dve", w[:, 12:16], gsc[:, 4 * d:4 * d + 4], w[:, 0:4], ALU.subtract)
            fw.act(w[:, 4:16], w[:, 4:16], AF.Exp)
            fw.tt("dve", mst[:], gsc[:, 24 + 4 * d:28 + 4 * d], w[:, 0:4], ALU.add)
            hd = tk.get()
            for h in range(4):
                fw.ts("pool", flat(Cst[:, h]), flat(Cst[:, h]), w[:, 4 + h:5 + h], ALU.mult)
                fw.ts("dve", nst[:, h, :], nst[:, h, :], w[:, 4 + h:5 + h], ALU.mult)
                p = K["ps"].get()
                for dt in range(4):
                    fw.mm(p[:, 0:128], kT[:, 4 * h + dt, :], qT[:, 4 * h + dt, :], dt == 0, dt == 3)
                P = t128.get()
                fw.stt("dve", P[:], p[:, 0:128], w[:, 12 + h:13 + h], C.m_ge if d else C.m_le, ALU.mult, ALU.mult)
                fw.mm(pnum[:], P[:], vtok[:, 512 * h:512 * h + 512], True, False)
                for dt in range(4):
                    fw.mm(pnum[:], qT[:, 4 * h + dt, :], Cst[:, h, dt, :], False, dt == 3)
                fw.mm(pden[:, 0:1], P[:], C.ones[:, 0:1], True, False)
                for dt in range(4):
                    fw.mm(pden[:, 0:1], qT[:, 4 * h + dt, :], nst[:, h, dt:dt + 1], False, dt == 3)
                rr = smallp.get()
                fw.ts("dve", rr[:, 2:3], pden[:, 0:1], -1.0, ALU.mult)
                fw.tt("dve", rr[:, 0:1], rr[:, 2:3], pden[:, 0:1], ALU.max)
                fw.tt("dve", rr[:, 0:1], rr[:, 0:1], w[:, 8 + h:9 + h], ALU.max)
                fw.op("dve", (lambda o, i: (lambda e: e.reciprocal(o, i)))(rr[:, 1:2].ap, rr[:, 0:1].ap), [rr], [rr])
                fw.act(hd[:, 512 * h:512 * h + 512], pnum[:], AF.Copy, scale=rr[:, 1:2])
                emit_mstate(fw, K, C, ktok, vtok, w, Cst, nst, h, 12 + h, kpp, pn)
            if d == 0:
                fw.dma(D["hf"][ch], hd[:])
                continue
            hf = tk.get()
            fw.dma(hf[:], D["hf"][ch])
            fw.tt("pool", hd[:], hd[:], hf[:], ALU.add)
            h4 = lambda v: v.re("p (h e) -> p h e", h=4)
            st_ = smallp.get()
            fw.rsum("dve", st_[:, 0:4], h4(hd[:]))
            fw.ts("dve", st_[:, 0:4], st_[:, 0:4], -1.0 / 512, ALU.mult)
            fw.tt("dve", h4(hd[:]), h4(hd[:]), st_[:, 0:4].unsq(2).bc([128, 4, 512]), ALU.add)
            fw.act(hf[:], hd[:], AF.Square)
            fw.rsum("dve", st_[:, 4:8], h4(hf[:]))
            emit_rstd(fw, st_[:, 4:8], st_[:, 4:8], 1.0 / 512, LN_EPS)
            fw.tt("dve", h4(hd[:]), h4(hd[:]), st_[:, 4:8].unsq(2).bc([128, 4, 512]), ALU.mult)
            hnT = fmp.get()
            for ft in range(16):
                pt = K["ps"].get()
                fw.tr(pt[:, 0:128], hd[:, 128 * ft:128 * ft + 128], C.ident)
                fw.copy("act" if ft % 2 else "dve", hnT[:, ft, :], pt[:, 0:128])
            xc = fmp.get(); fw.dma(xc[:], D["o_xc"][ch])
            sz = fmp.get(); fw.dma(sz[:], D["o_sz"][ch])
            fw.tt("dve", hnT[:], hnT[:], nwm[:].unsq(2).bc([128, 16, 128]), ALU.mult)
            fw.tt("pool", xc[:], xc[:], skp[:].unsq(2).bc([128, 16, 128]), ALU.mult)
            fw.tt("pool", hnT[:], hnT[:], xc[:], ALU.add)
            fw.tt("dve", hnT[:], hnT[:], sz[:], ALU.mult)
            fw.dma(D["ycT"][:, :, 128 * ch:128 * ch + 128], hnT[:])


def layer1(inp, lat1, cx1):
    consts = host_consts()
    segs = seg_inputs(lat1, cx1)
    up = inp["ml_up_w"][0]
    bd = []
    for wn in ("ml_wq", "ml_wk", "ml_wv"):
        w = inp[wn][0]
        t = np.zeros((16, 128, 128), np.float32)
        for b in range(512):
            ft, o = divmod(4 * b, 128)
            t[ft, o:o + 4, o:o + 4] = w[b]
        bd.append(t.transpose(1, 0, 2))
    wbd = np.ascontiguousarray(np.stack(bd, 1))
    cwl = np.ascontiguousarray(inp["ml_conv_w"][0].T.reshape(16, 128, 5).transpose(1, 0, 2))
    shared = {"consts": consts, "adaw": wblk(inp["ada_w"][1], 128), "adab": colvec(inp["ada_b"][1]),
              "nw": colvec(inp["norm_mix_w"][1]), "cw": cwl, "cb": colvec(inp["ml_conv_b"][0]),
              "w_xm": wblk(up[:, 0:2048], 128), "w_z": wblk(up[:, 2048:4096], 128), "wbd": wbd,
              "gw": np.ascontiguousarray(inp["ml_gate_w"][0].reshape(48, 128, 16).transpose(1, 0, 2)),
              "gb": rep(inp["ml_gate_b"][0])}
    maps = []
    for r in range(NCORE):
        b, s = divmod(r, 4)
        m = dict(shared)
        m.update(segs[r])
        m["cvec"] = np.ascontiguousarray(np.stack([colvec(inp["c"][b]), colvec(inp["c_ctx"])], -1))
        maps.append(m)
    ins = {k: list(v.shape) for k, v in maps[0].items()}
    tot_shapes = {"mod_out": [128, 48, 2], "o_totC": [2, 2, 4, 128, 2048], "o_totn": [2, 2, 128, 16], "o_totm": [2, 128, 16]}
    res1 = run_prog([l1p1a, l1p1b], ins, dict(tot_shapes), dict(SCR1), maps)
    maps3 = []
    shared3 = {"nwm": colvec(inp["ml_norm_w"][0]), "skip": colvec(inp["ml_skip"][0]),
               "nwf": colvec(inp["norm_ffn_w"][1]), "w_out": wblk(inp["ml_down_w"][0], 128),
               "w1": wblk(inp["ffn_w1"][1], 128), "w3": wblk(inp["ffn_w3"][1], 128), "w2": wblk(inp["ffn_w2"][1], 128),
               "nwfin": colvec(inp["final_norm_w"])}
    for r in range(NCORE):
        b, s = divmod(r, 4)
        base = 4 * b
        pC = np.zeros((2, 5, 4, 128, 2048), np.float32)
        pn = np.zeros((128, 2, 5, 16), np.float32)
        pM = np.zeros((128, 2, 5, 8), np.float32)
        pM[:, :, :, 0:4] = -1e30
        fwd = [(base + 0, 1, True), (base + 1, 1, True)] + [(base + k, 0, k < s) for k in range(3)]
        bwd = [(base + 1, 1, True), (base + 0, 1, True)] + [(base + k, 0, k > s) for k in (3, 2, 1)]
        for d, lst in enumerate((fwd, bwd)):
            for e, (cr, cs_, u) in enumerate(lst):
                if not u:
                    continue
                pC[d, e] = res1[cr]["o_totC"][cs_, d]
                pn[:, d, e] = res1[cr]["o_totn"][cs_, d]
                tm = res1[cr]["o_totm"][cs_]
                pM[:, d, e, 0:4] = tm[:, 4 * d:4 * d + 4]
                pM[:, d, e, 4:8] = tm[:, 8 + 4 * d:12 + 4 * d]
        m = dict(maps[r])
        m.update(shared3)
        m["pre_C"] = pC
        m["pre_n"] = pn
        m["pre_M"] = pM
        maps3.append(m)
    ins3 = {k: list(v.shape) for k, v in maps3[0].items()}
    outs3 = {"xo0": [128, 8, 2048]}
    scr3 = {"hf": [NCH, 128, 2048], "ycT": [128, 16, NCH * 128]}
    scr3.update(SCR1)
    scr3.update(tot_shapes)
    res3 = run_prog([l1p1a, l1p3_sweeps, lambda fw, D: emit_tail(fw, D, 16, True)], ins3, outs3, scr3, maps3)
    out = np.zeros_like(lat1)
    for r in range(NCORE):
        b, s = divmod(r, 4)
        out[b, s * 2048:(s + 1) * 2048] = res3[r]["xo0"].transpose(2, 1, 0).reshape(2048, 1024)
    return out


def kernel(**inp):
    inp = {k: np.asarray(v, np.float32) for k, v in inp.items()}
    lat1, cx1 = layer0(inp, inp["x"], inp["ctx"])
    return layer1(inp, lat1, cx1)
```

```python
import numpy as np
from contextlib import ExitStack
import concourse.bass as bass
import concourse.mybir as mybir
from concourse.bass_utils import run_bass_kernel_spmd

F32 = mybir.dt.float32
F32R = mybir.dt.float32r
AF = mybir.ActivationFunctionType
ALU = mybir.AluOpType
AX = mybir.AxisListType

NCORE = 8
DEBUG = None
GC = 2
GC1 = 2
NCH = 17
L = 128
RMS_EPS = 1e-6
LN_EPS = 1e-5


class View:
    def __init__(self, b, ap):
        self.b = b
        self.ap = ap

    def __getitem__(self, k):
        return View(self.b, self.ap[k])

    def unsq(self, ax):
        return View(self.b, self.ap.unsqueeze(ax))

    def bc(self, shape):
        return View(self.b, self.ap.to_broadcast(list(shape)))

    def re(self, s, **kw):
        return View(self.b, self.ap.rearrange(s, **kw))


class Buf:
    def __init__(self, name, t, psum=False, r32=False):
        self.name = name
        self.t = t
        self.psum = psum
        self.r32 = r32
        self.last_w = None
        self.reads = {}

    def __getitem__(self, k):
        return View(self, self.t[k])


def _bufs(*vs):
    out = []
    for v in vs:
        if isinstance(v, View) and v.b not in out:
            out.append(v.b)
    return out


def _ap(v):
    return v.ap if isinstance(v, View) else v


def _o(v):
    return v.ap.bitcast(F32R) if v.b.r32 else v.ap


class FW:
    ENGS = ("pe", "act", "dve", "pool", "sp")
    EOBJ = {"pe": "tensor", "act": "scalar", "dve": "vector", "pool": "gpsimd", "sp": "sync"}
    NSLOT = 8

    def __init__(self, nc, stack):
        self.nc = nc
        self.sem = {e: stack.enter_context(nc.semaphore("s_" + e)) for e in self.ENGS}
        self.count = {e: 0 for e in self.ENGS}
        self.prog = {e: [] for e in self.ENGS}
        self.waited = {e: {} for e in self.ENGS}
        self.slots = {}
        for q in ("sp", "pool", "act"):
            self.slots[q] = [[stack.enter_context(nc.semaphore("d_%s%d" % (q, i))), 0] for i in range(self.NSLOT)]
        self.slot_i = {q: 0 for q in self.slots}
        self.nbuf = 0
        self.stack = None
        self.dq = 0

    def sb(self, shape, name=None, r32=False):
        self.nbuf += 1
        name = (name or "sb") + "_%d" % self.nbuf
        return Buf(name, self.stack.enter_context(self.nc.sbuf_tensor(name, list(shape), F32)), r32=r32)

    def ps(self, shape, name=None):
        self.nbuf += 1
        name = (name or "ps") + "_%d" % self.nbuf
        return Buf(name, self.stack.enter_context(self.nc.psum_tensor(name, list(shape), F32)), psum=True)

    def dram(self, name, shape, kind="Internal"):
        return Buf(name, self.nc.dram_tensor(name, list(shape), F32, kind=kind).ap())

    def _waits(self, eng, reads, writes):
        need = {}

        def add(tok):
            if tok is None:
                return
            s, v = tok
            if id(s) not in need or need[id(s)][1] < v:
                need[id(s)] = (s, v)

        for b in reads:
            add(b.last_w)
            if b.psum:
                for k_, tok in b.reads.items():
                    if k_ != eng:
                        add(tok)
        for b in writes:
            add(b.last_w)
            for tok in b.reads.values():
                add(tok)
        out = []
        for k, (s, v) in need.items():
            if eng == "pe" and s is self.sem["pe"]:
                continue
            if self.waited[eng].get(k, 0) >= v:
                continue
            self.waited[eng][k] = v
            out.append((s, v))
        return out

    def op(self, eng, fn, reads, writes):
        waits = self._waits(eng, reads, writes)
        self.count[eng] += 1
        tok = (self.sem[eng], self.count[eng])
        self.prog[eng].append((waits, fn, self.sem[eng], 1))
        for b in reads:
            b.reads[eng] = tok
        for b in writes:
            b.last_w = tok
            b.reads = {}
        return tok

    def dma(self, out, in_, q=None):
        if out.b.r32:
            q = "pool"
        elif q is None:
            q = ("sp", "pool")[self.dq % 2]
            self.dq += 1
        reads, writes = [in_.b], [out.b]
        sl = self.slots[q]
        i = self.slot_i[q]
        self.slot_i[q] = (i + 1) % len(sl)
        s, v = sl[i]
        waits = self._waits(q, reads, writes)
        if v > 0 and self.waited[q].get(id(s), 0) < v:
            self.waited[q][id(s)] = v
            waits.append((s, v))
        sl[i][1] = v + 16
        tok = (s, v + 16)
        oa, ia = out.ap, in_.ap
        if out.b.r32:
            oa, ia = oa.bitcast(F32R), ia.bitcast(F32R)
        self.prog[q].append((waits, lambda e: e.dma_start(out=oa, in_=ia), s, 16))
        for b in reads:
            b.reads[("dma", q, i)] = tok
        for b in writes:
            b.last_w = tok
            b.reads = {}
        return tok

    def allgather(self, out, in_, groups):
        q = "pool"
        reads, writes = [in_.b], [out.b]
        sl = self.slots[q]
        i = self.slot_i[q]
        self.slot_i[q] = (i + 1) % len(sl)
        s, v = sl[i]
        waits = self._waits(q, reads, writes)
        if v > 0 and self.waited[q].get(id(s), 0) < v:
            self.waited[q][id(s)] = v
            waits.append((s, v))
        sl[i][1] = v + 16
        tok = (s, v + 16)
        oa, ia = out.ap, in_.ap
        self.prog[q].append((waits, lambda e: e.collective_compute("AllGather", ALU.bypass, replica_groups=groups,
                                                                    ins=[ia], outs=[oa]), s, 16))
        for b in reads:
            b.reads[("dma", q, i)] = tok
        for b in writes:
            b.last_w = tok
            b.reads = {}
        return tok

    def flush(self, final=False):
        fin = {}
        for q in self.slots:
            for s, v in self.slots[q]:
                if v > 0:
                    fin[id(s)] = (s, v)
        for e in self.ENGS:
            if self.count[e] > 0:
                fin[id(self.sem[e])] = (self.sem[e], self.count[e])
        progs = self.prog
        self.prog = {e: [] for e in self.ENGS}
        with self.nc.Block() as block:
            for e in self.ENGS:
                extra = []
                for k, (s, v) in fin.items():
                    if self.waited[e].get(k, 0) < v:
                        self.waited[e][k] = v
                        extra.append((s, v))

                def body(eng, prog=progs[e], extra=extra):
                    for waits, fn, s, inc in prog:
                        for ws, wv in waits:
                            eng.wait_ge(ws, wv)
                        fn(eng).then_inc(s, inc)
                    for ws, wv in extra:
                        eng.wait_ge(ws, wv)

                getattr(block, self.EOBJ[e])(body)

    def mm(self, out, lhsT, rhs, start=True, stop=True):
        o, l, r = out.ap, lhsT.ap, rhs.ap
        if lhsT.b.r32 and rhs.b.r32:
            l, r = l.bitcast(F32R), r.bitcast(F32R)
        return self.op("pe", lambda e: e.matmul(o, l, r, start=start, stop=stop), _bufs(lhsT, rhs), _bufs(out))

    def tr(self, out, in_, ident):
        o, i, d = out.ap, in_.ap, ident.ap
        return self.op("pe", lambda e: e.transpose(o, i, d), _bufs(in_, ident), _bufs(out))

    def act(self, out, in_, func, bias=None, scale=None):
        kw = {}
        if bias is not None:
            kw["bias"] = _ap(bias)
        if scale is not None:
            kw["scale"] = _ap(scale)
        o, i = _o(out), in_.ap
        return self.op("act", lambda e: e.activation(o, i, func, **kw), _bufs(in_, bias, scale), _bufs(out))

    def tt(self, eng, out, a, b, op):
        o, x, y = _o(out), a.ap, b.ap
        return self.op(eng, lambda e: e.tensor_tensor(o, x, y, op), _bufs(a, b), _bufs(out))

    def ts(self, eng, out, a, s1, op0, s2=None, op1=None):
        o, x, p1, p2 = _o(out), a.ap, _ap(s1), _ap(s2)
        if op1 is None:
            fn = lambda e: e.tensor_scalar(o, x, p1, None, op0)
        else:
            fn = lambda e: e.tensor_scalar(o, x, p1, p2, op0, op1)
        return self.op(eng, fn, _bufs(a, s1, s2), _bufs(out))

    def stt(self, eng, out, a, sc, b, op0, op1):
        o, x, s, y = _o(out), a.ap, _ap(sc), b.ap
        return self.op(eng, lambda e: e.scalar_tensor_tensor(o, x, s, y, op0, op1), _bufs(a, sc, b), _bufs(out))

    def copy(self, eng, out, in_):
        o, i = _o(out), in_.ap
        if eng == "act":
            return self.op("act", lambda e: e.copy(o, i), _bufs(in_), _bufs(out))
        return self.op(eng, lambda e: e.tensor_copy(o, i), _bufs(in_), _bufs(out))

    def memset(self, eng, out, val):
        o = out.ap
        return self.op(eng, lambda e: e.memset(o, val), [], _bufs(out))

    def rsum(self, eng, out, in_):
        o, i = out.ap, in_.ap
        return self.op(eng, lambda e: e.reduce_sum(o, i, AX.X), _bufs(in_), _bufs(out))

    def rmax(self, eng, out, in_):
        o, i = out.ap, in_.ap
        return self.op(eng, lambda e: e.reduce_max(o, i, AX.X), _bufs(in_), _bufs(out))


class Pool:
    def __init__(self, fw, shape, n, psum=False, name=None, r32=False):
        self.bufs = [fw.ps(shape, name) if psum else fw.sb(shape, name, r32=r32) for _ in range(n)]
        self.i = 0

    def get(self):
        b = self.bufs[self.i]
        self.i = (self.i + 1) % len(self.bufs)
        return b


class Consts:
    def __init__(self, fw, cdram):
        self.t = fw.sb([128, 6, 128], "consts")
        fw.dma(self.t[:], cdram[:])
        self.ident = self.t[:, 0, :]
        self.m_le = self.t[:, 1, :]
        self.m_ge = self.t[:, 2, :]
        self.m_gt = self.t[:, 3, :]
        self.m_lt = self.t[:, 4, :]
        self.ones = self.t[:, 5, :]


def host_consts():
    j = np.arange(128)[:, None]
    i = np.arange(128)[None, :]
    c = np.stack([(j == i), (j <= i), (j >= i), (j > i), (j < i), np.ones((128, 128), bool)], 0).astype(np.float32)
    return np.ascontiguousarray(c.transpose(1, 0, 2))


def emit_norm(fw, K, xin, hout, c0, c1, gs, sh, col, nk=8, dim=1024.0):
    n = c1 - c0
    ss = K["ps"].get()
    for kt in range(nk):
        sq = K["t512"].get()
        fw.act(sq[:, :n], xin[:, kt, c0:c1], AF.Square)
        fw.mm(ss[:, :n], K["c"].ones, sq[:, :n], kt == 0, kt == nk - 1)
    if "nrm" not in K:
        K["nrm"] = Pool(fw, [128, 512], 2, name="nrm")
    t = K["nrm"].get()
    fw.ts("dve", t[:, :n], ss[:, :n], 1.0 / dim, ALU.mult, RMS_EPS, ALU.add)
    fw.act(t[:, :n], t[:, :n], AF.Ln)
    r = K["nrm"].get()
    fw.act(r[:, :n], t[:, :n], AF.Exp, scale=-0.5)
    for kt in range(nk):
        t2 = K["t512"].get()
        fw.stt("dve", t2[:, :n], xin[:, kt, c0:c1], gs[:, kt, col:col + 1], r[:, :n], ALU.mult, ALU.mult)
        if sh is None:
            fw.copy("act", hout[:, kt, c0:c1], t2[:, :n])
        else:
            fw.act(hout[:, kt, c0:c1], t2[:, :n], AF.Identity, bias=sh[:, kt, col:col + 1])


def emit_mods(fw, K, cvec_d, adaw_d, adab_d, nw_d, nwf_d, mod_out_d):
    cv = fw.sb([128, 8, 2], "cvec")
    fw.dma(cv[:], cvec_d[:])
    sc = fw.sb([128, 8, 2], "silu_c")
    fw.act(sc[:], cv[:], AF.Silu)
    ab = fw.sb([128, 48], "adab")
    fw.dma(ab[:], adab_d[:])
    mod = fw.sb([128, 48, 2], "mod")
    wp = Pool(fw, [128, 8, 128], 3, name="adaw")
    for ft in range(48):
        w = wp.get()
        fw.dma(w[:], adaw_d[ft])
        p = K["ps"].get()
        for kt in range(8):
            fw.mm(p[:, 0:2], w[:, kt, :], sc[:, kt, :], kt == 0, kt == 7)
        fw.ts("dve", mod[:, ft, :], p[:, 0:2], ab[:, ft:ft + 1], ALU.add)
    if mod_out_d is not None:
        fw.dma(mod_out_d[:], mod[:])
    return mod


def emit_gs(fw, mod, nw_d, scale_ft0, name):
    nw = fw.sb([128, 8], name + "_nw")
    fw.dma(nw[:], nw_d[:])
    gs = fw.sb([128, 8, 2], name)
    fw.ts("dve", gs[:], mod[:, scale_ft0:scale_ft0 + 8, :], 1.0, ALU.add)
    fw.tt("dve", gs[:], gs[:], nw[:].unsq(2).bc([128, 8, 2]), ALU.mult)
    return gs


SEGS = ((0, 16, 2052), (16, 1, 132))


def l0p1(fw, D, totals=True):
    st = fw.stack
    K = {"ps": Pool(fw, [128, 512], 8, psum=True, name="ps"), "t512": Pool(fw, [128, 512], 6, name="t512")}
    K["c"] = C = Consts(fw, D["consts"])
    mod = emit_mods(fw, K, D["cvec"], D["adaw"], D["adab"], None, None, D["mod_out"])
    gs = emit_gs(fw, mod, D["nw"], 8, "gs")
    sh = mod[:, 0:8, :]
    hm = fw.sb([128, 4], "hm"); fw.dma(hm[:], D["hm"][:])
    cw = fw.sb([128, 16, 5], "cw"); fw.dma(cw[:], D["cw"][:])
    cb = fw.sb([128, 16], "cb"); fw.dma(cb[:], D["cb"][:])
    wdt = fw.sb([128, 8, 16], "wdt"); fw.dma(wdt[:], D["w_dt"][:])
    sm = fw.sb([128, 72], "small"); fw.dma(sm[:], D["small"][:])
    iota = fw.sb([128, 2], "iota"); fw.dma(iota[:], D["iota"][:])
    negA = fw.sb([128, 32], "negA")
    fw.act(negA[:], sm[:, 32:64], AF.Exp)
    fw.ts("dve", negA[:], negA[:], -1.0, ALU.mult)
    lg = fw.sb([128, 8], "lg")
    fw.act(lg[:], sm[:, 64:72], AF.Exp, scale=-1.0)
    fw.act(lg[:], lg[:], AF.Ln, bias=1.0)
    fw.ts("dve", lg[:], lg[:], -1.0, ALU.mult)
    rsc = fw.sb([128, 16], "rsc")
    ip1 = fw.sb([128, 4], "ip1")
    fw.ts("dve", ip1[:, 0:1], iota[:, 0:1], 1.0, ALU.add)
    fw.ts("dve", ip1[:, 1:2], iota[:, 1:2], 1.0, ALU.add)
    fw.copy("dve", ip1[:, 2:3], iota[:, 1:2])
    fw.copy("dve", ip1[:, 3:4], iota[:, 0:1])
    for n_, (lgc, ic) in enumerate(((0, 0), (4, 1), (0, 2), (4, 3))):
        fw.ts("dve", rsc[:, 4 * n_:4 * n_ + 4], lg[:, lgc:lgc + 4], ip1[:, ic:ic + 1], ALU.mult)
    fw.act(rsc[:], rsc[:], AF.Exp)
    Aret = fw.sb([128, 8], "Aret")
    fw.act(Aret[:], lg[:], AF.Exp, scale=128.0)

    xg = fw.sb([128, 8, 128 * GC + 4], "xg")
    hT = fw.sb([128, 8, 128 * GC + 4], "hT", r32=True)
    xbc = fw.sb([128, 16, 128 * GC], "xbcT")
    wxp = Pool(fw, [128, 8, 128], 3, name="wx", r32=True)
    wtp = Pool(fw, [128, 8, 256], 2, name="wt", r32=True)
    cacc = Pool(fw, [128, 128], 3, name="cacc")
    tokp = Pool(fw, [128, 1024], 5, name="tok")
    tok5 = Pool(fw, [128, 512], 10, name="tok5")
    fmp = Pool(fw, [128, 16, 128], 2, name="fm")
    kstp = Pool(fw, [128, 1024], 3, name="kst")
    qkst = Pool(fw, [128, 512], 2 * GC, name="qkst")
    smallp = Pool(fw, [128, 64], 8, name="sm")
    Stot = fw.sb([128, 4, 1024], "Stot")
    Pb = fw.sb([128, 20], "Pb")
    Af = fw.sb([128, 20], "Af")
    zst = fw.sb([128, GC, 1024], "zstage")

    for si, (ch0, nch, ncol) in enumerate(SEGS):
        xT_d = D["xT%d" % si]
        cs_d = D["cs%d" % si]
        fw.memset("pool", Stot[:], 0.0)
        fw.memset("pool", Pb[:], 1.0)
        fw.memset("pool", Af[:], 1.0)
        ngrp = (nch + GC - 1) // GC
        for g in range(ngrp):
            gch = min(GC, nch - GC * g)
            W = gch * 128
            c0 = 128 * GC * g
            fw.dma(xg[:, :, 0:W + 4], xT_d[:, :, c0:c0 + W + 4])
            for a in range(0, W + 4, 512):
                b_ = min(a + 512, W + 4)
                emit_norm(fw, K, xg, hT, a, b_, gs, sh, si)
            if g == 0:
                fw.ts("dve", hT[:, :, 0:2], hT[:, :, 0:2], hm[:, 2 * si:2 * si + 1], ALU.mult)
            if g == ngrp - 1:
                fw.ts("dve", hT[:, :, W + 2:W + 4], hT[:, :, W + 2:W + 4], hm[:, 2 * si + 1:2 * si + 2], ALU.mult)
            for ft in range(16):
                w = wxp.get()
                fw.dma(w[:], D["w_xbc"][ft])
                p = K["ps"].get()
                for kt in range(8):
                    fw.mm(p[:, 0:W + 4], w[:, kt, :], hT[:, kt, 0:W + 4], kt == 0, kt == 7)
                for j in range(gch):
                    acc = cacc.get()
                    fw.ts("dve", acc[:], p[:, 128 * j:128 * j + 128], cw[:, ft, 0:1], ALU.mult)
                    for k in range(1, 5):
                        fw.stt("dve", acc[:], p[:, 128 * j + k:128 * j + k + 128], cw[:, ft, k:k + 1], acc[:], ALU.mult, ALU.add)
                    fw.act(xbc[:, ft, 128 * j:128 * j + 128], acc[:], AF.Silu, bias=cb[:, ft:ft + 1])
            kstc = {}
            qkT = {}
            for name, b0, nb in (("z", 0, 4), ("q", 4, 2), ("k", 6, 2), ("v", 8, 4), ("g", 12, 4)):
                stg = {}
                for bi in range(nb):
                    w = wtp.get()
                    fw.dma(w[:], D["w_tm"][b0 + bi])
                    for j in range(gch):
                        ch = ch0 + GC * g + j
                        if bi == 0:
                            stg[j] = zst[:, j, :] if name == "v" else (tokp.get()[:] if name in "zg" else qkst.get()[:])
                        p = K["ps"].get()
                        for kt in range(8):
                            fw.mm(p[:, 0:256], hT[:, kt, 2 + 128 * j:2 + 128 * j + 128], w[:, kt, :], kt == 0, kt == 7)
                        o = stg[j][:, 256 * bi:256 * bi + 256]
                        if name in "zg":
                            fw.act(o, p[:, 0:256], AF.Silu)
                        elif name == "k":
                            fw.act(o, p[:, 0:256], AF.Copy, scale=float(128.0 ** -0.5))
                        else:
                            fw.copy("act", o, p[:, 0:256])
                        if bi == nb - 1:
                            if name == "z":
                                fw.dma(D["o_z"][ch], stg[j])
                            elif name == "g":
                                fw.dma(D["o_g"][ch], stg[j])
                            elif name == "v":
                                fw.dma(D["o_v"][ch], stg[j])
                            elif name == "q":
                                qkT[j] = fmp.get()
                                emit_rope_q(fw, K, C, stg[j], cs_d, GC * g + j, rsc, qkT[j], tok5)
                            else:
                                kstc[j] = emit_rope_k(fw, K, C, stg[j], cs_d, GC * g + j, rsc, qkT[j], tok5, kstp)
                                fw.dma(D["o_qk"][ch], qkT[j][:])
                                fw.dma(D["o_kst"][ch], kstc[j][:])
            for j in range(gch):
                ch = ch0 + GC * g + j
                p = K["ps"].get()
                for kt in range(8):
                    fw.mm(p[:, 0:16], hT[:, kt, 2 + 128 * j:2 + 128 * j + 128], wdt[:, kt, :], kt == 0, kt == 7)
                dl = smallp.get()
                fw.tt("dve", dl[:, 0:16], p[:, 0:16], sm[:, 0:16], ALU.add)
                fw.tt("dve", dl[:, 16:32], p[:, 0:16], sm[:, 16:32], ALU.add)
                fw.act(dl[:, 0:32], dl[:, 0:32], AF.Exp)
                fw.act(dl[:, 0:32], dl[:, 0:32], AF.Ln, bias=1.0)
                fw.tt("dve", dl[:, 32:64], dl[:, 0:32], negA[:], ALU.mult)
                fw.dma(D["o_dl"][ch], dl[:])
                fw.dma(D["o_cb"][ch], xbc[:, 8:16, 128 * j:128 * j + 128])
                xs = tokp.get()
                for ft in range(8):
                    pt = K["ps"].get()
                    fw.tr(pt[:, 0:128], xbc[:, ft, 128 * j:128 * j + 128], C.ident)
                    fw.copy("act" if ft % 2 else "dve", xs[:, 128 * ft:128 * ft + 128], pt[:, 0:128])
                fw.dma(D["o_xs"][ch], xs[:])
                bm = tok5.get()
                for ft in range(4):
                    pt = K["ps"].get()
                    fw.tr(pt[:, 0:128], xbc[:, 8 + ft, 128 * j:128 * j + 128], C.ident)
                    fw.copy("act" if ft % 2 else "dve", bm[:, 128 * ft:128 * ft + 128], pt[:, 0:128])
                fw.dma(D["o_bm"][ch], bm[:])
                if totals:
                    emit_ssd_state(fw, K, C, dl, xs, bm, Stot[:, 0, :], Stot[:, 1, :], Af, Pb, smallp, tokp, "p1")
                    emit_ret_state(fw, K, kstc[j], zst[:, j, :], Stot[:, 2, :], Stot[:, 3, :], Af, Pb, Aret, tokp, "p1")
        if not totals:
            continue
        for k in range(4):
            fw.dma(D["o_tot"][si, k], Stot[:, k, :])
        ta = smallp.get()
        fw.copy("dve", ta[:, 0:20], Af[:])
        fw.copy("dve", ta[:, 20:40], Pb[:])
        fw.dma(D["o_totA"][si], ta[:, 0:40])


def emit_rope(fw, src, cs, out, tok5, scale):
    s3 = src.re("p (h d) -> p h d", h=4)
    o3 = out[:].re("p (h d) -> p h d", h=4)
    cos = cs[:, 0:64].unsq(1).bc([128, 4, 64])
    sin = cs[:, 64:128].unsq(1).bc([128, 4, 64])
    t1 = tok5.get(); t2 = tok5.get()
    a = t1[:, 0:256].re("p (h d) -> p h d", h=4)
    b = t1[:, 256:512].re("p (h d) -> p h d", h=4)
    c = t2[:, 0:256].re("p (h d) -> p h d", h=4)
    d = t2[:, 256:512].re("p (h d) -> p h d", h=4)
    fw.tt("dve", a, s3[:, :, 0:64], cos, ALU.mult)
    fw.tt("dve", b, s3[:, :, 64:128], sin, ALU.mult)
    fw.tt("dve", c, s3[:, :, 0:64], sin, ALU.mult)
    fw.tt("dve", d, s3[:, :, 64:128], cos, ALU.mult)
    fw.tt("pool", o3[:, :, 0:64], a, b, ALU.subtract)
    fw.tt("pool", o3[:, :, 64:128], c, d, ALU.add)


def emit_rope_q(fw, K, C, qs, cs_d, lc, rsc, qk, tok5):
    cs = tok5.get()
    fw.dma(cs[:, 0:128], cs_d[lc])
    qr = tok5.get()
    emit_rope(fw, qs, cs[:, 0:128], qr, tok5, 1.0)
    variants = [qr]
    for v in range(2):
        o = tok5.get()
        fw.tt("dve" if v else "pool", o[:].re("p (h d) -> p h d", h=4), qr[:].re("p (h d) -> p h d", h=4),
              rsc[:, 4 * v:4 * v + 4].unsq(2).bc([128, 4, 128]), ALU.mult)
        variants.append(o)
    for vi, src in enumerate(variants):
        for h in range(4):
            pt = K["ps"].get()
            fw.tr(pt[:, 0:128], src[:, 128 * h:128 * h + 128], C.ident)
            fw.copy("act" if h % 2 else "dve", qk[:, 4 * vi + h, :], pt[:, 0:128])


def emit_rope_k(fw, K, C, ks, cs_d, lc, rsc, qk, tok5, kstp):
    cs = tok5.get()
    fw.dma(cs[:, 0:128], cs_d[lc])
    kr = tok5.get()
    emit_rope(fw, ks, cs[:, 0:128], kr, tok5, 1.0)
    for h in range(4):
        pt = K["ps"].get()
        fw.tr(pt[:, 0:128], kr[:, 128 * h:128 * h + 128], C.ident)
        fw.copy("act" if h % 2 else "dve", qk[:, 12 + h, :], pt[:, 0:128])
    kst = kstp.get()
    for v in range(2):
        fw.tt("dve" if v else "pool", kst[:, 512 * v:512 * v + 512].re("p (h d) -> p h d", h=4),
              kr[:].re("p (h d) -> p h d", h=4), rsc[:, 8 + 4 * v:12 + 4 * v].unsq(2).bc([128, 4, 128]), ALU.mult)
    return kst


def emit_ssd_state(fw, K, C, dl, xs, bm, Sf, Sb, Af, Pb, smallp, tokp, mode):
    dirs = {"p1": (0, 1), "f": (0,), "b": (1,)}[mode]
    for d in dirs:
        la = dl[:, 32 + 16 * d:48 + 16 * d]
        pr = K["ps"].get()
        fw.mm(pr[:, 0:16], C.m_lt if d else C.m_gt, la, True, True)
        fw.mm(pr[:, 16:32], C.ones, la, True, True)
        wv = smallp.get()
        fw.act(wv[:, 0:32], pr[:, 0:32], AF.Exp)
        fw.tt("dve", wv[:, 0:16], wv[:, 0:16], dl[:, 16 * d:16 * d + 16], ALU.mult)
        vs = tokp.get()
        fw.tt("pool", vs[:].re("p (h e) -> p h e", h=16), xs[:].re("p (h e) -> p h e", h=16),
              wv[:, 0:16].unsq(2).bc([128, 16, 64]), ALU.mult)
        S = Sb if d else Sf
        for half in range(2):
            pf = K["ps"].get()
            for gg in range(2):
                g4 = 2 * half + gg
                fw.mm(pf[:, 256 * gg:256 * gg + 256], bm[:, 128 * g4:128 * g4 + 128], vs[:, 256 * g4:256 * g4 + 256], True, True)
            Sh = S[:, 512 * half:512 * half + 512].re("p (h e) -> p h e", h=8)
            Abc = wv[:, 16 + 8 * half:24 + 8 * half].unsq(2).bc([128, 8, 64])
            pf3 = pf[:].re("p (h e) -> p h e", h=8)
            if mode == "p1" and d == 1:
                t = K["t512"].get()
                fw.tt("dve", t[:].re("p (h e) -> p h e", h=8), pf3, Pb[:, 8 * half:8 * half + 8].unsq(2).bc([128, 8, 64]), ALU.mult)
                fw.tt("pool", S[:, 512 * half:512 * half + 512], S[:, 512 * half:512 * half + 512], t[:], ALU.add)
            else:
                fw.tt("dve", Sh, Sh, Abc, ALU.mult)
                fw.tt("dve", Sh, Sh, pf3, ALU.add)
        if mode == "p1":
            if d == 0:
                fw.tt("dve", Af[:, 0:16], Af[:, 0:16], wv[:, 16:32], ALU.mult)
            else:
                fw.tt("dve", Pb[:, 0:16], Pb[:, 0:16], wv[:, 16:32], ALU.mult)


def emit_ret_state(fw, K, kst, v, Sf, Sb, Af, Pb, Aret, tokp, mode):
    dirs = {"p1": (0, 1), "f": (0,), "b": (1,)}[mode]
    for d in dirs:
        S = Sb if d else Sf
        for half in range(2):
            pf = K["ps"].get()
            for hh in range(2):
                h = 2 * half + hh
                fw.mm(pf[:, 256 * hh:256 * hh + 256], kst[:, 512 * d + 128 * h:512 * d + 128 * h + 128],
                      v[:, 256 * h:256 * h + 256], True, True)
            Sh = S[:, 512 * half:512 * half + 512].re("p (h e) -> p h e", h=2)
            pf3 = pf[:].re("p (h e) -> p h e", h=2)
            if mode == "p1" and d == 1:
                t = K["t512"].get()
                fw.tt("dve", t[:].re("p (h e) -> p h e", h=2), pf3, Pb[:, 16 + 2 * half:18 + 2 * half].unsq(2).bc([128, 2, 256]), ALU.mult)
                fw.tt("pool", S[:, 512 * half:512 * half + 512], S[:, 512 * half:512 * half + 512], t[:], ALU.add)
            else:
                fw.tt("dve", Sh, Sh, Aret[:, 4 * d + 2 * half:4 * d + 2 * half + 2].unsq(2).bc([128, 2, 256]), ALU.mult)
                fw.tt("dve", Sh, Sh, pf3, ALU.add)
        if mode == "p1":
            if d == 0:
                fw.tt("dve", Af[:, 16:20], Af[:, 16:20], Aret[:, 0:4], ALU.mult)
            else:
                fw.tt("dve", Pb[:, 16:20], Pb[:, 16:20], Aret[:, 4:8], ALU.mult)


def emit_rstd(fw, out, in_, scale, eps):
    fw.ts("dve", out, in_, scale, ALU.mult, eps, ALU.add)
    fw.act(out, out, AF.Ln)
    fw.act(out, out, AF.Exp, scale=-0.5)


def l0p3_sweeps(fw, D):
    K = {"ps": Pool(fw, [128, 512], 4, psum=True, name="ps"), "t512": Pool(fw, [128, 512], 4, name="t512")}
    K["c"] = C = Consts(fw, D["consts"])
    py = [fw.ps([128, 512], "py") for _ in range(2)]
    pr = [fw.ps([128, 512], "pr") for _ in range(2)]
    sm = fw.sb([128, 72], "small"); fw.dma(sm[:], D["small"][:])
    dsk = fw.sb([128, 16], "dskip"); fw.dma(dsk[:], D["dskip"][:])
    nws = fw.sb([128, 1024], "nws"); fw.dma(nws[:], D["nws"][:])
    diff = fw.sb([128, 2, 128], "diff"); fw.dma(diff[:], D["diff"][:])
    lg = fw.sb([128, 8], "lg")
    fw.act(lg[:], sm[:, 64:72], AF.Exp, scale=-1.0)
    fw.act(lg[:], lg[:], AF.Ln, bias=1.0)
    fw.ts("dve", lg[:], lg[:], -1.0, ALU.mult)
    Aret = fw.sb([128, 8], "Aret")
    fw.act(Aret[:], lg[:], AF.Exp, scale=128.0)
    DT = fw.sb([128, 8, 128], "DT")
    for d in range(2):
        for h in range(4):
            fw.ts("dve", DT[:, 4 * d + h, :], diff[:, d, :], lg[:, 4 * d + h:4 * d + h + 1], ALU.mult)
            fw.act(DT[:, 4 * d + h, :], DT[:, 4 * d + h, :], AF.Exp)
            fw.tt("dve", DT[:, 4 * d + h, :], DT[:, 4 * d + h, :], C.m_ge if d else C.m_le, ALU.mult)
    preA = fw.sb([128, 2, 5, 40], "preA"); fw.dma(preA[:], D["pre_A"][:])
    S = fw.sb([128, 4, 1024], "S")
    tokp = Pool(fw, [128, 1024], 7, name="tok")
    t128 = Pool(fw, [128, 128], 8, name="t128")
    smallp = Pool(fw, [128, 64], 8, name="sm")
    ld = {n: Pool(fw, sh, 2, name="ld_" + n) for n, sh in
          (("cb", [128, 8, 128]), ("dl", [128, 64]), ("xs", [128, 1024]), ("bm", [128, 512]),
           ("qk", [128, 16, 128]), ("v", [128, 1024]), ("kst", [128, 1024]))}
    yfp = Pool(fw, [128, 2048], 1, name="yf")
    SCb = fw.sb([128, 4, 128], "SC")
    lmq = Pool(fw, [128, 4, 128], 4, name="lmq")
    eqp = Pool(fw, [128, 4, 128], 4, name="eqp")
    ycp = Pool(fw, [128, 16, 128], 2, name="ycT")
    h16 = lambda v: v.re("p (h e) -> p h e", h=16)
    h4 = lambda v: v.re("p (h e) -> p h e", h=4)

    for si, (ch0, nch, ncol) in enumerate(SEGS):
        fw.memset("pool", S[:], 0.0)
        for e in range(5):
            for k in range(4):
                Fb = tokp.get()
                fw.dma(Fb[:], D["pre_F"][si, e, k])
                if k < 2:
                    A = preA[:, si, e, 20 * k:20 * k + 16].unsq(2).bc([128, 16, 64])
                    Sv = h16(S[:, k, :])
                else:
                    A = preA[:, si, e, 20 * (k - 2) + 16:20 * (k - 2) + 20].unsq(2).bc([128, 4, 256])
                    Sv = h4(S[:, k, :])
                fw.tt("dve", Sv, Sv, A, ALU.mult)
                fw.tt("pool", S[:, k, :], S[:, k, :], Fb[:], ALU.add)
        for d in range(2):
            order = range(nch) if d == 0 else range(nch - 1, -1, -1)
            for lc in order:
                ch = ch0 + lc
                t = {}
                for n, src in (("cb", "o_cb"), ("dl", "o_dl"), ("xs", "o_xs"), ("bm", "o_bm"),
                               ("qk", "o_qk"), ("v", "o_v"), ("kst", "o_kst")):
                    t[n] = ld[n].get()
                    fw.dma(t[n][:], D[src][ch])
                cbm, dl, xs, bm, qk, v, kst = (t[n] for n in ("cb", "dl", "xs", "bm", "qk", "v", "kst"))
                for g4 in range(4):
                    p = K["ps"].get()
                    fw.mm(p[:, 0:128], cbm[:, g4, :], cbm[:, 4 + g4, :])
                    fw.tt("dve", SCb[:, g4, :], p[:, 0:128], C.m_ge if d else C.m_le, ALU.mult)
                vdt = tokp.get()
                fw.tt("pool", h16(vdt[:]), h16(xs[:]), dl[:, 16 * d:16 * d + 16].unsq(2).bc([128, 16, 64]), ALU.mult)
                la = dl[:, 32 + 16 * d:48 + 16 * d]
                mask1 = C.m_lt if d else C.m_gt
                mask2 = C.m_ge if d else C.m_le
                pdb = []
                for q in range(4):
                    Lm = lmq.get()
                    fw.tt("pool" if q % 2 else "dve", Lm[:], mask1.unsq(1).bc([128, 4, 128]),
                          la[:, 4 * q:4 * q + 4].unsq(2).bc([128, 4, 128]), ALU.mult)
                    pd = K["ps"].get()
                    pdb.append(pd)
                    for hh in range(4):
                        fw.mm(pd[:, 128 * hh:128 * hh + 128], Lm[:, hh, :], mask2)
                Pq = []
                for q in range(4):
                    E = eqp.get()
                    fw.act(E[:].re("p a b -> p (a b)"), pdb[q][:], AF.Exp)
                    fw.tt("dve" if q % 2 else "pool", E[:], E[:], SCb[:, q, :].unsq(1).bc([128, 4, 128]), ALU.mult)
                    Pq.append(E)
                for h in range(16):
                    fw.mm(py[h // 8][:, 64 * (h % 8):64 * (h % 8) + 64], Pq[h // 4][:, h % 4, :], vdt[:, 64 * h:64 * h + 64])
                pu = [K["ps"].get(), K["ps"].get()]
                for g4 in range(4):
                    fw.mm(pu[g4 // 2][:, 256 * (g4 % 2):256 * (g4 % 2) + 256], cbm[:, 4 + g4, :], S[:, d, 256 * g4:256 * g4 + 256])
                pc = K["ps"].get()
                fw.mm(pc[:, 0:16], C.m_ge if d else C.m_le, la)
                ecs = smallp.get()
                fw.act(ecs[:, 0:16], pc[:, 0:16], AF.Exp)
                y = tokp.get()
                for half in range(2):
                    tq = K["t512"].get()
                    fw.tt("dve", tq[:].re("p (h e) -> p h e", h=8), pu[half][:].re("p (h e) -> p h e", h=8),
                          ecs[:, 8 * half:8 * half + 8].unsq(2).bc([128, 8, 64]), ALU.mult)
                    fw.tt("dve", y[:, 512 * half:512 * half + 512], tq[:], py[half][:], ALU.add)
                emit_ssd_state(fw, K, C, dl, xs, bm, S[:, 0, :], S[:, 1, :], None, None, smallp, tokp, "b" if d else "f")
                for h in range(4):
                    p = K["ps"].get()
                    fw.mm(p[:, 0:128], qk[:, 12 + h, :], qk[:, h, :])
                    P = t128.get()
                    fw.tt("dve", P[:], p[:, 0:128], DT[:, 4 * d + h, :], ALU.mult)
                    o = pr[h // 2][:, 256 * (h % 2):256 * (h % 2) + 256]
                    fw.mm(o, P[:], v[:, 256 * h:256 * h + 256], True, False)
                    fw.mm(o, qk[:, 4 * (1 + d) + h, :], S[:, 2 + d, 256 * h:256 * h + 256], False, True)
                yr = tokp.get()
                fw.copy("act", yr[:, 0:512], pr[0][:])
                fw.copy("act", yr[:, 512:1024], pr[1][:])
                emit_ret_state(fw, K, kst, v[:], S[:, 2, :], S[:, 3, :], None, None, Aret, tokp, "b" if d else "f")
                if d == 0:
                    fw.dma(D["yf"][ch, :, 0:1024], y[:])
                    fw.dma(D["yf"][ch, :, 1024:2048], yr[:])
                    continue
                yf = yfp.get()
                fw.dma(yf[:], D["yf"][ch])
                sz = tokp.get(); fw.dma(sz[:], D["o_z"][ch])
                sg = tokp.get(); fw.dma(sg[:], D["o_g"][ch])
                fw.tt("dve", y[:], y[:], yf[:, 0:1024], ALU.add)
                tq = tokp.get()
                fw.tt("pool", h16(tq[:]), h16(xs[:]), dsk[:].unsq(2).bc([128, 16, 64]), ALU.mult)
                fw.tt("pool", y[:], y[:], tq[:], ALU.add)
                fw.tt("dve", y[:], y[:], sz[:], ALU.mult)
                fw.act(tq[:], y[:], AF.Square)
                st_ = smallp.get()
                fw.rsum("dve", st_[:, 0:4], h4(tq[:]))
                emit_rstd(fw, st_[:, 0:4], st_[:, 0:4], 1.0 / 256, RMS_EPS)
                fw.tt("dve", h4(y[:]), h4(y[:]), st_[:, 0:4].unsq(2).bc([128, 4, 256]), ALU.mult)
                fw.tt("pool", y[:], y[:], nws[:], ALU.mult)
                fw.tt("dve", yr[:], yr[:], yf[:, 1024:2048], ALU.add)
                fw.rsum("dve", st_[:, 8:12], h4(yr[:]))
                fw.ts("dve", st_[:, 8:12], st_[:, 8:12], -1.0 / 256, ALU.mult)
                fw.tt("dve", h4(yr[:]), h4(yr[:]), st_[:, 8:12].unsq(2).bc([128, 4, 256]), ALU.add)
                fw.act(tq[:], yr[:], AF.Square)
                fw.rsum("dve", st_[:, 12:16], h4(tq[:]))
                emit_rstd(fw, st_[:, 12:16], st_[:, 12:16], 1.0 / 256, LN_EPS)
                fw.tt("dve", h4(yr[:]), h4(yr[:]), st_[:, 12:16].unsq(2).bc([128, 4, 256]), ALU.mult)
                fw.tt("pool", yr[:], yr[:], sg[:], ALU.mult)
                ycT = ycp.get()
                for kt in range(16):
                    src = y if kt < 8 else yr
                    pt = K["ps"].get()
                    fw.tr(pt[:, 0:128], src[:, 128 * (kt % 8):128 * (kt % 8) + 128], C.ident)
                    fw.copy("act" if kt % 2 else "dve", ycT[:, kt, :], pt[:, 0:128])
                fw.dma(D["ycT"][:, :, 128 * ch:128 * ch + 128], ycT[:])


def emit_tail(fw, D, nk_in, last):
    K = {"ps": Pool(fw, [128, 512], 8, psum=True, name="ps"), "t512": Pool(fw, [128, 512], 4, name="t512")}
    K["c"] = Consts(fw, D["consts"])
    mod = fw.sb([128, 48, 2], "mod"); fw.dma(mod[:], D["mod_out"][:])
    gsf = emit_gs(fw, mod, D["nwf"], 32, "gsf")
    shf = mod[:, 24:32, :]
    if last:
        gfin = fw.sb([128, 8, 1], "gfin"); fw.dma(gfin[:, :, 0], D["nwfin"][:])
    ycat = fw.sb([128, nk_in, 512], "ycat", r32=True)
    x1 = fw.sb([128, 8, 512], "x1")
    h2 = fw.sb([128, 8, 512], "h2", r32=True)
    x2 = fw.sb([128, 8, 512], "x2")
    uT = fw.sb([128, 22, 512], "uT", r32=True)
    wop = Pool(fw, [128, nk_in, 128], 1, name="wo", r32=True)
    w13 = Pool(fw, [128, 8, 128], 4, name="w13", r32=True)
    w2p = Pool(fw, [128, 22, 128], 1, name="w2", r32=True)
    for si, (ch0, nch, ncol) in enumerate(SEGS):
        if last and si == 1:
            continue
        for c0 in range(0, nch * 128, 512):
            W = min(512, nch * 128 - c0)
            fw.dma(ycat[:, :, 0:W], D["ycT"][:, :, 128 * ch0 + c0:128 * ch0 + c0 + W])
            fw.dma(x1[:, :, 0:W], D["xT%d" % si][:, :, 2 + c0:2 + c0 + W])
            for ft in range(8):
                w = wop.get()
                fw.dma(w[:], D["w_out"][ft])
                p = K["ps"].get()
                for kt in range(nk_in):
                    fw.mm(p[:, 0:W], w[:, kt, :], ycat[:, kt, 0:W], kt == 0, kt == nk_in - 1)
                fw.stt("dve", x1[:, ft, 0:W], p[:, 0:W], mod[:, 16 + ft, si:si + 1], x1[:, ft, 0:W], ALU.mult, ALU.add)
            emit_norm(fw, K, x1, h2, 0, W, gsf, shf, si)
            for ft in range(22):
                wa = w13.get(); fw.dma(wa[:], D["w1"][ft])
                wb = w13.get(); fw.dma(wb[:], D["w3"][ft])
                pa = K["ps"].get(); pb = K["ps"].get()
                for kt in range(8):
                    fw.mm(pa[:, 0:W], wa[:, kt, :], h2[:, kt, 0:W], kt == 0, kt == 7)
                for kt in range(8):
                    fw.mm(pb[:, 0:W], wb[:, kt, :], h2[:, kt, 0:W], kt == 0, kt == 7)
                sa = K["t512"].get()
                fw.act(sa[:, 0:W], pa[:, 0:W], AF.Silu)
                fw.tt("dve", uT[:, ft, 0:W], sa[:, 0:W], pb[:, 0:W], ALU.mult)
            for f2 in range(8):
                w = w2p.get()
                fw.dma(w[:], D["w2"][f2])
                p = K["ps"].get()
                for ft in range(22):
                    fw.mm(p[:, 0:W], w[:, ft, :], uT[:, ft, 0:W], ft == 0, ft == 21)
                fw.stt("dve", x2[:, f2, 0:W], p[:, 0:W], mod[:, 40 + f2, si:si + 1], x1[:, f2, 0:W], ALU.mult, ALU.add)
            if last:
                emit_norm(fw, K, x2, x1, 0, W, gfin, None, 0)
                fw.dma(D["xo%d" % si][:, :, c0:c0 + W], x1[:, :, 0:W])
            else:
                fw.dma(D["xo%d" % si][:, :, c0:c0 + W], x2[:, :, 0:W])


def fm(x2d):
    T, F = x2d.shape
    return np.ascontiguousarray(x2d.T.reshape(F // 128, 128, T).transpose(1, 0, 2))


def wblk(W, bw):
    K_, N = W.shape
    return np.ascontiguousarray(W.reshape(K_ // 128, 128, N // bw, bw).transpose(2, 1, 0, 3))


def colvec(v):
    return np.ascontiguousarray(v.reshape(-1, 128).T)


def rep(v):
    v = np.asarray(v, np.float32).reshape(-1)
    return np.ascontiguousarray(np.broadcast_to(v[None, :], (128, v.size)))


def run_prog(phases, ins, outs, scratch, in_maps):
    nc = bass.Bass("TRN2", target_bir_lowering=False)
    with ExitStack() as top:
        fw = FW(nc, top)
        D = {}
        for name, shape in ins.items():
            D[name] = fw.dram(name, shape, kind="ExternalInput")
        for name, shape in outs.items():
            D[name] = fw.dram(name, shape, kind="ExternalOutput")
        for name, shape in scratch.items():
            D[name] = fw.dram(name, shape, kind="Internal")
        for ph in phases:
            with ExitStack() as st:
                fw.stack = st
                ph(fw, D)
                fw.flush()
    maps = [{k: np.ascontiguousarray(m[k], dtype=np.float32) for k in ins} for m in in_maps]
    for m in maps:
        for k, shape in ins.items():
            assert tuple(m[k].shape) == tuple(shape), (k, m[k].shape, shape)
    res = run_bass_kernel_spmd(nc, maps, core_ids=list(range(NCORE)))
    return res.results


def seg_inputs(lat, cx):
    out = []
    for r in range(NCORE):
        b, s = divmod(r, 4)
        x0 = np.zeros((2052, 1024), np.float32)
        lo, hi = s * 2048 - 2, s * 2048 + 2050
        a, e = max(lo, 0), min(hi, 8192)
        x0[a - lo:e - lo] = lat[b, a:e]
        x1 = np.zeros((132, 1024), np.float32)
        hm = np.zeros(4, np.float32)
        hm[0] = float(s > 0)
        hm[1] = float(s < 3)
        if s < 2:
            lo, hi = s * 128 - 2, s * 128 + 130
            a, e = max(lo, 0), min(hi, 256)
            x1[a - lo:e - lo] = cx[b, a:e]
            hm[2] = float(s > 0)
            hm[3] = float(s < 1)
        out.append({"xT0": fm(x0), "xT1": fm(x1), "hm": rep(hm)})
    return out


def rope_tables():
    r = np.repeat(np.arange(128, dtype=np.float32), 64)
    col = np.tile(np.arange(64, dtype=np.float32), 128)
    nf = 32
    inv = (np.float32(10000.0) ** (-np.arange(nf, dtype=np.float32) / nf)).astype(np.float32)
    ang = np.concatenate([r[:, None] * inv, col[:, None] * inv], -1).astype(np.float32)
    return np.concatenate([np.cos(ang), np.sin(ang)], -1).astype(np.float32)


def prefix_lists(tot, totA, nk, a_half):
    outF, outA = [], []
    for r in range(NCORE):
        b, s = divmod(r, 4)
        base = 4 * b
        F = np.zeros((2, 5) + tot[0].shape[1:], np.float32)
        A = np.ones((128, 2, 5, totA[0].shape[-1]), np.float32)
        fwd0 = [(base + 0, 1), (base + 1, 1)] + [(base + k, 0) for k in range(3)]
        use_f0 = [True, True] + [k < s for k in range(3)]
        bwd0 = [(base + 1, 1), (base + 0, 1)] + [(base + k, 0) for k in (3, 2, 1)]
        use_b0 = [True, True] + [k > s for k in (3, 2, 1)]
        fwd1 = [(base + 0, 1)]
        use_f1 = [s == 1]
        bwd1 = [(base + 1, 1)]
        use_b1 = [s == 0]
        for si, (fl, uf, bl, ub) in enumerate(((fwd0, use_f0, bwd0, use_b0), (fwd1, use_f1, bwd1, use_b1))):
            if si == 1 and s >= 2:
                continue
            for e, ((cr, cs_), u) in enumerate(zip(fl, uf)):
                if u:
                    for k in FWD_KINDS[nk]:
                        F[si, e, k] = tot[cr][cs_, k]
                    A[:, si, e, 0:a_half] = totA[cr][cs_][:, 0:a_half]
            for e, ((cr, cs_), u) in enumerate(zip(bl, ub)):
                if u:
                    for k in BWD_KINDS[nk]:
                        F[si, e, k] = tot[cr][cs_, k]
                    A[:, si, e, a_half:2 * a_half] = totA[cr][cs_][:, a_half:2 * a_half]
        outF.append(F)
        outA.append(A)
    return outF, outA


FWD_KINDS = {4: (0, 2)}
BWD_KINDS = {4: (1, 3)}

SCR0 = {"o_z": [NCH, 128, 1024], "o_g": [NCH, 128, 1024], "o_v": [NCH, 128, 1024], "o_qk": [NCH, 128, 16, 128],
        "o_kst": [NCH, 128, 1024], "o_dl": [NCH, 128, 64], "o_cb": [NCH, 128, 8, 128], "o_xs": [NCH, 128, 1024],
        "o_bm": [NCH, 128, 512]}


def layer0(inp, lat, cx, mods_only=False):
    consts = host_consts()
    j = np.arange(128, dtype=np.float32)
    iota = np.stack([j, 127 - j], 1).astype(np.float32)
    di = np.arange(128)[None, :] - np.arange(128)[:, None]
    diff = np.ascontiguousarray(np.stack([np.maximum(di, 0), np.maximum(-di, 0)], 1).astype(np.float32))
    W = inp["ab_in_w"][0]
    w_xbc = wblk(W[:, 1024:3072], 128)
    w_dt = np.ascontiguousarray(wblk(W[:, 3072:3088], 16)[0])
    w_tm = wblk(np.concatenate([W[:, 0:1024], W[:, 3088:]], 1), 256)
    cwl = np.ascontiguousarray(inp["ab_conv_w"][0].T.reshape(16, 128, 5).transpose(1, 0, 2))
    small = np.concatenate([rep(inp["ssd_dt_bias_f"][0]), rep(inp["ssd_dt_bias_b"][0]), rep(inp["ssd_a_log_f"][0]),
                            rep(inp["ssd_a_log_b"][0]), rep(inp["ret_logit_f"][0]), rep(inp["ret_logit_b"][0])], 1)
    rt = rope_tables()
    ones_cs = np.concatenate([np.ones((128, 64), np.float32), np.zeros((128, 64), np.float32)], 1)[None]
    segs = seg_inputs(lat, cx)
    shared = {"consts": consts, "adaw": wblk(inp["ada_w"][0], 128), "adab": colvec(inp["ada_b"][0]),
              "nw": colvec(inp["norm_mix_w"][0]), "cw": cwl, "cb": colvec(inp["ab_conv_b"][0]), "w_dt": w_dt,
              "small": small, "iota": iota, "w_xbc": w_xbc, "w_tm": w_tm, "cs1": ones_cs}
    maps = []
    for r in range(NCORE):
        b, s = divmod(r, 4)
        m = dict(shared)
        m.update(segs[r])
        m["cvec"] = np.ascontiguousarray(np.stack([colvec(inp["c"][b]), colvec(inp["c_ctx"])], -1))
        m["cs0"] = np.ascontiguousarray(rt[s * 2048:(s + 1) * 2048].reshape(16, 128, 128))
        maps.append(m)
    ins = {k: list(v.shape) for k, v in maps[0].items()}
    tot_shapes = {"mod_out": [128, 48, 2], "o_tot": [2, 4, 128, 1024], "o_totA": [2, 128, 40]}
    res1 = run_prog([l0p1], ins, dict(tot_shapes), dict(SCR0), maps)
    preF, preA = prefix_lists([r_["o_tot"] for r_ in res1], [r_["o_totA"] for r_ in res1], 4, 20)
    shared3 = {"dskip": rep(inp["ssd_d"][0]), "nws": rep(inp["ssd_norm_w"][0]),
               "diff": diff, "nwf": colvec(inp["norm_ffn_w"][0]), "w_out": wblk(inp["ab_out_w"][0], 128),
               "w1": wblk(inp["ffn_w1"][0], 128), "w3": wblk(inp["ffn_w3"][0], 128), "w2": wblk(inp["ffn_w2"][0], 128)}
    maps3 = []
    for r in range(NCORE):
        m = dict(maps[r])
        m.update(shared3)
        m["pre_F"] = preF[r]
        m["pre_A"] = preA[r]
        maps3.append(m)
    ins3 = {k: list(v.shape) for k, v in maps3[0].items()}
    outs3 = {"xo0": [128, 8, 2048], "xo1": [128, 8, 128]}
    scr3 = {"yf": [NCH, 128, 2048], "ycT": [128, 16, NCH * 128]}
    scr3.update(SCR0)
    scr3.update(tot_shapes)
    res3 = run_prog([lambda fw, D: l0p1(fw, D, False), l0p3_sweeps, lambda fw, D: emit_tail(fw, D, 16, False)], ins3, outs3, scr3, maps3)
    lat1 = np.zeros_like(lat)
    cx1 = np.zeros_like(cx)
    for r in range(NCORE):
        b, s = divmod(r, 4)
        lat1[b, s * 2048:(s + 1) * 2048] = res3[r]["xo0"].transpose(2, 1, 0).reshape(2048, 1024)
        if s < 2:
            cx1[b, s * 128:(s + 1) * 128] = res3[r]["xo1"].transpose(2, 1, 0).reshape(128, 1024)
    return lat1, cx1


MS = float(512.0 ** -0.5)
SCR1 = {"o_qT": [NCH, 128, 16, 128], "o_kT": [NCH, 128, 16, 128], "o_ktok": [NCH, 128, 2048],
        "o_vtok": [NCH, 128, 2048], "o_gsc": [NCH, 128, 32], "o_xc": [NCH, 128, 16, 128], "o_sz": [NCH, 128, 16, 128]}


def l1p1a(fw, D):
    K = {"ps": Pool(fw, [128, 512], 8, psum=True, name="ps"), "t512": Pool(fw, [128, 512], 4, name="t512")}
    K["c"] = C = Consts(fw, D["consts"])
    mod = emit_mods(fw, K, D["cvec"], D["adaw"], D["adab"], None, None, D["mod_out"])
    gs = emit_gs(fw, mod, D["nw"], 8, "gs")
    sh = mod[:, 0:8, :]
    hm = fw.sb([128, 4], "hm"); fw.dma(hm[:], D["hm"][:])
    cw = fw.sb([128, 16, 5], "cw"); fw.dma(cw[:], D["cw"][:])
    cb = fw.sb([128, 16], "cb"); fw.dma(cb[:], D["cb"][:])
    wbd = fw.sb([128, 3, 16, 128], "wbd"); fw.dma(wbd[:], D["wbd"][:])
    gw = fw.sb([128, 48, 16], "gw"); fw.dma(gw[:], D["gw"][:])
    gb = fw.sb([128, 16], "gb"); fw.dma(gb[:], D["gb"][:])
    xg = fw.sb([128, 8, 128 * GC1 + 4], "xg")
    hT = fw.sb([128, 8, 128 * GC1 + 4], "hT", r32=True)
    xmT_g = fw.sb([128, 16, 128 * GC1], "xmT")
    xcT_g = fw.sb([128, 16, 128 * GC1], "xcT")
    szT_g = fw.sb([128, 16, 128 * GC1], "szT")
    wup = Pool(fw, [128, 8, 128], 3, name="wup", r32=True)
    cacc = Pool(fw, [128, 128], 3, name="cacc")
    qkvp = Pool(fw, [128, 16, 384], 1, name="qkv")
    kTsp = Pool(fw, [128, 16, 128], 2, name="kTs")
    tk = Pool(fw, [128, 2048], 3, name="tk")
    smallp = Pool(fw, [128, 32], 10, name="sm")
    for si, (ch0, nch, ncol) in enumerate(SEGS):
        xT_d = D["xT%d" % si]
        ngrp = (nch + GC1 - 1) // GC1
        for g in range(ngrp):
          gch = min(GC1, nch - GC1 * g)
          W = 128 * gch
          c0 = 128 * GC1 * g
          fw.dma(xg[:, :, 0:W + 4], xT_d[:, :, c0:c0 + W + 4])
          emit_norm(fw, K, xg, hT, 0, W + 4, gs, sh, si)
          if g == 0:
              fw.ts("dve", hT[:, :, 0:2], hT[:, :, 0:2], hm[:, 2 * si:2 * si + 1], ALU.mult)
          if g == ngrp - 1:
              fw.ts("dve", hT[:, :, W + 2:W + 4], hT[:, :, W + 2:W + 4], hm[:, 2 * si + 1:2 * si + 2], ALU.mult)
          for ft in range(16):
              w = wup.get()
              fw.dma(w[:], D["w_xm"][ft])
              p = K["ps"].get()
              for kt in range(8):
                  fw.mm(p[:, 0:W + 4], w[:, kt, :], hT[:, kt, 0:W + 4], kt == 0, kt == 7)
              fw.copy("act", xmT_g[:, ft, 0:W], p[:, 2:W + 2])
              for j in range(gch):
                  acc = cacc.get()
                  fw.ts("dve", acc[:], p[:, 128 * j:128 * j + 128], cw[:, ft, 0:1], ALU.mult)
                  for k in range(1, 5):
                      fw.stt("dve", acc[:], p[:, 128 * j + k:128 * j + k + 128], cw[:, ft, k:k + 1], acc[:], ALU.mult, ALU.add)
                  fw.act(xcT_g[:, ft, 128 * j:128 * j + 128], acc[:], AF.Silu, bias=cb[:, ft:ft + 1])
          for ft in range(16):
              w = wup.get()
              fw.dma(w[:], D["w_z"][ft])
              p = K["ps"].get()
              for kt in range(8):
                  fw.mm(p[:, 0:W], w[:, kt, :], hT[:, kt, 2:W + 2], kt == 0, kt == 7)
              fw.act(szT_g[:, ft, 0:W], p[:, 0:W], AF.Silu)
          for j in range(gch):
            ch = ch0 + GC1 * g + j
            xmT = xmT_g[:, :, 128 * j:128 * j + 128]
            xcT = xcT_g[:, :, 128 * j:128 * j + 128]
            szT = szT_g[:, :, 128 * j:128 * j + 128]
            fw.dma(D["o_xc"][ch], xcT)
            fw.dma(D["o_sz"][ch], szT)
            qkv = qkvp.get()
            for ft in range(16):
                p = K["ps"].get()
                fw.mm(p[:, 0:128], wbd[:, 0, ft, :], xcT[:, ft, :])
                fw.mm(p[:, 128:256], wbd[:, 1, ft, :], xcT[:, ft, :])
                fw.mm(p[:, 256:384], wbd[:, 2, ft, :], xmT[:, ft, :])
                fw.copy("act" if ft % 2 else "dve", qkv[:, ft, :], p[:, 0:384])
            kTs = kTsp.get()
            fw.act(kTs[:], qkv[:, :, 128:256], AF.Copy, scale=MS)
            fw.dma(D["o_qT"][ch], qkv[:, :, 0:128])
            fw.dma(D["o_kT"][ch], kTs[:])
            ktok = tk.get(); vtok = tk.get()
            for g4 in range(4):
                pk = K["ps"].get(); pv = K["ps"].get()
                for ff in range(4):
                    ft = 4 * g4 + ff
                    fw.mm(pk[:, 128 * ff:128 * ff + 128], xcT[:, ft, :], wbd[:, 1, ft, :])
                    fw.mm(pv[:, 128 * ff:128 * ff + 128], xmT[:, ft, :], wbd[:, 2, ft, :])
                fw.act(ktok[:, 512 * g4:512 * g4 + 512], pk[:], AF.Copy, scale=MS)
                fw.copy("dve", vtok[:, 512 * g4:512 * g4 + 512], pv[:])
            fw.dma(D["o_ktok"][ch], ktok[:])
            fw.dma(D["o_vtok"][ch], vtok[:])
            pg = K["ps"].get()
            for idx in range(48):
                t_, ft = divmod(idx, 16)
                fw.mm(pg[:, 0:16], qkv[:, ft, 128 * t_:128 * t_ + 128], gw[:, idx, :], idx == 0, idx == 47)
            gt = smallp.get()
            fw.tt("dve", gt[:, 0:16], pg[:, 0:16], gb[:], ALU.add)
            lf = smallp.get()
            fw.copy("dve", lf[:, 0:4], gt[:, 4:8])
            fw.copy("dve", lf[:, 4:8], gt[:, 12:16])
            fw.act(lf[:, 0:8], lf[:, 0:8], AF.Exp, scale=-1.0)
            fw.act(lf[:, 0:8], lf[:, 0:8], AF.Ln, bias=1.0)
            fw.ts("dve", lf[:, 0:8], lf[:, 0:8], -1.0, ALU.mult)
            pf = K["ps"].get()
            fw.mm(pf[:, 0:4], C.m_le, lf[:, 0:4])
            fw.mm(pf[:, 4:8], C.m_ge, lf[:, 4:8])
            fw.mm(pf[:, 8:16], C.ones, lf[:, 0:8])
            gsc = smallp.get()
            fw.copy("dve", gsc[:, 8:16], pf[:, 0:8])
            fw.copy("dve", gsc[:, 24:32], pf[:, 8:16])
            fw.tt("dve", gsc[:, 0:4], gt[:, 0:4], gsc[:, 8:12], ALU.subtract)
            fw.tt("dve", gsc[:, 4:8], gt[:, 8:12], gsc[:, 12:16], ALU.subtract)
            emit_colmax(fw, K, C, gsc[:, 0:8], gsc[:, 16:24], smallp)
            fw.dma(D["o_gsc"][ch], gsc[:])


def emit_colmax(fw, K, C, a, out, smallp):
    pt = K["ps"].get()
    fw.tr(pt[0:8, 0:128], a, C.ident)
    am = smallp.get()
    fw.rmax("dve", am[0:8, 0:1], pt[0:8, 0:128])
    d8 = smallp.get()
    fw.ts("dve", d8[0:8, 0:8], C.ident[0:8, 0:8], am[0:8, 0:1], ALU.mult)
    pb = K["ps"].get()
    fw.mm(pb[:, 0:8], C.ones[0:8, :], d8[0:8, 0:8])
    fw.copy("dve", out, pb[:, 0:8])


def emit_mstate(fw, K, C, ktok, vtok, u, Cst, nst, h, ucol, kpp, pn):
    kp = kpp.get()
    fw.ts("dve", kp[:], ktok[:, 512 * h:512 * h + 512], u[:, ucol:ucol + 1], ALU.mult)
    for dt in range(4):
        pF = K["ps"].get()
        fw.mm(pF[:], kp[:, 128 * dt:128 * dt + 128], vtok[:, 512 * h:512 * h + 512])
        fw.mm(pn[:, dt:dt + 1], kp[:, 128 * dt:128 * dt + 128], C.ones[:, 0:1])
        fw.tt("dve", Cst[:, h, dt, :], Cst[:, h, dt, :], pF[:], ALU.add)
    fw.tt("dve", nst[:, h, :], nst[:, h, :], pn[:, 0:4], ALU.add)


def l1p1b(fw, D):
    K = {"ps": Pool(fw, [128, 512], 6, psum=True, name="ps")}
    K["c"] = C = Consts(fw, D["consts"])
    pn = fw.ps([128, 512], "pn")
    Cst = [fw.sb([128, 4, 4, 512], "Ctot%d" % d) for d in range(2)]
    nst = [fw.sb([128, 4, 4], "ntot%d" % d) for d in range(2)]
    sc = fw.sb([128, 16], "msc")
    tk = Pool(fw, [128, 2048], 4, name="tk", r32=True)
    kpp = Pool(fw, [128, 512], 3, name="kp", r32=True)
    smallp = Pool(fw, [128, 32], 10, name="sm")
    for si, (ch0, nch, ncol) in enumerate(SEGS):
        for d in range(2):
            fw.memset("pool", Cst[d][:], 0.0)
            fw.memset("pool", nst[d][:], 0.0)
        fw.memset("pool", sc[:], 0.0)
        for lc in range(nch):
            ch = ch0 + lc
            ktok = tk.get(); fw.dma(ktok[:], D["o_ktok"][ch])
            vtok = tk.get(); fw.dma(vtok[:], D["o_vtok"][ch])
            gsc = smallp.get(); fw.dma(gsc[:], D["o_gsc"][ch])
            w = smallp.get()
            fw.tt("dve", w[:, 0:4], sc[:, 0:4], gsc[:, 16:20], ALU.max)
            fw.tt("dve", w[:, 8:12], sc[:, 0:4], w[:, 0:4], ALU.subtract)
            fw.tt("dve", w[:, 16:20], gsc[:, 16:20], w[:, 0:4], ALU.subtract)
            fw.tt("dve", w[:, 24:28], sc[:, 12:16], gsc[:, 20:24], ALU.add)
            fw.tt("dve", w[:, 24:28], w[:, 24:28], gsc[:, 28:32], ALU.add)
            fw.tt("dve", w[:, 4:8], sc[:, 4:8], w[:, 24:28], ALU.max)
            fw.tt("dve", w[:, 12:16], sc[:, 4:8], w[:, 4:8], ALU.subtract)
            fw.tt("dve", w[:, 20:24], w[:, 24:28], w[:, 4:8], ALU.subtract)
            fw.act(w[:, 8:24], w[:, 8:24], AF.Exp)
            fw.tt("dve", sc[:, 0:4], gsc[:, 24:28], w[:, 0:4], ALU.add)
            fw.copy("dve", sc[:, 4:8], w[:, 4:8])
            fw.tt("dve", sc[:, 8:12], sc[:, 8:12], gsc[:, 24:28], ALU.add)
            fw.tt("dve", sc[:, 12:16], sc[:, 12:16], gsc[:, 28:32], ALU.add)
            u = smallp.get()
            fw.tt("dve", u[:, 0:8], gsc[:, 0:8], gsc[:, 16:24], ALU.subtract)
            fw.act(u[:, 0:8], u[:, 0:8], AF.Exp)
            fw.tt("dve", u[:, 0:8], u[:, 0:8], w[:, 16:24], ALU.mult)
            for d in range(2):
                for h in range(4):
                    fw.ts("pool", Cst[d][:, h].re("p a b -> p (a b)"), Cst[d][:, h].re("p a b -> p (a b)"),
                          w[:, 8 + 4 * d + h:9 + 4 * d + h], ALU.mult)
                    fw.ts("dve", nst[d][:, h, :], nst[d][:, h, :], w[:, 8 + 4 * d + h:9 + 4 * d + h], ALU.mult)
                    emit_mstate(fw, K, C, ktok, vtok, u, Cst[d], nst[d], h, 4 * d + h, kpp, pn)
        for d in range(2):
            for h in range(4):
                fw.dma(D["o_totC"][si, d, h], Cst[d][:, h].re("p a b -> p (a b)"))
            fw.dma(D["o_totn"][si, d], nst[d][:].re("p a b -> p (a b)"))
        fw.dma(D["o_totm"][si], sc[:])


def l1p3_sweeps(fw, D):
    K = {"ps": Pool(fw, [128, 512], 5, psum=True, name="ps")}
    K["c"] = C = Consts(fw, D["consts"])
    pnum = fw.ps([128, 512], "pnum")
    pden = fw.ps([128, 512], "pden")
    pn = fw.ps([128, 512], "pn")
    nwm = fw.sb([128, 16], "nwm"); fw.dma(nwm[:], D["nwm"][:])
    skp = fw.sb([128, 16], "skip"); fw.dma(skp[:], D["skip"][:])
    preM = fw.sb([128, 2, 5, 8], "preM"); fw.dma(preM[:], D["pre_M"][:])
    preN = fw.sb([128, 2, 5, 16], "preN"); fw.dma(preN[:], D["pre_n"][:])
    Cst = fw.sb([128, 4, 4, 512], "C")
    nst = fw.sb([128, 4, 4], "n")
    mst = fw.sb([128, 4], "m")
    ld = {n: Pool(fw, sh, 2, name="ld_" + n, r32=(n == "vtok")) for n, sh in
          (("qT", [128, 16, 128]), ("kT", [128, 16, 128]), ("ktok", [128, 2048]), ("vtok", [128, 2048]), ("gsc", [128, 32]))}
    tk = Pool(fw, [128, 2048], 3, name="tk")
    fmp = Pool(fw, [128, 16, 128], 3, name="fm")
    kpp = Pool(fw, [128, 512], 3, name="kp", r32=True)
    t128 = Pool(fw, [128, 128], 4, name="t128", r32=True)
    smallp = Pool(fw, [128, 32], 12, name="sm")
    si, (ch0, nch, ncol) = 0, SEGS[0]
    flat = lambda v: v.re("p a b -> p (a b)")
    for d in range(2):
        fw.memset("pool", Cst[:], 0.0)
        fw.memset("pool", nst[:], 0.0)
        fw.memset("pool", mst[:], 0.0)
        for e in range(5):
            w = smallp.get()
            fw.tt("dve", w[:, 0:4], mst[:], preM[:, d, e, 4:8], ALU.add)
            fw.tt("dve", w[:, 4:8], w[:, 0:4], preM[:, d, e, 0:4], ALU.max)
            fw.tt("dve", w[:, 8:12], w[:, 0:4], w[:, 4:8], ALU.subtract)
            fw.tt("dve", w[:, 12:16], preM[:, d, e, 0:4], w[:, 4:8], ALU.subtract)
            fw.act(w[:, 8:16], w[:, 8:16], AF.Exp)
            fw.copy("dve", mst[:], w[:, 4:8])
            for h in range(4):
                Fb = tk.get()
                fw.dma(Fb[:], D["pre_C"][d, e, h])
                fw.ts("dve", flat(Cst[:, h]), flat(Cst[:, h]), w[:, 8 + h:9 + h], ALU.mult)
                fw.stt("dve", flat(Cst[:, h]), Fb[:], w[:, 12 + h:13 + h], flat(Cst[:, h]), ALU.mult, ALU.add)
                fw.ts("dve", nst[:, h, :], nst[:, h, :], w[:, 8 + h:9 + h], ALU.mult)
                fw.stt("dve", nst[:, h, :], preN[:, d, e, 4 * h:4 * h + 4], w[:, 12 + h:13 + h], nst[:, h, :], ALU.mult, ALU.add)
        order = range(nch) if d == 0 else range(nch - 1, -1, -1)
        for lc in order:
            ch = ch0 + lc
            t = {}
            for n, src in (("qT", "o_qT"), ("kT", "o_kT"), ("ktok", "o_ktok"), ("vtok", "o_vtok"), ("gsc", "o_gsc")):
                t[n] = ld[n].get()
                fw.dma(t[n][:], D[src][ch])
            qT, kT, ktok, vtok, gsc = (t[n] for n in ("qT", "kT", "ktok", "vtok", "gsc"))
            w = smallp.get()
            fw.tt("dve", w[:, 0:4], mst[:], gsc[:, 16 + 4 * d:20 + 4 * d], ALU.max)
            fw.tt("dve", w[:, 4:8], mst[:], w[:, 0:4], ALU.subtract)
            fw.tt("dve", w[:, 8:12], gsc[:, 8 + 4 * d:12 + 4 * d], w[:, 0:4], ALU.add)
            fw.ts("dve", w[:, 8:12], w[:, 8:12], -1.0, ALU.mult)
            fw.tt("dve", w[:, 12:16], gsc[:, 4 * d:4 * d + 4], w[:, 0:4], ALU.subtract)
            fw.act(w[:, 4:16], w[:, 4:16], AF.Exp)
            fw.tt("dve", mst[:], gsc[:, 24 + 4 * d:28 + 4 * d], w[:, 0:4], ALU.add)
            hd = tk.get()
            for h in range(4):
                fw.ts("pool", flat(Cst[:, h]), flat(Cst[:, h]), w[:, 4 + h:5 + h], ALU.mult)
                fw.ts("dve", nst[:, h, :], nst[:, h, :], w[:, 4 + h:5 + h], ALU.mult)
                p = K["ps"].get()
                for dt in range(4):
                    fw.mm(p[:, 0:128], kT[:, 4 * h + dt, :], qT[:, 4 * h + dt, :], dt == 0, dt == 3)
                P = t128.get()
                fw.stt("dve", P[:], p[:, 0:128], w[:, 12 + h:13 + h], C.m_ge if d else C.m_le, ALU.mult, ALU.mult)
                fw.mm(pnum[:], P[:], vtok[:, 512 * h:512 * h + 512], True, False)
                for dt in range(4):
                    fw.mm(pnum[:], qT[:, 4 * h + dt, :], Cst[:, h, dt, :], False, dt == 3)
                fw.mm(pden[:, 0:1], P[:], C.ones[:, 0:1], True, False)
                for dt in range(4):
                    fw.mm(pden[:, 0:1], qT[:, 4 * h + dt, :], nst[:, h, dt:dt + 1], False, dt == 3)
                rr = smallp.get()
                fw.ts("dve", rr[:, 2:3], pden[:, 0:1], -1.0, ALU.mult)
                fw.tt("dve", rr[:, 0:1], rr[:, 2:3], pden[:, 0:1], ALU.max)
                fw.tt("dve", rr[:, 0:1], rr[:, 0:1], w[:, 8 + h:9 + h], ALU.max)
                fw.op("dve", (lambda o, i: (lambda e: e.reciprocal(o, i)))(rr[:, 1:2].ap, rr[:, 0:1].ap), [rr], [rr])
                fw.act(hd[:, 512 * h:512 * h + 512], pnum[:], AF.Copy, scale=rr[:, 1:2])
                emit_mstate(fw, K, C, ktok, vtok, w, Cst, nst, h, 12 + h, kpp, pn)
            if d == 0:
                fw.dma(D["hf"][ch], hd[:])
                continue
            hf = tk.get()
            fw.dma(hf[:], D["hf"][ch])
            fw.tt("pool", hd[:], hd[:], hf[:], ALU.add)
            h4 = lambda v: v.re("p (h e) -> p h e", h=4)
            st_ = smallp.get()
            fw.rsum("dve", st_[:, 0:4], h4(hd[:]))
            fw.ts("dve", st_[:, 0:4], st_[:, 0:4], -1.0 / 512, ALU.mult)
            fw.tt("dve", h4(hd[:]), h4(hd[:]), st_[:, 0:4].unsq(2).bc([128, 4, 512]), ALU.add)
            fw.act(hf[:], hd[:], AF.Square)
            fw.rsum("dve", st_[:, 4:8], h4(hf[:]))
            emit_rstd(fw, st_[:, 4:8], st_[:, 4:8], 1.0 / 512, LN_EPS)
            fw.tt("dve", h4(hd[:]), h4(hd[:]), st_[:, 4:8].unsq(2).bc([128, 4, 512]), ALU.mult)
            hnT = fmp.get()
            for ft in range(16):
                pt = K["ps"].get()
                fw.tr(pt[:, 0:128], hd[:, 128 * ft:128 * ft + 128], C.ident)
                fw.copy("act" if ft % 2 else "dve", hnT[:, ft, :], pt[:, 0:128])
            xc = fmp.get(); fw.dma(xc[:], D["o_xc"][ch])
            sz = fmp.get(); fw.dma(sz[:], D["o_sz"][ch])
            fw.tt("dve", hnT[:], hnT[:], nwm[:].unsq(2).bc([128, 16, 128]), ALU.mult)
            fw.tt("pool", xc[:], xc[:], skp[:].unsq(2).bc([128, 16, 128]), ALU.mult)
            fw.tt("pool", hnT[:], hnT[:], xc[:], ALU.add)
            fw.tt("dve", hnT[:], hnT[:], sz[:], ALU.mult)
            fw.dma(D["ycT"][:, :, 128 * ch:128 * ch + 128], hnT[:])


def layer1(inp, lat1, cx1):
    consts = host_consts()
    segs = seg_inputs(lat1, cx1)
    up = inp["ml_up_w"][0]
    bd = []
    for wn in ("ml_wq", "ml_wk", "ml_wv"):
        w = inp[wn][0]
        t = np.zeros((16, 128, 128), np.float32)
        for b in range(512):
            ft, o = divmod(4 * b, 128)
            t[ft, o:o + 4, o:o + 4] = w[b]
        bd.append(t.transpose(1, 0, 2))
    wbd = np.ascontiguousarray(np.stack(bd, 1))
    cwl = np.ascontiguousarray(inp["ml_conv_w"][0].T.reshape(16, 128, 5).transpose(1, 0, 2))
    shared = {"consts": consts, "adaw": wblk(inp["ada_w"][1], 128), "adab": colvec(inp["ada_b"][1]),
              "nw": colvec(inp["norm_mix_w"][1]), "cw": cwl, "cb": colvec(inp["ml_conv_b"][0]),
              "w_xm": wblk(up[:, 0:2048], 128), "w_z": wblk(up[:, 2048:4096], 128), "wbd": wbd,
              "gw": np.ascontiguousarray(inp["ml_gate_w"][0].reshape(48, 128, 16).transpose(1, 0, 2)),
              "gb": rep(inp["ml_gate_b"][0])}
    maps = []
    for r in range(NCORE):
        b, s = divmod(r, 4)
        m = dict(shared)
        m.update(segs[r])
        m["cvec"] = np.ascontiguousarray(np.stack([colvec(inp["c"][b]), colvec(inp["c_ctx"])], -1))
        maps.append(m)
    ins = {k: list(v.shape) for k, v in maps[0].items()}
    tot_shapes = {"mod_out": [128, 48, 2], "o_totC": [2, 2, 4, 128, 2048], "o_totn": [2, 2, 128, 16], "o_totm": [2, 128, 16]}
    res1 = run_prog([l1p1a, l1p1b], ins, dict(tot_shapes), dict(SCR1), maps)
    maps3 = []
    shared3 = {"nwm": colvec(inp["ml_norm_w"][0]), "skip": colvec(inp["ml_skip"][0]),
               "nwf": colvec(inp["norm_ffn_w"][1]), "w_out": wblk(inp["ml_down_w"][0], 128),
               "w1": wblk(inp["ffn_w1"][1], 128), "w3": wblk(inp["ffn_w3"][1], 128), "w2": wblk(inp["ffn_w2"][1], 128),
               "nwfin": colvec(inp["final_norm_w"])}
    for r in range(NCORE):
        b, s = divmod(r, 4)
        base = 4 * b
        pC = np.zeros((2, 5, 4, 128, 2048), np.float32)
        pn = np.zeros((128, 2, 5, 16), np.float32)
        pM = np.zeros((128, 2, 5, 8), np.float32)
        pM[:, :, :, 0:4] = -1e30
        fwd = [(base + 0, 1, True), (base + 1, 1, True)] + [(base + k, 0, k < s) for k in range(3)]
        bwd = [(base + 1, 1, True), (base + 0, 1, True)] + [(base + k, 0, k > s) for k in (3, 2, 1)]
        for d, lst in enumerate((fwd, bwd)):
            for e, (cr, cs_, u) in enumerate(lst):
                if not u:
                    continue
                pC[d, e] = res1[cr]["o_totC"][cs_, d]
                pn[:, d, e] = res1[cr]["o_totn"][cs_, d]
                tm = res1[cr]["o_totm"][cs_]
                pM[:, d, e, 0:4] = tm[:, 4 * d:4 * d + 4]
                pM[:, d, e, 4:8] = tm[:, 8 + 4 * d:12 + 4 * d]
        m = dict(maps[r])
        m.update(shared3)
        m["pre_C"] = pC
        m["pre_n"] = pn
        m["pre_M"] = pM
        maps3.append(m)
    ins3 = {k: list(v.shape) for k, v in maps3[0].items()}
    outs3 = {"xo0": [128, 8, 2048]}
    scr3 = {"hf": [NCH, 128, 2048], "ycT": [128, 16, NCH * 128]}
    scr3.update(SCR1)
    scr3.update(tot_shapes)
    res3 = run_prog([l1p1a, l1p3_sweeps, lambda fw, D: emit_tail(fw, D, 16, True)], ins3, outs3, scr3, maps3)
    out = np.zeros_like(lat1)
    for r in range(NCORE):
        b, s = divmod(r, 4)
        out[b, s * 2048:(s + 1) * 2048] = res3[r]["xo0"].transpose(2, 1, 0).reshape(2048, 1024)
    return out


def kernel(**inp):
    inp = {k: np.asarray(v, np.float32) for k, v in inp.items()}
    lat1, cx1 = layer0(inp, inp["x"], inp["ctx"])
    return layer1(inp, lat1, cx1)
```
